# Optimizing a Trainium2 kernel written in Bass

```python
import math
import jax
import jax.numpy as jnp
from jax import lax
import numpy as np

D_MODEL = 1024
BATCH = 2
SEQ = 16384
DEPTH = 4

N_MIXERS = 2
RET_HEADS = 4
RET_QK_DIM = D_MODEL // RET_HEADS
RET_V_DIM = 2 * RET_QK_DIM
RET_CHUNK = 128
ROPE_BASE = 10000.0
DIL_PATTERN = ((128, 1), (512, 4), (2048, 16))
N_GROUPS = len(DIL_PATTERN)
HEADS_PER_GROUP = 4
ATT_HEAD_DIM = 128
ATT_Q_BLOCK = 128
D_FF = (7 * D_MODEL) // 2
N_EXPERTS = 8
TOP_K = 2
MOE_BLOCK = 512
ALPHA = (2 * DEPTH) ** 0.25
BETA = (8 * DEPTH) ** -0.25
LN_EPS = 1e-5
N_RET = (DEPTH + 1) // 2
N_ATT = DEPTH // 2

kernel_name = 'hybrid_retention_dilated_attn_moe'


def layer_norm(x, g, b):
    xf = x.astype(jnp.float32)
    mu = jnp.mean(xf, axis=-1, keepdims=True)
    var = jnp.mean(jnp.square(xf - mu), axis=-1, keepdims=True)
    return ((xf - mu) * lax.rsqrt(var + LN_EPS) * g + b).astype(x.dtype)


def rotary(x, pos):
    half = x.shape[-1] // 2
    inv = ROPE_BASE ** (-jnp.arange(half, dtype=jnp.float32) / half)
    ang = pos.astype(jnp.float32)[:, None] * inv[None, :]
    cos = jnp.cos(ang)[None, :, None, :]
    sin = jnp.sin(ang)[None, :, None, :]
    x1 = x[..., :half].astype(jnp.float32)
    x2 = x[..., half:].astype(jnp.float32)
    return jnp.concatenate([x1 * cos - x2 * sin, x2 * cos + x1 * sin], axis=-1).astype(x.dtype)


def retention(h, w_in, gn_gain, w_out):
    B, S, _ = h.shape
    H, dk, dv, C = RET_HEADS, RET_QK_DIM, RET_V_DIM, RET_CHUNK
    nC = S // C
    proj = h @ w_in
    q, k, v, g = jnp.split(proj, [H * dk, 2 * H * dk, 2 * H * dk + H * dv], axis=-1)
    pos = jnp.arange(S)
    q = rotary(q.reshape(B, S, H, dk), pos)
    k = rotary(k.reshape(B, S, H, dk), pos) * (dk ** -0.5)
    v = v.reshape(B, S, H, dv)
    log_gamma = jnp.log(1.0 - 2.0 ** (-5.0 - jnp.arange(H, dtype=jnp.float32)))
    idx = jnp.arange(C, dtype=jnp.float32)
    rel = idx[:, None] - idx[None, :]
    decay_intra = jnp.where(rel >= 0, jnp.exp(log_gamma[:, None, None] * jnp.maximum(rel, 0.0)), 0.0)
    q_dec = jnp.exp(log_gamma[None, :] * (idx[:, None] + 1.0))
    k_dec = jnp.exp(log_gamma[None, :] * (C - 1.0 - idx[:, None]))
    chunk_dec = jnp.exp(log_gamma * C)
    qc = q.reshape(B, nC, C, H, dk)
    kc = k.reshape(B, nC, C, H, dk)
    vc = v.reshape(B, nC, C, H, dv)
    scores = jnp.einsum('bnihd,bnjhd->bnhij', qc, kc).astype(jnp.float32) * decay_intra[None, None]
    intra = jnp.einsum('bnhij,bnjhe->bnihe', scores.astype(vc.dtype), vc)
    xs = (jnp.moveaxis(qc * q_dec[None, None, :, :, None], 1, 0),
          jnp.moveaxis(kc * k_dec[None, None, :, :, None], 1, 0),
          jnp.moveaxis(vc, 1, 0))

    def step(R, inp):
        qn, kn, vn = inp
        out = jnp.einsum('bihd,bhde->bihe', qn, R)
        R = chunk_dec[None, :, None, None] * R + jnp.einsum('bjhd,bjhe->bhde', kn, vn)
        return R, out

    R0 = jnp.zeros((B, H, dk, dv), jnp.float32)
    _, cross = lax.scan(step, R0, xs)
    r = (intra.astype(jnp.float32) + jnp.moveaxis(cross, 0, 1).astype(jnp.float32)).reshape(B, S, H, dv)
    mu = jnp.mean(r, axis=-1, keepdims=True)
    var = jnp.mean(jnp.square(r - mu), axis=-1, keepdims=True)
    normed = ((r - mu) * lax.rsqrt(var + LN_EPS)).reshape(B, S, H * dv) * gn_gain
    return (jax.nn.silu(g) * normed.astype(g.dtype)) @ w_out


def dilated_attention(h, w_qkv, w_out):
    B, S, _ = h.shape
    G, Hg, dh, Q = N_GROUPS, HEADS_PER_GROUP, ATT_HEAD_DIM, ATT_Q_BLOCK
    proj = (h @ w_qkv).reshape(B, S, 3, G, Hg, dh)
    qkv = jnp.transpose(proj, (2, 3, 0, 4, 1, 5))
    qs = [qkv[0, gi] * (dh ** -0.5) for gi in range(G)]
    ks_ = [qkv[1, gi] for gi in range(G)]
    vs = [qkv[2, gi] for gi in range(G)]
    offs = jnp.arange(Q)

    def block(bidx):
        s0 = bidx * Q
        qpos = s0 + offs
        outs, lses = [], []
        for gi, (win, dil) in enumerate(DIL_PATTERN):
            n_keys = win // dil + 1
            kpos = qpos[:, None] - dil * jnp.arange(n_keys)[None, :]
            valid = kpos >= 0
            kidx = jnp.maximum(kpos, 0)
            qb = lax.dynamic_slice_in_dim(qs[gi], s0, Q, axis=2)
            kg = jnp.take(ks_[gi], kidx, axis=2)
            vg = jnp.take(vs[gi], kidx, axis=2)
            logits = jnp.einsum('bhqd,bhqjd->bhqj', qb, kg).astype(jnp.float32)
            logits = jnp.where(valid[None, None], logits, -jnp.inf)
            lse = jax.nn.logsumexp(logits, axis=-1)
            p = jnp.exp(logits - lse[..., None])
            outs.append(jnp.einsum('bhqj,bhqjd->bhqd', p.astype(vg.dtype), vg))
            lses.append(lse)
        wgt = jax.nn.softmax(jnp.stack(lses, axis=0), axis=0)
        o = jnp.sum(wgt[..., None] * jnp.stack(outs, axis=0).astype(jnp.float32), axis=0)
        return o.astype(h.dtype)

    o = lax.map(block, jnp.arange(S // Q))
    o = jnp.transpose(o, (1, 0, 3, 2, 4)).reshape(B, S, Hg * dh)
    return o @ w_out


def swiglu(h, w_gate, w_up, w_down):
    return (jax.nn.silu(h @ w_gate) * (h @ w_up)) @ w_down


def moe_swiglu(h, w_router, w_gate, w_up, w_down):
    B, S, D = h.shape
    T = B * S
    xt = h.reshape(T, D)
    logits = (xt @ w_router).astype(jnp.float32)
    top_val, top_idx = lax.top_k(logits, TOP_K)
    gates = jax.nn.softmax(top_val, axis=-1)
    A = T * TOP_K
    e_flat = top_idx.reshape(A)
    tok_flat = jnp.arange(A, dtype=jnp.int32) // TOP_K
    g_flat = gates.reshape(A)
    order = jnp.argsort(e_flat)
    e_sorted = e_flat[order]
    counts = jnp.bincount(e_flat, length=N_EXPERTS)
    padded = ((counts + MOE_BLOCK - 1) // MOE_BLOCK) * MOE_BLOCK
    pad_end = jnp.cumsum(padded)
    pad_start = pad_end - padded
    start = jnp.cumsum(counts) - counts
    dest = pad_start[e_sorted] + (jnp.arange(A) - start[e_sorted])
    NB = -(-A // MOE_BLOCK) + N_EXPERTS
    P = NB * MOE_BLOCK
    tok_buf = jnp.full((P,), T, jnp.int32).at[dest].set(tok_flat[order])
    gate_buf = jnp.zeros((P,), jnp.float32).at[dest].set(g_flat[order])
    blk_exp = jnp.minimum(jnp.searchsorted(pad_end, jnp.arange(NB) * MOE_BLOCK, side='right'), N_EXPERTS - 1)
    x_pad = jnp.concatenate([xt, jnp.zeros((1, D), xt.dtype)], axis=0)

    def run_block(args):
        tok, e = args
        xb = x_pad[tok]
        return swiglu(xb, w_gate[e], w_up[e], w_down[e])

    y = lax.map(run_block, (tok_buf.reshape(NB, MOE_BLOCK), blk_exp)).reshape(P, D)
    y = y * gate_buf[:, None].astype(y.dtype)
    out = jnp.zeros((T + 1, D), y.dtype).at[tok_buf].add(y)[:T]
    return out.reshape(B, S, D)


def setup_inputs(seed: int = 0) -> dict:
    key = jax.random.key(seed)
    ks = jax.random.split(key, 16)
    f32 = jnp.float32

    def nrm(k, shape, fan_in, scale=1.0):
        return jax.random.normal(k, shape, f32) * (scale * fan_in ** -0.5)

    ret_in_cols = RET_HEADS * (2 * RET_QK_DIM + 2 * RET_V_DIM)
    ret_v_width = RET_HEADS * RET_V_DIM
    att_cols = 3 * N_GROUPS * HEADS_PER_GROUP * ATT_HEAD_DIM
    att_out_in = HEADS_PER_GROUP * ATT_HEAD_DIM
    return {
        'x': jax.random.normal(ks[0], (BATCH, SEQ, D_MODEL), f32),
        'ln_gain': 1.0 + 0.01 * jax.random.normal(ks[1], (DEPTH, 2, D_MODEL), f32),
        'ln_bias': 0.01 * jax.random.normal(ks[2], (DEPTH, 2, D_MODEL), f32),
        'ret_w_in': nrm(ks[3], (N_RET, D_MODEL, ret_in_cols), D_MODEL),
        'ret_gn_gain': 1.0 + 0.01 * jax.random.normal(ks[4], (N_RET, ret_v_width), f32),
        'ret_w_out': nrm(ks[5], (N_RET, ret_v_width, D_MODEL), ret_v_width, BETA),
        'att_w_qkv': nrm(ks[6], (N_ATT, D_MODEL, att_cols), D_MODEL),
        'att_w_out': nrm(ks[7], (N_ATT, att_out_in, D_MODEL), att_out_in, BETA),
        'ffn_w_gate': nrm(ks[8], (N_RET, D_MODEL, D_FF), D_MODEL),
        'ffn_w_up': nrm(ks[9], (N_RET, D_MODEL, D_FF), D_MODEL),
        'ffn_w_down': nrm(ks[10], (N_RET, D_FF, D_MODEL), D_FF, BETA),
        'moe_w_router': nrm(ks[11], (N_ATT, D_MODEL, N_EXPERTS), D_MODEL),
        'moe_w_gate': nrm(ks[12], (N_ATT, N_EXPERTS, D_MODEL, D_FF), D_MODEL),
        'moe_w_up': nrm(ks[13], (N_ATT, N_EXPERTS, D_MODEL, D_FF), D_MODEL),
        'moe_w_down': nrm(ks[14], (N_ATT, N_EXPERTS, D_FF, D_MODEL), D_FF, BETA),
    }


def reference(x, ln_gain, ln_bias, ret_w_in, ret_gn_gain, ret_w_out, att_w_qkv, att_w_out,
              ffn_w_gate, ffn_w_up, ffn_w_down, moe_w_router, moe_w_gate, moe_w_up, moe_w_down):
    for i in range(DEPTH):
        j = i // 2
        if i % N_MIXERS == 0:
            mix = retention(x, ret_w_in[j], ret_gn_gain[j], ret_w_out[j])
        else:
            mix = dilated_attention(x, att_w_qkv[j], att_w_out[j])
        x = layer_norm(ALPHA * x + mix, ln_gain[i, 0], ln_bias[i, 0])
        if i % 2 == 0:
            f = swiglu(x, ffn_w_gate[j], ffn_w_up[j], ffn_w_down[j])
        else:
            f = moe_swiglu(x, moe_w_router[j], moe_w_gate[j], moe_w_up[j], moe_w_down[j])
        x = layer_norm(ALPHA * x + f, ln_gain[i, 1], ln_bias[i, 1])
    return x
```

```python
import numpy as np
from contextlib import ExitStack
import concourse.bass as bass
import concourse.mybir as mybir
from concourse.bass_utils import run_bass_kernel_spmd

F32 = mybir.dt.float32
BF16 = mybir.dt.bfloat16
AF = mybir.ActivationFunctionType
ALU = mybir.AluOpType
AX = mybir.AxisListType

D_MODEL = 1024
DEPTH = 4
D_FF = 3584
N_EXPERTS = 8
ALPHA = (2 * DEPTH) ** 0.25
LN_EPS = 1e-5
NCORES = 8
KC = D_MODEL // 128

ENGS = ("pe", "act", "dve", "pool", "sp")


class Buf:
    __slots__ = ("name", "w", "r", "dkey", "dcnt")

    def __init__(self, name):
        self.name = name
        self.w = None
        self.r = {}
        self.dkey = None
        self.dcnt = 0


class Prog:
    def __init__(self):
        self.nc = bass.Bass("TRN2", target_bir_lowering=False)
        self.es = ExitStack()
        self.ops = {e: [] for e in ENGS}
        self.cnt = {e: 0 for e in ENGS}
        self.seen = {e: {} for e in ENGS}
        self.sems = {}
        self.dtot = {}
        self.nbuf = 0
        for e in ("pe", "act", "dve", "pool"):
            self.sems[e] = self.es.enter_context(self.nc.semaphore("s_" + e))
        self.stack = [self.es]
        self.banks = None
        self.bank_i = 0
        self.free_dsems = []
        self.phase_sems = [[]]

    def push(self):
        st = ExitStack()
        self.stack.append(st)
        self.phase_sems.append([])

    def pop(self):
        self.barrier()
        self.flush()
        self.stack.pop().close()
        self.banks = None
        self.free_dsems.extend(self.phase_sems.pop())

    def _dsem(self, dst, prefix):
        if self.free_dsems:
            dst.dkey = self.free_dsems.pop()
            dst.dcnt = self.dtot[dst.dkey]
        else:
            dst.dkey = prefix + dst.name + "_%d" % len(self.sems)
            self.sems[dst.dkey] = self.es.enter_context(self.nc.semaphore(dst.dkey))
        self.phase_sems[-1].append(dst.dkey)

    def barrier(self):
        allev = [(k, self.cnt[k]) for k in ("pe", "act", "dve", "pool") if self.cnt[k] > 0]
        allev += [(k, self.dtot[k]) for k in self.dtot]
        for e in ENGS:
            waits = []
            for k, v in allev:
                if self.seen[e].get(k, 0) >= v:
                    continue
                self.seen[e][k] = v
                waits.append((k, v))
            if waits:
                self.ops[e].append((waits, None, None))

    def bank(self):
        if self.banks is None:
            self.banks = [(self.ps("bank%d_%d" % (i, self.nbuf), [128, 512]), self.buf("bank%d" % i)) for i in range(8)]
        b = self.banks[self.bank_i % 8]
        self.bank_i += 1
        return b

    def sb(self, name, shape, dtype):
        self.nbuf += 1
        return self.stack[-1].enter_context(self.nc.sbuf_tensor("sb%d_%s" % (self.nbuf, name), list(shape), dtype))

    def ps(self, name, shape, dtype=F32):
        self.nbuf += 1
        return self.stack[-1].enter_context(self.nc.psum_tensor("ps%d_%s" % (self.nbuf, name), list(shape), dtype))

    def dram(self, name, shape, dtype, kind):
        return self.nc.dram_tensor(name, list(shape), dtype, kind=kind).ap()

    def buf(self, name=None):
        self.nbuf += 1
        return Buf(name or ("b%d" % self.nbuf))

    def dram_or(self, io, name, shape, dtype, kind):
        if io is not None and name in io:
            return io[name]
        return self.dram(name, shape, dtype, kind)

    def allgather(self, in_ap, out_ap, dst, reads, ncores):
        if dst.dkey is None:
            self._dsem(dst, "c_")
        waits = self._waits("pool", reads, (dst,))
        dst.dcnt += 1
        self.dtot[dst.dkey] = dst.dcnt
        ev = (dst.dkey, dst.dcnt)
        fn = lambda e, i=in_ap, o=out_ap: e.collective_compute(
            "AllGather", ALU.bypass, replica_groups=[list(range(ncores))], ins=[i.opt()], outs=[o.opt()])
        self.ops["pool"].append((waits, fn, (dst.dkey, None)))
        self._commit(ev, reads, (dst,))

    def _waits(self, eng, reads, writes):
        deps = {}

        def add(ev):
            if ev is None:
                return
            k, v = ev
            if deps.get(k, 0) < v:
                deps[k] = v

        for b in reads:
            add(b.w)
        for b in writes:
            add(b.w)
            for k, v in b.r.items():
                add((k, v))
        waits = []
        for k, v in deps.items():
            if k == eng and eng == "pe":
                continue
            if self.seen[eng].get(k, 0) >= v:
                continue
            self.seen[eng][k] = v
            waits.append((k, v))
        return waits

    def _commit(self, ev, reads, writes):
        k, v = ev
        for b in reads:
            if b.r.get(k, 0) < v:
                b.r[k] = v
        for b in writes:
            b.w = ev
            b.r = {}

    def op(self, eng, fn, reads=(), writes=()):
        waits = self._waits(eng, reads, writes)
        self.cnt[eng] += 1
        ev = (eng, self.cnt[eng])
        self.ops[eng].append((waits, fn, (eng, 1)))
        self._commit(ev, reads, writes)

    def dma(self, q, out_ap, in_ap, dst, reads=(), extra_writes=()):
        if dst.dkey is None:
            self._dsem(dst, "d_")
        writes = (dst,) + tuple(extra_writes)
        waits = self._waits(q, reads, writes)
        dst.dcnt += 16
        self.dtot[dst.dkey] = dst.dcnt
        ev = (dst.dkey, dst.dcnt)
        self.ops[q].append((waits, lambda e, o=out_ap, i=in_ap: e.dma_start(out=o, in_=i), (dst.dkey, 16)))
        self._commit(ev, reads, writes)

    def finish(self, bufs, eng="sp"):
        waits = self._waits(eng, bufs, ())
        self.ops[eng].append((waits, None, None))

    def emit(self):
        self.flush()
        while self.stack:
            self.stack.pop().close()
        return self.nc

    def flush(self):
        nc = self.nc
        if not any(self.ops[e] for e in ENGS):
            return
        ops = self.ops
        self.ops = {e: [] for e in ENGS}
        with nc.Block() as block:
            def run(engname):
                def body(e):
                    for waits, fn, inc in ops[engname]:
                        for k, v in waits:
                            e.wait_ge(self.sems[k], v)
                        if fn is not None:
                            ins = fn(e)
                            if inc[1] is None:
                                ins.then_inc(self.sems[inc[0]])
                            else:
                                ins.then_inc(self.sems[inc[0]], inc[1])
                return body
            block.tensor(run("pe"))
            block.scalar(run("act"))
            block.vector(run("dve"))
            block.gpsimd(run("pool"))
            block.sync(run("sp"))


def mm_group(out_ap, pairs):
    def fn(e):
        n = len(pairs)
        ins = None
        for i, (l, r) in enumerate(pairs):
            ins = e.matmul(out_ap, l, r, start=(i == 0), stop=(i == n - 1))
        return ins
    return fn


class Common:
    def __init__(self, P, ln_g_ap, ln_b_ap, ident_ap, nslots=2):
        self.P = P
        nc = P.nc
        self.ident_f = P.sb("ident_f", [128, 128], F32)
        self.ident = P.sb("ident_bf", [128, 128], BF16)
        self.lng = P.sb("lng", [128, D_MODEL], F32)
        self.lnb = P.sb("lnb", [128, D_MODEL], F32)
        self.neghalf = P.sb("neghalf", [128, 1], F32)
        self.b_ident = P.buf("ident")
        self.b_identf = P.buf("identf")
        self.b_lng = P.buf("lng")
        self.b_lnb = P.buf("lnb")
        self.b_nh = P.buf("nh")
        P.dma("sp", self.ident_f[:], ident_ap, self.b_identf)
        P.dma("sp", self.lng[:], ln_g_ap, self.b_lng)
        P.dma("sp", self.lnb[:], ln_b_ap, self.b_lnb)
        P.op("dve", lambda e: e.tensor_copy(self.ident[:], self.ident_f[:]), reads=[self.b_identf], writes=[self.b_ident])
        P.op("pool", lambda e: e.memset(self.neghalf[:], -0.5), writes=[self.b_nh])
        NS = self.NS = nslots
        self.s = [P.sb("ln_s%d" % i, [128, D_MODEL], F32) for i in range(NS)]
        self.b_s = [P.buf("ln_s%d" % i) for i in range(NS)]
        self.st = [P.sb("ln_st%d" % i, [128, 2, 6], F32) for i in range(NS)]
        self.mv = [P.sb("ln_mv%d" % i, [128, 2], F32) for i in range(NS)]
        self.rs = [P.sb("ln_rs%d" % i, [128, 2], F32) for i in range(NS)]
        self.b_small = [P.buf("ln_small%d" % i) for i in range(NS)]
        self.xo = [P.sb("ln_xo%d" % i, [128, D_MODEL], F32) for i in range(NS)]
        self.b_xo = [P.buf("ln_xo%d" % i) for i in range(NS)]
        self.k = 0

    def layernorm(self, fill_s, fill_reads, out_dram_ap, out_buf, post=None):
        P = self.P
        i = self.k % self.NS
        self.k += 1
        s, st, mv, rs, xo = self.s[i], self.st[i], self.mv[i], self.rs[i], self.xo[i]
        bs, bsm, bxo = self.b_s[i], self.b_small[i], self.b_xo[i]
        for eng, fn in fill_s(s):
            P.op(eng, fn, reads=fill_reads, writes=[bs])
        P.op("dve", lambda e: e.bn_stats(st[:, 0, :], s[:, 0:512]), reads=[bs], writes=[bsm])
        P.op("dve", lambda e: e.bn_stats(st[:, 1, :], s[:, 512:1024]), reads=[bs], writes=[bsm])
        P.op("dve", lambda e: e.bn_aggr(mv[:], st[:].rearrange("p a b -> p (a b)")), reads=[bsm], writes=[bsm])
        P.op("pool", lambda e: e.tensor_scalar(rs[:, 0:1], mv[:, 1:2], LN_EPS, None, ALU.add), reads=[bsm], writes=[bsm])
        P.op("pool", lambda e: e.tensor_tensor(rs[:, 1:2], rs[:, 0:1], self.neghalf[:], ALU.pow), reads=[bsm, self.b_nh], writes=[bsm])
        P.op("dve", lambda e: e.tensor_scalar(s[:], s[:], mv[:, 0:1], rs[:, 1:2], ALU.subtract, ALU.mult), reads=[bs, bsm], writes=[bs])
        P.op("pool", lambda e: e.tensor_tensor(xo[:], s[:], self.lng[:], ALU.mult), reads=[bs, self.b_lng], writes=[bxo])
        P.op("pool", lambda e: e.tensor_tensor(xo[:], xo[:], self.lnb[:], ALU.add), reads=[bxo, self.b_lnb], writes=[bxo])
        if out_dram_ap is not None:
            P.dma("sp", out_dram_ap, xo[:], out_buf, reads=[bxo])
        if post is not None:
            post(xo, bxo)


def build_ffn(T, E, TG=1024, P=None, io=None):
    own = P is None
    if own:
        P = Prog()
    P.push()
    nc = P.nc
    NT = T // 128
    NG = T // TG
    TPG = TG // 128
    NTB = TG // 512
    NJB = D_FF // 512

    x_d = P.dram_or(io, "x", [T, D_MODEL], F32, "ExternalInput")
    y_d = P.dram_or(io, "y", [T, D_MODEL], F32, "ExternalOutput")
    lng_d = P.dram_or(io, "ln_g", [128, D_MODEL], F32, "ExternalInput")
    lnb_d = P.dram_or(io, "ln_b", [128, D_MODEL], F32, "ExternalInput")
    id_d = P.dram_or(io, "ident", [128, 128], F32, "ExternalInput")
    wg_d = P.dram_or(io, "wg", [E, D_MODEL, D_FF], F32, "ExternalInput")
    wu_d = P.dram_or(io, "wu", [E, D_MODEL, D_FF], F32, "ExternalInput")
    wd_d = P.dram_or(io, "wd", [E, D_FF, D_MODEL], F32, "ExternalInput")
    if E > 1:
        wr_d = P.dram_or(io, "wr", [D_MODEL, E], F32, "ExternalInput")

    C = Common(P, lng_d, lnb_d, id_d)
    b_y = P.buf("y_dram")

    xT = P.sb("xT", [128, KC, TG], BF16)
    b_xT = [P.buf("xT%d" % t) for t in range(TPG)]
    acc = P.sb("acc", [128, TPG, D_MODEL], F32)
    b_acc = [P.buf("acc%d" % t) for t in range(TPG)]
    xs = [P.sb("xs%d" % i, [128, D_MODEL], F32) for i in range(2)]
    b_xs = [P.buf("xs%d" % i) for i in range(2)]
    xb = [P.sb("xb%d" % i, [128, D_MODEL], BF16) for i in range(2)]
    b_xb = [P.buf("xb%d" % i) for i in range(2)]
    wg_s = [P.sb("wg%d" % i, [128, KC, 512], BF16) for i in range(2)]
    wu_s = [P.sb("wu%d" % i, [128, KC, 512], BF16) for i in range(2)]
    wd_s = [P.sb("wd%d" % i, [128, 4, D_MODEL], BF16) for i in range(2)]
    b_wg = [P.buf("wg%d" % i) for i in range(2)]
    b_wu = [P.buf("wu%d" % i) for i in range(2)]
    b_wd = [P.buf("wd%d" % i) for i in range(2)]
    hT = [P.sb("hT%d" % i, [128, 4, 512], BF16) for i in range(2)]
    b_hT = [[P.buf("hT%d_%d" % (i, c)) for c in range(4)] for i in range(2)]
    sg = [P.sb("sg%d" % i, [128, 512], F32) for i in range(2)]
    b_sg = [P.buf("sg%d" % i) for i in range(2)]
    if E > 1:
        wr_f = P.sb("wr_f", [128, KC, E], F32)
        wr_s = P.sb("wr_s", [128, KC, E], BF16)
        b_wrf = P.buf("wrf")
        b_wr = P.buf("wr")
        lg = P.sb("lg", [128, TPG, E], F32)
        top = P.sb("top", [128, TPG, 8], F32)
        gsm = P.sb("gsm", [128, TPG, 4], F32)
        gA = P.sb("gA", [128, TPG, E], F32)
        gates = P.sb("gates", [128, TPG, E], F32)
        b_g = [P.buf("gate%d" % t) for t in range(TPG)]
        P.dma("sp", wr_f[:], wr_d.rearrange("(k p) e -> p k e", p=128), b_wrf)
        P.op("dve", lambda e: e.tensor_copy(wr_s[:], wr_f[:]), reads=[b_wrf], writes=[b_wr])

    pg = [P.ps("pg%d" % i, [128, 512]) for i in range(2)]
    pu = [P.ps("pu%d" % i, [128, 512]) for i in range(2)]
    pd = [P.ps("pd%d" % i, [128, 512]) for i in range(2)]
    pt = [P.ps("pt%d" % i, [128, 512]) for i in range(2)]
    b_pg = [P.buf("pg%d" % i) for i in range(2)]
    b_pu = [P.buf("pu%d" % i) for i in range(2)]
    b_pd = [P.buf("pd%d" % i) for i in range(2)]
    b_pt = [P.buf("pt%d" % i) for i in range(2)]

    blocks = [(g, e, jb) for g in range(NG) for e in range(E) for jb in range(NJB)]

    def load_w(idx):
        g, e, jb = blocks[idx]
        s = idx % 2
        c0 = jb * 512
        P.dma("pool", wg_s[s][:], wg_d[e, :, c0:c0 + 512].rearrange("(k p) c -> p k c", p=128), b_wg[s])
        P.dma("pool", wu_s[s][:], wu_d[e, :, c0:c0 + 512].rearrange("(k p) c -> p k c", p=128), b_wu[s])
        P.dma("pool", wd_s[s][:], wd_d[e, c0:c0 + 512, :].rearrange("(k p) c -> p k c", p=128), b_wd[s])

    load_w(0)
    if len(blocks) > 1:
        load_w(1)

    cchunk = [0]
    cdown = [0]
    cunit = [0]

    def emit_gu(idx, tb):
        g, e, jb = blocks[idx]
        s = idx % 2
        hs = cunit[0] % 2
        for c in range(4):
            k = cchunk[0] % 2
            cchunk[0] += 1
            rhs = lambda kc: xT[:, kc, tb * 512:(tb + 1) * 512]
            rd = [b_xT[tb * 4 + i] for i in range(4)]
            P.op("pe", mm_group(pg[k][:], [(wg_s[s][:, kc, c * 128:(c + 1) * 128], rhs(kc)) for kc in range(KC)]),
                 reads=[b_wg[s]] + rd, writes=[b_pg[k]])
            P.op("pe", mm_group(pu[k][:], [(wu_s[s][:, kc, c * 128:(c + 1) * 128], rhs(kc)) for kc in range(KC)]),
                 reads=[b_wu[s]] + rd, writes=[b_pu[k]])
            P.op("act", lambda en, k=k: en.activation(sg[k][:], pg[k][:], AF.Silu), reads=[b_pg[k]], writes=[b_sg[k]])
            P.op("dve", lambda en, k=k, c=c, hs=hs: en.tensor_tensor(hT[hs][:, c, :], sg[k][:], pu[k][:], ALU.mult),
                 reads=[b_sg[k], b_pu[k]], writes=[b_hT[hs][c]])
        u = (idx, tb, hs)
        cunit[0] += 1
        return u

    def emit_down(u, first):
        idx, tb, hs = u
        g, e, jb = blocks[idx]
        s = idx % 2
        for t in range(4):
            tt = tb * 4 + t
            for nb in range(2):
                k = cdown[0] % 2
                cdown[0] += 1
                P.op("pe", mm_group(pd[k][:], [(hT[hs][:, c, t * 128:(t + 1) * 128], wd_s[s][:, c, nb * 512:(nb + 1) * 512]) for c in range(4)]),
                     reads=[b_wd[s]] + b_hT[hs], writes=[b_pd[k]])
                a = acc[:, tt, nb * 512:(nb + 1) * 512]
                if E > 1:
                    gsc = gates[:, tt, e:e + 1]
                    if first:
                        P.op("dve", lambda en, a=a, k=k, gsc=gsc: en.tensor_scalar(a, pd[k][:], gsc, None, ALU.mult),
                             reads=[b_pd[k], b_g[tt]], writes=[b_acc[tt]])
                    else:
                        P.op("dve", lambda en, a=a, k=k, gsc=gsc: en.scalar_tensor_tensor(a, pd[k][:], gsc, a, ALU.mult, ALU.add),
                             reads=[b_pd[k], b_g[tt], b_acc[tt]], writes=[b_acc[tt]])
                else:
                    if first:
                        P.op("dve", lambda en, a=a, k=k: en.tensor_copy(a, pd[k][:]), reads=[b_pd[k]], writes=[b_acc[tt]])
                    else:
                        P.op("dve", lambda en, a=a, k=k: en.tensor_tensor(a, a, pd[k][:], ALU.add),
                             reads=[b_pd[k], b_acc[tt]], writes=[b_acc[tt]])

    xcount = [0]

    def load_x_tile(g, t):
        i = xcount[0] % 2
        xcount[0] += 1
        r0 = g * TG + t * 128
        P.dma("sp", xs[i][:], x_d[r0:r0 + 128, :], b_xs[i])
        return i

    bidx = 0
    for g in range(NG):
        for t in range(TPG):
            i = load_x_tile(g, t)
            P.op("act", lambda en, i=i: en.copy(xb[i][:], xs[i][:]), reads=[b_xs[i]], writes=[b_xb[i]])
            for half in range(2):
                for q in range(4):
                    kc = half * 4 + q
                    P.op("pe", mm_group(pt[half][:, q * 128:(q + 1) * 128], [(xb[i][:, kc * 128:(kc + 1) * 128], C.ident[:])]),
                         reads=[b_xb[i], C.b_ident], writes=[b_pt[half]])
                P.op("dve", lambda en, half=half, t=t: en.tensor_copy(
                    xT[:, half * 4:(half + 1) * 4, t * 128:(t + 1) * 128], pt[half][:].rearrange("p (a b) -> p a b", a=4)),
                    reads=[b_pt[half]], writes=[b_xT[t]])
            if E > 1:
                P.op("pe", mm_group(pt[0][:, 0:E], [(xT[:, kc, t * 128:(t + 1) * 128], wr_s[:, kc, :]) for kc in range(KC)]),
                     reads=[b_xT[t], b_wr], writes=[b_pt[0]])
                L = lg[:, t, :]
                P.op("dve", lambda en, L=L: en.tensor_copy(L, pt[0][:, 0:E]), reads=[b_pt[0]], writes=[b_g[t]])
                P.op("dve", lambda en, L=L, t=t: en.max(top[:, t, :], L), reads=[b_g[t]], writes=[b_g[t]])
                P.op("dve", lambda en, t=t: en.tensor_tensor(gsm[:, t, 0:1], top[:, t, 1:2], top[:, t, 0:1], ALU.subtract), reads=[b_g[t]], writes=[b_g[t]])
                P.op("act", lambda en, t=t: en.activation(gsm[:, t, 1:2], gsm[:, t, 0:1], AF.Exp), reads=[b_g[t]], writes=[b_g[t]])
                P.op("dve", lambda en, t=t: en.tensor_scalar(gsm[:, t, 2:3], gsm[:, t, 1:2], 1.0, None, ALU.add), reads=[b_g[t]], writes=[b_g[t]])
                P.op("dve", lambda en, t=t: en.reciprocal(gsm[:, t, 2:3], gsm[:, t, 2:3]), reads=[b_g[t]], writes=[b_g[t]])
                P.op("dve", lambda en, t=t: en.tensor_tensor(gsm[:, t, 3:4], gsm[:, t, 1:2], gsm[:, t, 2:3], ALU.mult), reads=[b_g[t]], writes=[b_g[t]])
                P.op("dve", lambda en, t=t: en.tensor_tensor(gsm[:, t, 0:1], gsm[:, t, 2:3], gsm[:, t, 3:4], ALU.subtract), reads=[b_g[t]], writes=[b_g[t]])
                P.op("dve", lambda en, L=L, t=t: en.tensor_scalar(gA[:, t, :], L, top[:, t, 1:2], gsm[:, t, 3:4], ALU.is_ge, ALU.mult), reads=[b_g[t]], writes=[b_g[t]])
                P.op("dve", lambda en, L=L, t=t: en.tensor_scalar(gates[:, t, :], L, top[:, t, 0:1], gsm[:, t, 0:1], ALU.is_ge, ALU.mult), reads=[b_g[t]], writes=[b_g[t]])
                P.op("dve", lambda en, t=t: en.tensor_tensor(gates[:, t, :], gates[:, t, :], gA[:, t, :], ALU.add), reads=[b_g[t]], writes=[b_g[t]])
        pending = None
        for e in range(E):
            for jb in range(NJB):
                first = (e == 0 and jb == 0)
                for tb in range(NTB):
                    u = emit_gu(bidx, tb)
                    if pending is not None:
                        emit_down(*pending)
                    pending = (u, first)
                bidx += 1
                if bidx + 1 < len(blocks):
                    if pending is not None:
                        emit_down(*pending)
                        pending = None
                    load_w(bidx + 1)
        if pending is not None:
            emit_down(*pending)
            pending = None
        for t in range(TPG):
            i = load_x_tile(g, t)
            r0 = g * TG + t * 128

            def fill(s, i=i, t=t):
                return [("dve", lambda en: en.scalar_tensor_tensor(s[:], xs[i][:], float(ALPHA), acc[:, t, :], ALU.mult, ALU.add))]
            C.layernorm(fill, [b_xs[i], b_acc[t]], y_d[r0:r0 + 128, :], b_y)
    P.finish([b_y])
    P.pop()
    if own:
        return P.emit()


def _rep(v):
    return np.ascontiguousarray(np.broadcast_to(np.asarray(v, np.float32)[None, :], (128, v.shape[-1])))


def run_ffn(x_shards, ln_g, ln_b, wg, wu, wd, wr=None, TG=1024):
    T = x_shards[0].shape[0]
    E = wg.shape[0]
    nc = build_ffn(T, E, TG=min(TG, T))
    ident = np.eye(128, dtype=np.float32)
    maps = []
    for xs in x_shards:
        m = {"x": np.ascontiguousarray(xs), "ln_g": _rep(ln_g), "ln_b": _rep(ln_b), "ident": ident,
             "wg": wg, "wu": wu, "wd": wd}
        if E > 1:
            m["wr"] = wr
        maps.append(m)
    res = run_bass_kernel_spmd(nc, maps, core_ids=list(range(len(maps))))
    return [r["y"] for r in res.results]


def emit_proj_ln(P, T, KD, x_d, u_d, w_d, rowgain_d, lng_d, lnb_d, id_d, y_d, b_u, src_fm):
    NE = KD // 128
    NTL = T // 128
    P.push()
    C = Common(P, lng_d, lnb_d, id_d, nslots=4)
    b_y = P.buf("y_dram")
    w_s = P.sb("pw", [128, NE, D_MODEL], BF16)
    b_w = [P.buf("pw%d" % i) for i in range(NE)]
    if rowgain_d is not None:
        rg = P.sb("rg", [128, NE], F32)
        b_rg = P.buf("rg")
        P.dma("sp", rg[:], rowgain_d, b_rg)
        wst = [P.sb("wst%d" % i, [128, D_MODEL], F32) for i in range(2)]
        b_wst = [P.buf("wst%d" % i) for i in range(2)]
        for ec in range(NE):
            i = ec % 2
            P.dma("sp", wst[i][:], w_d[ec * 128:(ec + 1) * 128, :], b_wst[i])
            P.op("dve", lambda en, i=i, ec=ec: en.tensor_scalar(w_s[:, ec, :], wst[i][:], rg[:, ec:ec + 1], None, ALU.mult),
                 reads=[b_wst[i], b_rg], writes=[b_w[ec]])
    else:
        for ec in range(NE):
            P.dma("pool", w_s[:, ec, :], w_d[ec * 128:(ec + 1) * 128, :], b_w[ec])
    ut = [P.sb("ut%d" % i, [128, KD], BF16) for i in range(4)]
    b_ut = [P.buf("ut%d" % i) for i in range(4)]
    uT = [P.sb("uT%d" % i, [128, NE, 128], BF16) for i in range(4)]
    b_uT = [P.buf("uT%d" % i) for i in range(4)]
    xs = [P.sb("pxs%d" % i, [128, D_MODEL], F32) for i in range(4)]
    b_xs = [P.buf("pxs%d" % i) for i in range(4)]
    for t in range(NTL):
        i = t % 4
        r0 = t * 128
        P.dma("sp", xs[i][:], x_d[r0:r0 + 128, :], b_xs[i])
        if src_fm:
            for ec in range(NE):
                P.dma("sp", uT[i][:, ec, :], u_d[ec * 128:(ec + 1) * 128, r0:r0 + 128], b_uT[i], reads=[b_u])
        else:
            P.dma("sp", ut[i][:], u_d[r0:r0 + 128, :], b_ut[i], reads=[b_u])
            for q4 in range(NE // 4):
                bk, bb = P.bank()
                for j in range(4):
                    ec = q4 * 4 + j
                    P.op("pe", mm_group(bk[:, j * 128:(j + 1) * 128], [(ut[i][:, ec * 128:(ec + 1) * 128], C.ident[:])]),
                         reads=[b_ut[i], C.b_ident], writes=[bb])
                P.op("act", lambda en, q4=q4, bk=bk, i=i: en.copy(uT[i][:, q4 * 4:(q4 + 1) * 4, :], bk[:].rearrange("p (a b) -> p a b", a=4)),
                     reads=[bb], writes=[b_uT[i]])
        bks = [P.bank(), P.bank()]
        for nb in range(2):
            P.op("pe", mm_group(bks[nb][0][:], [(uT[i][:, ec, :], w_s[:, ec, nb * 512:(nb + 1) * 512]) for ec in range(NE)]),
                 reads=[b_uT[i]] + b_w, writes=[bks[nb][1]])

        def fill(s, i=i, bks=bks):
            return [("dve", lambda en, nb=nb: en.scalar_tensor_tensor(s[:, nb * 512:(nb + 1) * 512], xs[i][:, nb * 512:(nb + 1) * 512],
                                                                     float(ALPHA), bks[nb][0][:], ALU.mult, ALU.add)) for nb in range(2)]
        C.layernorm(fill, [b_xs[i], bks[0][1], bks[1][1]], y_d[r0:r0 + 128, :], b_y)
    P.finish([b_y])
    P.pop()


RET_H = 4
GAMMA = [1.0 - 2.0 ** (-5.0 - h) for h in range(RET_H)]


def build_ret(T, mode, P=None, io=None, gathered=False):
    own = P is None
    if own:
        P = Prog()
    NCH = T // 128
    cdec = [g ** 128 for g in GAMMA]
    full = (mode == "B")
    x_d = P.dram_or(io, "x", [T, D_MODEL], F32, "ExternalInput")
    win_d = P.dram_or(io, "w_in", [D_MODEL, 6144], F32, "ExternalInput")
    id_d = P.dram_or(io, "ident", [128, 128], F32, "ExternalInput")
    cos_d = P.dram_or(io, "cosT", [128, T], F32, "ExternalInput")
    sin_d = P.dram_or(io, "sinT", [128, T], F32, "ExternalInput")
    kdec_d = P.dram_or(io, "kdec", [128, RET_H], F32, "ExternalInput")
    if full:
        y_d = P.dram_or(io, "y", [T, D_MODEL], F32, "ExternalOutput")
        wout_d = P.dram_or(io, "w_out", [2048, D_MODEL], F32, "ExternalInput")
        gn_d = P.dram_or(io, "gn", [128, 16], F32, "ExternalInput")
        lng_d = P.dram_or(io, "ln_g", [128, D_MODEL], F32, "ExternalInput")
        lnb_d = P.dram_or(io, "ln_b", [128, D_MODEL], F32, "ExternalInput")
        dm_d = P.dram_or(io, "dmT", [128, RET_H, 128], F32, "ExternalInput")
        qdec_d = P.dram_or(io, "qdec", [128, RET_H, 128], F32, "ExternalInput")
        if gathered:
            rg_d = io["rg"]
            cf_d = io["ret_cf"]
        else:
            rp_d = P.dram("rprev", [3, RET_H, 2, 128, 512], F32, "ExternalInput")
        u_d = P.dram_or(io, "u_scr", [T, 2048], BF16, "Internal")
        b_u = P.buf("u_dram")
    else:
        r_d = P.dram_or(io, "r_out", [RET_H, 2, 128, 512], F32, "ExternalOutput")
        b_rd = P.buf("r_dram")

    P.push()
    ident_f = P.sb("ident_f", [128, 128], F32)
    ident = P.sb("ident", [128, 128], BF16)
    b_idf, b_id = P.buf("idf"), P.buf("id")
    P.dma("sp", ident_f[:], id_d, b_idf)
    P.op("dve", lambda e: e.tensor_copy(ident[:], ident_f[:]), reads=[b_idf], writes=[b_id])
    kdec = P.sb("kdec", [128, RET_H], F32)
    b_kdec = P.buf("kdec")
    P.dma("sp", kdec[:], kdec_d, b_kdec)
    w_in = P.sb("w_in", [128, KC, 6144], BF16)
    b_win = [P.buf("win%d" % k) for k in range(KC)]
    for kc in range(KC):
        P.dma("pool", w_in[:, kc, :], win_d[kc * 128:(kc + 1) * 128, :], b_win[kc])
    R = P.sb("R", [128, RET_H, 2, 512], F32)
    Rb = P.sb("Rb", [128, RET_H, 2, 512], BF16)
    b_R = [P.buf("R%d" % h) for h in range(RET_H)]
    b_Rb = [P.buf("Rb%d" % h) for h in range(RET_H)]
    if full:
        dmT = P.sb("dmT", [128, RET_H, 128], F32)
        qdec = P.sb("qdec", [128, RET_H, 128], F32)
        b_dm, b_qdec = P.buf("dm"), P.buf("qdec")
        P.dma("sp", dmT[:], dm_d, b_dm)
        P.dma("sp", qdec[:], qdec_d, b_qdec)
        epsb = P.sb("eps_b", [128, 1], F32)
        nhb = P.sb("nh_b", [128, 1], F32)
        b_cst = P.buf("cst")
        P.op("pool", lambda e: e.memset(nhb[:], -0.5), writes=[b_cst])
        rtmp = [P.sb("rtmp%d" % i, [128, 2, 512], F32) for i in range(2)]
        b_rtmp = [P.buf("rtmp%d" % i) for i in range(2)]
        cnt = 0
        if gathered:
            cf = P.sb("ret_cf", [128, NCORES * RET_H], F32)
            b_cf = P.buf("ret_cf")
            P.dma("sp", cf[:], cf_d, b_cf)
            for p in range(NCORES):
                for h in range(RET_H):
                    i = cnt % 2
                    cnt += 1
                    csc = cf[:, p * RET_H + h:p * RET_H + h + 1]
                    P.dma("sp", rtmp[i][:], rg_d[p, h].rearrange("c p e -> p c e"), b_rtmp[i])
                    if p == 0:
                        P.op("dve", lambda en, i=i, h=h, csc=csc: en.tensor_scalar(R[:, h, :, :], rtmp[i][:], csc, None, ALU.mult),
                             reads=[b_rtmp[i], b_cf], writes=[b_R[h]])
                    else:
                        P.op("dve", lambda en, i=i, h=h, csc=csc: en.scalar_tensor_tensor(R[:, h, :, :], rtmp[i][:], csc, R[:, h, :, :], ALU.mult, ALU.add),
                             reads=[b_rtmp[i], b_cf, b_R[h]], writes=[b_R[h]])
        else:
            for h in range(RET_H):
                P.dma("sp", R[:, h, :, :], rp_d[0, h].rearrange("c p e -> p c e"), b_R[h])
            for k in (1, 2):
                for h in range(RET_H):
                    coef = float(GAMMA[h] ** (T * k))
                    i = cnt % 2
                    cnt += 1
                    P.dma("sp", rtmp[i][:], rp_d[k, h].rearrange("c p e -> p c e"), b_rtmp[i])
                    P.op("dve", lambda en, i=i, h=h, coef=coef: en.scalar_tensor_tensor(R[:, h, :, :], rtmp[i][:], coef, R[:, h, :, :], ALU.mult, ALU.add),
                         reads=[b_rtmp[i], b_R[h]], writes=[b_R[h]])
        for h in range(RET_H):
            P.op("act", lambda en, h=h: en.copy(Rb[:, h, :, :], R[:, h, :, :]), reads=[b_R[h]], writes=[b_Rb[h]])
    else:
        for h in range(RET_H):
            P.op("pool", lambda en, h=h: en.memset(R[:, h, :, :], 0.0), writes=[b_R[h]])

    def dbl(name, shape, dt):
        return [P.sb("%s%d" % (name, i), shape, dt) for i in range(2)], [P.buf("%s%d" % (name, i)) for i in range(2)]
    xb, b_xb = dbl("xb", [128, D_MODEL], BF16)
    xT, b_xT = dbl("xT", [128, KC, 128], BF16)
    cs, b_cs = dbl("cs", [128, 2, 128], F32)
    qk = [[P.sb("qk%d_%d" % (i, h), [128, 4, 128], BF16) for h in range(RET_H)] for i in range(2)]
    b_qk = [[P.buf("qk%d_%d" % (i, h)) for h in range(RET_H)] for i in range(2)]
    kd = [[P.sb("kd%d_%d" % (i, h), [128, 256], BF16) for h in range(RET_H)] for i in range(2)]
    b_kd = [[P.buf("kd%d_%d" % (i, h)) for h in range(RET_H)] for i in range(2)]
    v, b_v = dbl("v", [128, 2048], BF16)
    b_vh = [[P.buf("v%d_%d" % (i, h)) for h in range(RET_H)] for i in range(2)]
    t1, b_t1 = dbl("t1", [128, 4, 128], F32)
    t2, b_t2 = dbl("t2", [128, 4, 128], F32)
    if full:
        sgt = [P.sb("sgt%d" % i, [128, 2048], BF16) for i in range(2)]
        b_sgt = [[P.buf("sgt%d_%d" % (i, h)) for h in range(RET_H)] for i in range(2)]
        PT = P.sb("PT", [128, RET_H, 128], BF16)
        b_PT = P.buf("PT")
        qd = [P.sb("qd%d" % h, [128, 2, 128], BF16) for h in range(RET_H)]
        b_qd = [P.buf("qd%d" % h) for h in range(RET_H)]
        u = [P.sb("u%d" % i, [128, 2048], BF16) for i in range(2)]
        b_us = [P.buf("u%d" % i) for i in range(2)]
        gst = P.sb("gst", [128, RET_H, 6], F32)
        gmv = P.sb("gmv", [128, RET_H, 2], F32)
        grs = P.sb("grs", [128, RET_H, 2], F32)
        b_gs = [P.buf("gs%d" % h) for h in range(RET_H)]

    def stage1(n):
        i = n % 2
        r0 = n * 128
        P.dma("pool", xb[i][:], x_d[r0:r0 + 128, :], b_xb[i])
        P.dma("sp", cs[i][:, 0, :], cos_d[:, r0:r0 + 128], b_cs[i])
        P.dma("sp", cs[i][:, 1, :], sin_d[:, r0:r0 + 128], b_cs[i])
        for half in range(2):
            bk, bb = P.bank()
            for q in range(4):
                kc = half * 4 + q
                P.op("pe", mm_group(bk[:, q * 128:(q + 1) * 128], [(xb[i][:, kc * 128:(kc + 1) * 128], ident[:])]),
                     reads=[b_xb[i], b_id], writes=[bb])
            P.op("dve", lambda en, half=half, bk=bk: en.tensor_copy(xT[i][:, half * 4:(half + 1) * 4, :], bk[:].rearrange("p (a b) -> p a b", a=4)),
                 reads=[bb], writes=[b_xT[i]])
        for h in range(RET_H):
            bk, bb = P.bank()
            for q4, cc in enumerate([2 * h, 2 * h + 1, 8 + 2 * h, 8 + 2 * h + 1]):
                P.op("pe", mm_group(bk[:, q4 * 128:(q4 + 1) * 128], [(w_in[:, kc, cc * 128:(cc + 1) * 128], xT[i][:, kc, :]) for kc in range(KC)]),
                     reads=b_win + [b_xT[i]], writes=[bb])
            j = h % 2
            bk3 = bk[:].rearrange("p (a b) -> p a b", a=4)
            cosb = cs[i][:, 0, :].unsqueeze(1).broadcast_to([128, 4, 128])
            sinb = cs[i][:, 1, :].unsqueeze(1).broadcast_to([128, 4, 128])
            P.op("dve", lambda en, j=j, bk3=bk3, cosb=cosb: en.tensor_tensor(t1[j][:], bk3, cosb, ALU.mult), reads=[bb, b_cs[i]], writes=[b_t1[j]])
            P.op("dve", lambda en, j=j, bk3=bk3, sinb=sinb: en.tensor_tensor(t2[j][:], bk3, sinb, ALU.mult), reads=[bb, b_cs[i]], writes=[b_t2[j]])
            t1v = t1[j][:].rearrange("p (a b) c -> p a b c", a=2)
            t2v = t2[j][:].rearrange("p (a b) c -> p a b c", a=2)
            qkv = qk[i][h][:].rearrange("p (a b) c -> p a b c", a=2)
            P.op("pool", lambda en, t1v=t1v, t2v=t2v, qkv=qkv: en.tensor_tensor(qkv[:, :, 0, :], t1v[:, :, 0, :], t2v[:, :, 1, :], ALU.subtract),
                 reads=[b_t1[j], b_t2[j]], writes=[b_qk[i][h]])
            P.op("pool", lambda en, t1v=t1v, t2v=t2v, qkv=qkv: en.tensor_tensor(qkv[:, :, 1, :], t1v[:, :, 1, :], t2v[:, :, 0, :], ALU.add),
                 reads=[b_t1[j], b_t2[j], b_qk[i][h]], writes=[b_qk[i][h]])
            bk2, bb2 = P.bank()
            for dc in range(2):
                P.op("pe", mm_group(bk2[:, dc * 128:(dc + 1) * 128], [(qk[i][h][:, 2 + dc, :], ident[:])]), reads=[b_qk[i][h], b_id], writes=[bb2])
            P.op("act", lambda en, h=h, bk2=bk2: en.activation(kd[i][h][:], bk2[:, 0:256], AF.Copy, scale=kdec[:, h:h + 1]),
                 reads=[bb2, b_kdec], writes=[b_kd[i][h]])
        for nb in range(8 if full else 4):
            bk, bb = P.bank()
            c0 = 2048 + nb * 512
            P.op("pe", mm_group(bk[:], [(xT[i][:, kc, :], w_in[:, kc, c0:c0 + 512]) for kc in range(KC)]), reads=b_win + [b_xT[i]], writes=[bb])
            if nb < 4:
                P.op("act", lambda en, nb=nb, bk=bk: en.copy(v[i][:, nb * 512:(nb + 1) * 512], bk[:]), reads=[bb], writes=[b_vh[i][nb]])
            else:
                hh = nb - 4
                P.op("act", lambda en, hh=hh, bk=bk: en.activation(sgt[i][:, hh * 512:(hh + 1) * 512], bk[:], AF.Silu), reads=[bb], writes=[b_sgt[i][hh]])

    def stage2(n):
        i = n % 2
        r0 = n * 128
        if full:
            bkS, bbS = P.bank()
            for h in range(RET_H):
                P.op("pe", mm_group(bkS[:, h * 128:(h + 1) * 128], [(qk[i][h][:, 2, :], qk[i][h][:, 0, :]), (qk[i][h][:, 3, :], qk[i][h][:, 1, :])]),
                     reads=[b_qk[i][h]], writes=[bbS])
            P.op("dve", lambda en, bkS=bkS: en.tensor_tensor(PT[:], bkS[:].rearrange("p (a b) -> p a b", a=4), dmT[:], ALU.mult),
                 reads=[bbS, b_dm], writes=[b_PT])
            for h in range(RET_H):
                qdb = qdec[:, h, :].unsqueeze(1).broadcast_to([128, 2, 128])
                P.op("pool", lambda en, h=h, qdb=qdb: en.tensor_tensor(qd[h][:], qk[i][h][:, 0:2, :], qdb, ALU.mult),
                     reads=[b_qk[i][h], b_qdec], writes=[b_qd[h]])
                bkR, bbR = P.bank()
                P.op("pe", mm_group(bkR[:], [(PT[:, h, :], v[i][:, h * 512:(h + 1) * 512]),
                                            (qd[h][:, 0, :], Rb[:, h, 0, :]), (qd[h][:, 1, :], Rb[:, h, 1, :])]),
                     reads=[b_PT, b_vh[i][h], b_qd[h], b_Rb[h]], writes=[bbR])
                P.op("dve", lambda en, h=h, bkR=bkR: en.bn_stats(gst[:, h, :], bkR[:]), reads=[bbR], writes=[b_gs[h]])
                P.op("dve", lambda en, h=h: en.bn_aggr(gmv[:, h, :], gst[:, h, :]), reads=[b_gs[h]], writes=[b_gs[h]])
                P.op("pool", lambda en, h=h: en.tensor_scalar(grs[:, h, 0:1], gmv[:, h, 1:2], LN_EPS, None, ALU.add), reads=[b_gs[h]], writes=[b_gs[h]])
                P.op("pool", lambda en, h=h: en.tensor_tensor(grs[:, h, 1:2], grs[:, h, 0:1], nhb[:], ALU.pow), reads=[b_gs[h], b_cst], writes=[b_gs[h]])
                us = u[i][:, h * 512:(h + 1) * 512]
                P.op("dve", lambda en, h=h, bkR=bkR, us=us: en.tensor_scalar(us, bkR[:], gmv[:, h, 0:1], grs[:, h, 1:2], ALU.subtract, ALU.mult),
                     reads=[bbR, b_gs[h]], writes=[b_us[i]])
                P.op("pool", lambda en, h=h, us=us: en.tensor_tensor(us, us, sgt[i][:, h * 512:(h + 1) * 512], ALU.mult),
                     reads=[b_us[i], b_sgt[i][h]], writes=[b_us[i]])
            P.dma("sp", u_d[r0:r0 + 128, :], u[i][:], b_u, reads=[b_us[i]])
        for h in range(RET_H):
            for dc in range(2):
                bk, bb = P.bank()
                P.op("pe", mm_group(bk[:], [(kd[i][h][:, dc * 128:(dc + 1) * 128], v[i][:, h * 512:(h + 1) * 512])]),
                     reads=[b_kd[i][h], b_vh[i][h]], writes=[bb])
                P.op("dve", lambda en, h=h, dc=dc, bk=bk: en.scalar_tensor_tensor(R[:, h, dc, :], R[:, h, dc, :], float(cdec[h]), bk[:], ALU.mult, ALU.add),
                     reads=[bb, b_R[h]], writes=[b_R[h]])
            if full:
                P.op("act", lambda en, h=h: en.copy(Rb[:, h, :, :], R[:, h, :, :]), reads=[b_R[h]], writes=[b_Rb[h]])

    stage1(0)
    for n in range(NCH):
        if n + 1 < NCH:
            stage1(n + 1)
        stage2(n)
    if not full:
        for h in range(RET_H):
            P.dma("sp", r_d[h].rearrange("c p e -> p c e"), R[:, h, :, :], b_rd, reads=[b_R[h]])
        P.finish([b_rd])
    P.pop()
    if full:
        emit_proj_ln(P, T, 2048, x_d, u_d, wout_d, gn_d, lng_d, lnb_d, id_d, y_d, b_u, src_fm=False)
    if own:
        return P.emit()


def ret_consts(T, pos0):
    half = 128
    inv = (10000.0 ** (-np.arange(half, dtype=np.float32) / half)).astype(np.float32)
    pos = (pos0 + np.arange(T)).astype(np.float32)
    ang = pos[None, :] * inv[:, None]
    idx = np.arange(128, dtype=np.float64)
    dm = np.zeros((128, RET_H, 128), np.float32)
    qdec = np.zeros((128, RET_H, 128), np.float32)
    kdec = np.zeros((128, RET_H), np.float32)
    for h in range(RET_H):
        lg = np.log(GAMMA[h])
        rel = idx[None, :] - idx[:, None]
        dm[:, h, :] = np.where(rel >= 0, np.exp(lg * np.maximum(rel, 0.0)), 0.0) / 16.0
        qdec[:, h, :] = np.exp(lg * (idx + 1.0))[None, :]
        kdec[:, h] = np.exp(lg * (127.0 - idx)) / 16.0
    return {"cosT": np.cos(ang).astype(np.float32), "sinT": np.sin(ang).astype(np.float32),
            "dmT": dm, "qdec": qdec, "kdec": kdec}


def run_ret(mode, x_shards, pos0s, w_in, w_out=None, gn=None, ln_g=None, ln_b=None, rprevs=None):
    T = x_shards[0].shape[0]
    nc = build_ret(T, mode)
    ident = np.eye(128, dtype=np.float32)
    maps = []
    for c, xs in enumerate(x_shards):
        cst = ret_consts(T, pos0s[c])
        m = {"x": np.ascontiguousarray(xs), "w_in": w_in, "ident": ident, "cosT": cst["cosT"], "sinT": cst["sinT"], "kdec": cst["kdec"]}
        if mode == "B":
            m.update({"w_out": w_out, "gn": np.ascontiguousarray(gn.reshape(16, 128).T), "ln_g": _rep(ln_g), "ln_b": _rep(ln_b),
                      "dmT": cst["dmT"], "qdec": cst["qdec"], "rprev": rprevs[c]})
        maps.append(m)
    res = run_bass_kernel_spmd(nc, maps, core_ids=list(range(len(maps))))
    return [r["r_out" if mode == "A" else "y"] for r in res.results]


DIL = (1, 4, 16)
HALO = 2048
ATT_COLS = 4608


def build_att(T, debug=False, P=None, io=None):
    own = P is None
    if own:
        P = Prog()
    SK = "ExternalOutput" if debug else "Internal"
    H = HALO
    TE = H + T
    xe_d = P.dram_or(io, "x_ext", [TE, D_MODEL], F32, "ExternalInput")
    y_d = P.dram_or(io, "y", [T, D_MODEL], F32, "ExternalOutput")
    w_d = P.dram_or(io, "w_qkv", [D_MODEL, ATT_COLS], F32, "ExternalInput")
    wo_d = P.dram_or(io, "w_out", [512, D_MODEL], F32, "ExternalInput")
    id_d = P.dram_or(io, "ident", [128, 128], F32, "ExternalInput")
    lng_d = P.dram_or(io, "ln_g", [128, D_MODEL], F32, "ExternalInput")
    lnb_d = P.dram_or(io, "ln_b", [128, D_MODEL], F32, "ExternalInput")
    mask_d = P.dram_or(io, "mask2", [128, 256], F32, "ExternalInput")
    hv_d = P.dram_or(io, "halo_valid", [128, 1], F32, "ExternalInput")
    qT_d = P.dram_or(io, "qT_scr", [12, 128, T], BF16, SK)
    kT_d = P.dram_or(io, "kT_scr", [12, 128, TE], BF16, SK)
    v_d = P.dram_or(io, "v_scr", [TE, 1536], BF16, SK)
    oT_d = P.dram_or(io, "oT_scr", [512, T], BF16, SK)
    b_qd, b_kd, b_vd, b_od = P.buf("qT_d"), P.buf("kT_d"), P.buf("v_d"), P.buf("oT_d")

    P.push()
    ident_f = P.sb("ident_f", [128, 128], F32)
    ident = P.sb("ident", [128, 128], BF16)
    b_idf, b_id = P.buf("idf"), P.buf("id")
    P.dma("sp", ident_f[:], id_d, b_idf)
    P.op("dve", lambda e: e.tensor_copy(ident[:], ident_f[:]), reads=[b_idf], writes=[b_id])
    w_s = P.sb("wqkv", [128, KC, ATT_COLS], BF16)
    b_w = [P.buf("wqkv%d" % k) for k in range(KC)]
    for kc in range(KC):
        P.dma("pool", w_s[:, kc, :], w_d[kc * 128:(kc + 1) * 128, :], b_w[kc])
    xb = [P.sb("xb%d" % i, [128, 4, D_MODEL], BF16) for i in range(2)]
    b_xbt = [[P.buf("xb%d_%d" % (i, t)) for t in range(4)] for i in range(2)]
    xT = [P.sb("xT%d" % i, [128, KC, 512], BF16) for i in range(2)]
    b_xT = [P.buf("xT%d" % i) for i in range(2)]
    stg = [P.sb("stg%d" % i, [128, 512], BF16) for i in range(4)]
    b_stg = [P.buf("stg%d" % i) for i in range(4)]
    vst = [P.sb("vst%d" % i, [128, 1536], BF16) for i in range(2)]
    b_vst = [P.buf("vst%d" % i) for i in range(2)]
    nstg = 0
    nv = 0
    for blk in range(TE // 512):
        i = blk % 2
        t0 = blk * 512
        for tt in range(4):
            P.dma("pool", xb[i][:, tt, :], xe_d[t0 + tt * 128:t0 + (tt + 1) * 128, :], b_xbt[i][tt])
        for tt in range(4):
            for half in range(2):
                bk, bb = P.bank()
                for q in range(4):
                    kc = half * 4 + q
                    P.op("pe", mm_group(bk[:, q * 128:(q + 1) * 128], [(xb[i][:, tt, kc * 128:(kc + 1) * 128], ident[:])]),
                         reads=[b_xbt[i][tt], b_id], writes=[bb])
                P.op("dve", lambda en, half=half, bk=bk, tt=tt, i=i: en.tensor_copy(
                    xT[i][:, half * 4:(half + 1) * 4, tt * 128:(tt + 1) * 128], bk[:].rearrange("p (a b) -> p a b", a=4)),
                    reads=[bb], writes=[b_xT[i]])
        is_q = t0 >= H
        for cc in range(24):
            if cc < 12 and not is_q:
                continue
            bk, bb = P.bank()
            P.op("pe", mm_group(bk[:], [(w_s[:, kc, cc * 128:(cc + 1) * 128], xT[i][:, kc, :]) for kc in range(KC)]),
                 reads=b_w + [b_xT[i]], writes=[bb])
            s = nstg % 4
            nstg += 1
            eng = "act" if (nstg % 2) else "dve"
            if eng == "act":
                P.op("act", lambda en, s=s, bk=bk: en.copy(stg[s][:], bk[:]), reads=[bb], writes=[b_stg[s]])
            else:
                P.op("dve", lambda en, s=s, bk=bk: en.tensor_copy(stg[s][:], bk[:]), reads=[bb], writes=[b_stg[s]])
            if cc < 12:
                P.dma("sp", qT_d[cc, :, t0 - H:t0 - H + 512], stg[s][:], b_qd, reads=[b_stg[s]])
            else:
                P.dma("sp", kT_d[cc - 12, :, t0:t0 + 512], stg[s][:], b_kd, reads=[b_stg[s]])
        for tt in range(4):
            s = nv % 2
            nv += 1
            for g in range(3):
                bk, bb = P.bank()
                c0 = 3072 + g * 512
                P.op("pe", mm_group(bk[:], [(xT[i][:, kc, tt * 128:(tt + 1) * 128], w_s[:, kc, c0:c0 + 512]) for kc in range(KC)]),
                     reads=b_w + [b_xT[i]], writes=[bb])
                P.op("act", lambda en, s=s, g=g, bk=bk: en.copy(vst[s][:, g * 512:(g + 1) * 512], bk[:]), reads=[bb], writes=[b_vst[s]])
            P.dma("sp", v_d[t0 + tt * 128:t0 + (tt + 1) * 128, :], vst[s][:], b_vd, reads=[b_vst[s]])
    P.pop()

    P.push()
    mask_f = P.sb("mask_f", [128, 256], F32)
    mask2 = P.sb("mask2", [128, 256], BF16)
    maskH = P.sb("maskH", [128, 256], BF16)
    hv = P.sb("hv", [128, 1], F32)
    ones = P.sb("ones", [128, 128], BF16)
    b_mf, b_m2, b_mH, b_hv, b_ones = P.buf("mf"), P.buf("m2"), P.buf("mH"), P.buf("hv"), P.buf("ones")
    P.dma("sp", mask_f[:], mask_d, b_mf)
    P.dma("sp", hv[:], hv_d, b_hv)
    P.op("dve", lambda e: e.tensor_copy(mask2[:], mask_f[:]), reads=[b_mf], writes=[b_m2])
    P.op("dve", lambda e: e.tensor_copy(maskH[:, 128:256], mask_f[:, 128:256]), reads=[b_mf], writes=[b_mH])
    P.op("dve", lambda e: e.tensor_scalar(maskH[:, 0:128], mask_f[:, 0:128], hv[:, 0:1], None, ALU.mult), reads=[b_mf, b_hv, b_mH], writes=[b_mH])
    P.op("pool", lambda e: e.memset(ones[:], 1.0), writes=[b_ones])
    acc = P.sb("acc", [128, 2, T], F32)
    b_acc = P.buf("acc")
    qT = [P.sb("qT%d" % i, [128, T], BF16) for i in range(2)]
    b_qT = [P.buf("qT%d" % i) for i in range(2)]
    kT = [P.sb("kT%d" % i, [128, TE], BF16) for i in range(2)]
    b_kT = [P.buf("kT%d" % i) for i in range(2)]
    NBMAX = T // 128 + 1
    NVT = 4
    vt = [P.sb("vt%d" % i, [128, NBMAX, 128], BF16) for i in range(NVT)]
    b_vt = [P.buf("vt%d" % i) for i in range(NVT)]
    NET = 4
    ET = [P.sb("ET%d" % i, [128, 256], BF16) for i in range(NET)]
    b_ET = [P.buf("ET%d" % i) for i in range(NET)]
    oT = P.sb("oT", [128, T], BF16)
    b_oT = P.buf("oT")
    rz = P.sb("rz", [128, T], F32)
    b_rz = P.buf("rz")
    scale = float(128 ** -0.5)
    nqk = 0
    nvt = 0
    net = [0]
    for h in range(4):
        for g in range(3):
            d = DIL[g]
            Hg = 128 * d
            L = Hg + T
            i = nqk % 2
            nqk += 1
            P.dma("sp", qT[i][:], qT_d[g * 4 + h], b_qT[i], reads=[b_qd])
            P.dma("sp", kT[i][:, 0:L], kT_d[g * 4 + h, :, H - Hg:H + T], b_kT[i], reads=[b_kd])
            qv = qT[i][:].rearrange("p (m d) -> p d m", d=d)
            kv = kT[i][:, 0:L].rearrange("p (m d) -> p d m", d=d)
            av = acc[:].rearrange("p c (m d) -> p c d m", d=d)
            na = T // (128 * d)
            nb = na + 1
            tiles = []
            vsrc = {}
            for r in range(d):
                j = nvt % NVT
                nvt += 1
                vsrc[r] = (j, bass.AP(v_d.tensor, (H - Hg + r) * 1536 + g * 512 + h * 128,
                                      [[d * 1536, 128], [128 * d * 1536, nb], [1, 128]]))
                for a in range(na):
                    tiles.append((i, j, r, a, g, qv, kv, av))
            def sA(tl):
                i, j, r, a, g, qv, kv, av = tl
                bk, bb = P.bank()
                qa = qv[:, r, a * 128:(a + 1) * 128]
                P.op("pe", mm_group(bk[:, 0:128], [(kv[:, r, a * 128:(a + 1) * 128], qa)]), reads=[b_kT[i], b_qT[i]], writes=[bb])
                P.op("pe", mm_group(bk[:, 128:256], [(kv[:, r, (a + 1) * 128:(a + 2) * 128], qa)]), reads=[b_kT[i], b_qT[i]], writes=[bb])
                e = net[0] % NET
                net[0] += 1
                P.op("act", lambda en, e=e, bk=bk: en.activation(ET[e][:], bk[:, 0:256], AF.Exp, scale=scale), reads=[bb], writes=[b_ET[e]])
                mk, bmk = (maskH, b_mH) if a == 0 else (mask2, b_m2)
                P.op("pool", lambda en, e=e, mk=mk: en.tensor_tensor(ET[e][:], ET[e][:], mk[:], ALU.mult), reads=[b_ET[e], bmk], writes=[b_ET[e]])
                return e

            def sB(tl, e):
                i, j, r, a, g, qv, kv, av = tl
                bo, bbo = P.bank()
                P.op("pe", mm_group(bo[:, 0:128], [(vt[j][:, a, :], ET[e][:, 0:128]), (vt[j][:, a + 1, :], ET[e][:, 128:256])]),
                     reads=[b_vt[j], b_ET[e]], writes=[bbo])
                P.op("pe", mm_group(bo[:, 128:256], [(ones[:], ET[e][:, 0:128]), (ones[:], ET[e][:, 128:256])]),
                     reads=[b_ones, b_ET[e]], writes=[bbo])
                dst = av[:, :, r, a * 128:(a + 1) * 128]
                bo3 = bo[:, 0:256].rearrange("p (c m) -> p c m", c=2)
                if g == 0:
                    P.op("dve", lambda en, dst=dst, bo3=bo3: en.tensor_copy(dst, bo3), reads=[bbo], writes=[b_acc])
                else:
                    P.op("dve", lambda en, dst=dst, bo3=bo3: en.tensor_tensor(dst, dst, bo3, ALU.add), reads=[bbo, b_acc], writes=[b_acc])

            pend = []
            for tl in tiles:
                if tl[3] == 0:
                    jj, src = vsrc[tl[2]]
                    P.dma("sp", vt[jj][:, 0:nb, :], src, b_vt[jj], reads=[b_vd])
                pend.append((tl, sA(tl)))
                if len(pend) > 2:
                    sB(*pend.pop(0))
            for pp in pend:
                sB(*pp)
        P.op("dve", lambda en: en.reciprocal(rz[:], acc[:, 1, :]), reads=[b_acc], writes=[b_rz])
        P.op("dve", lambda en: en.tensor_tensor(oT[:], acc[:, 0, :], rz[:], ALU.mult), reads=[b_acc, b_rz], writes=[b_oT])
        P.dma("sp", oT_d[h * 128:(h + 1) * 128, :], oT[:], b_od, reads=[b_oT])
    P.pop()

    emit_proj_ln(P, T, 512, xe_d[H:TE, :], oT_d, wo_d, None, lng_d, lnb_d, id_d, y_d, b_od, src_fm=True)
    if own:
        return P.emit()


def att_mask():
    j = np.arange(128)[:, None]
    i = np.arange(128)[None, :]
    return np.concatenate([(j >= i), (j <= i)], axis=1).astype(np.float32)


def run_att(x_ext_shards, halo_valid, w_qkv, w_out, ln_g, ln_b, debug=False):
    T = x_ext_shards[0].shape[0] - HALO
    nc = build_att(T, debug)
    ident = np.eye(128, dtype=np.float32)
    maps = []
    for c, xe in enumerate(x_ext_shards):
        maps.append({"x_ext": np.ascontiguousarray(xe), "w_qkv": w_qkv, "w_out": w_out, "ident": ident,
                     "ln_g": _rep(ln_g), "ln_b": _rep(ln_b), "mask2": att_mask(),
                     "halo_valid": np.full((128, 1), float(halo_valid[c]), np.float32)})
    res = run_bass_kernel_spmd(nc, maps, core_ids=list(range(len(maps))))
    if debug:
        return res.results
    return [r["y"] for r in res.results]


def emit_halo(P, T, E1, hsrc, Hg, sel_d):
    H = HALO
    P.push()
    b_hs, b_hg, b_e1 = P.buf("hsrc"), P.buf("Hg"), P.buf("E1halo")
    P.dma("sp", hsrc, E1[H + T - H:H + T, :], b_hs)
    P.allgather(hsrc, Hg, b_hg, [b_hs], NCORES)
    sel = P.sb("hsel", [128, NCORES], F32)
    b_sel = P.buf("hsel")
    P.dma("sp", sel[:], sel_d, b_sel)
    tmp = [P.sb("htmp%d" % i, [128, D_MODEL], F32) for i in range(3)]
    b_tmp = [P.buf("htmp%d" % i) for i in range(3)]
    acc = [P.sb("hacc%d" % i, [128, D_MODEL], F32) for i in range(2)]
    b_acc = [P.buf("hacc%d" % i) for i in range(2)]
    n = 0
    for t in range(H // 128):
        a = t % 2
        for p in range(NCORES):
            i = n % 3
            n += 1
            r0 = p * H + t * 128
            P.dma("sp", tmp[i][:], Hg[r0:r0 + 128, :], b_tmp[i], reads=[b_hg])
            sc = sel[:, p:p + 1]
            if p == 0:
                P.op("dve", lambda en, i=i, a=a, sc=sc: en.tensor_scalar(acc[a][:], tmp[i][:], sc, None, ALU.mult),
                     reads=[b_tmp[i], b_sel], writes=[b_acc[a]])
            else:
                P.op("dve", lambda en, i=i, a=a, sc=sc: en.scalar_tensor_tensor(acc[a][:], tmp[i][:], sc, acc[a][:], ALU.mult, ALU.add),
                     reads=[b_tmp[i], b_sel, b_acc[a]], writes=[b_acc[a]])
        P.dma("sp", E1[t * 128:(t + 1) * 128, :], acc[a][:], b_e1, reads=[b_acc[a]])
    P.pop()


def build_fused(T):
    P = Prog()
    H = HALO
    inp = lambda n, shp: P.dram(n, shp, F32, "ExternalInput")
    x_d = inp("x", [T, D_MODEL])
    y_d = P.dram("y", [T, D_MODEL], F32, "ExternalOutput")
    lng = inp("ln_g", [2 * DEPTH, 128, D_MODEL])
    lnb = inp("ln_b", [2 * DEPTH, 128, D_MODEL])
    rwin = inp("ret_w_in", [2, D_MODEL, 6144])
    rwout = inp("ret_w_out", [2, 2048, D_MODEL])
    rgn = inp("ret_gn", [2, 128, 16])
    awq = inp("att_w_qkv", [2, D_MODEL, ATT_COLS])
    awo = inp("att_w_out", [2, 512, D_MODEL])
    fwg = inp("ffn_wg", [2, 1, D_MODEL, D_FF])
    fwu = inp("ffn_wu", [2, 1, D_MODEL, D_FF])
    fwd = inp("ffn_wd", [2, 1, D_FF, D_MODEL])
    mwr = inp("moe_wr", [2, D_MODEL, N_EXPERTS])
    mwg = inp("moe_wg", [2, N_EXPERTS, D_MODEL, D_FF])
    mwu = inp("moe_wu", [2, N_EXPERTS, D_MODEL, D_FF])
    mwd = inp("moe_wd", [2, N_EXPERTS, D_FF, D_MODEL])
    ident = inp("ident", [128, 128])
    cosT, sinT = inp("cosT", [128, T]), inp("sinT", [128, T])
    kdec, dmT, qdec = inp("kdec", [128, RET_H]), inp("dmT", [128, RET_H, 128]), inp("qdec", [128, RET_H, 128])
    mask2, hvalid = inp("mask2", [128, 256]), inp("halo_valid", [128, 1])
    hsel, rcf = inp("halo_sel", [128, NCORES]), inp("ret_cf", [128, NCORES * RET_H])
    itn = lambda n, shp, dt=F32: P.dram(n, shp, dt, "Internal")
    E1 = itn("E1", [H + T, D_MODEL])
    M = itn("Mmid", [T, D_MODEL])
    E2 = itn("E2", [T, D_MODEL])
    hsrc = itn("hsrc", [H, D_MODEL])
    Hg = itn("Hg", [NCORES * H, D_MODEL])
    Rl = itn("Rl", [RET_H * 2 * 128, 512])
    Rg = itn("Rg", [NCORES * RET_H * 2 * 128, 512])
    scr = {"u_scr": itn("u_scr", [T, 2048], BF16), "qT_scr": itn("qT_scr", [12, 128, T], BF16),
           "kT_scr": itn("kT_scr", [12, 128, H + T], BF16), "v_scr": itn("v_scr", [H + T, 1536], BF16),
           "oT_scr": itn("oT_scr", [512, T], BF16)}
    Rl5 = Rl.rearrange("(h c p) e -> h c p e", h=RET_H, c=2)
    Rg5 = Rg.rearrange("(n h c p) e -> n h c p e", n=NCORES, h=RET_H, c=2)
    cur = x_d
    for i in range(DEPTH):
        j = i // 2
        last = (i == DEPTH - 1)
        if i % 2 == 0:
            base = {"x": cur, "w_in": rwin[j], "ident": ident, "cosT": cosT, "sinT": sinT, "kdec": kdec}
            ioA = dict(base)
            ioA["r_out"] = Rl5
            build_ret(T, "A", P=P, io=ioA)
            P.push()
            b_rg = P.buf("Rg")
            P.allgather(Rl, Rg, b_rg, [], NCORES)
            P.pop()
            ioB = dict(base)
            ioB.update({"y": M, "w_out": rwout[j], "gn": rgn[j], "ln_g": lng[2 * i], "ln_b": lnb[2 * i], "dmT": dmT, "qdec": qdec,
                        "rg": Rg5, "ret_cf": rcf, "u_scr": scr["u_scr"]})
            build_ret(T, "B", P=P, io=ioB, gathered=True)
            out = E1[H:H + T, :]
            build_ffn(T, 1, TG=min(T, 2048), P=P, io={"x": M, "y": out, "ln_g": lng[2 * i + 1], "ln_b": lnb[2 * i + 1], "ident": ident,
                                     "wg": fwg[j], "wu": fwu[j], "wd": fwd[j]})
            cur = out
        else:
            emit_halo(P, T, E1, hsrc, Hg, hsel)
            ioT = {"x_ext": E1, "y": M, "w_qkv": awq[j], "w_out": awo[j], "ident": ident, "ln_g": lng[2 * i], "ln_b": lnb[2 * i],
                   "mask2": mask2, "halo_valid": hvalid}
            ioT.update({k: scr[k] for k in ("qT_scr", "kT_scr", "v_scr", "oT_scr")})
            build_att(T, P=P, io=ioT)
            out = y_d if last else E2
            build_ffn(T, N_EXPERTS, TG=min(T, 2048), P=P, io={"x": M, "y": out, "ln_g": lng[2 * i + 1], "ln_b": lnb[2 * i + 1], "ident": ident,
                                             "wg": mwg[j], "wu": mwu[j], "wd": mwd[j], "wr": mwr[j]})
            cur = out
    return P.emit()


_PROGS = {}


def kernel(x, ln_gain, ln_bias, ret_w_in, ret_gn_gain, ret_w_out, att_w_qkv, att_w_out,
           ffn_w_gate, ffn_w_up, ffn_w_down, moe_w_router, moe_w_gate, moe_w_up, moe_w_down):
    f = lambda a: np.ascontiguousarray(np.asarray(a, dtype=np.float32))
    x = f(x)
    B, S, D = x.shape
    T = (B * S) // NCORES
    CPB = NCORES // B
    if T not in _PROGS:
        _PROGS[T] = build_fused(T)
    nc = _PROGS[T]
    lg, lb = f(ln_gain).reshape(2 * DEPTH, D), f(ln_bias).reshape(2 * DEPTH, D)
    shared = {
        "ln_g": np.ascontiguousarray(np.broadcast_to(lg[:, None, :], (2 * DEPTH, 128, D))),
        "ln_b": np.ascontiguousarray(np.broadcast_to(lb[:, None, :], (2 * DEPTH, 128, D))),
        "ret_w_in": f(ret_w_in), "ret_w_out": f(ret_w_out),
        "ret_gn": np.ascontiguousarray(f(ret_gn_gain).reshape(2, 16, 128).transpose(0, 2, 1)),
        "att_w_qkv": f(att_w_qkv), "att_w_out": f(att_w_out),
        "ffn_wg": f(ffn_w_gate)[:, None], "ffn_wu": f(ffn_w_up)[:, None], "ffn_wd": f(ffn_w_down)[:, None],
        "moe_wr": f(moe_w_router), "moe_wg": f(moe_w_gate), "moe_wu": f(moe_w_up), "moe_wd": f(moe_w_down),
        "ident": np.eye(128, dtype=np.float32), "mask2": att_mask(),
    }
    xf = x.reshape(B * S, D)
    maps = []
    for c in range(NCORES):
        cst = ret_consts(T, (c % CPB) * T)
        first = (c % CPB == 0)
        hs = np.zeros((128, NCORES), np.float32)
        if not first:
            hs[:, c - 1] = 1.0
        cf = np.zeros((128, NCORES, RET_H), np.float32)
        for p in range(NCORES):
            if p < c and p // CPB == c // CPB:
                for h in range(RET_H):
                    cf[:, p, h] = GAMMA[h] ** (T * (c - 1 - p))
        m = dict(shared)
        m.update({"x": xf[c * T:(c + 1) * T], "cosT": cst["cosT"], "sinT": cst["sinT"], "kdec": cst["kdec"], "dmT": cst["dmT"],
                  "qdec": cst["qdec"], "halo_valid": np.full((128, 1), 0.0 if first else 1.0, np.float32),
                  "halo_sel": hs, "ret_cf": cf.reshape(128, NCORES * RET_H)})
        maps.append(m)
    res = run_bass_kernel_spmd(nc, maps, core_ids=list(range(NCORES))).results
    return np.concatenate([r["y"] for r in res], axis=0).reshape(B, S, D).astype(np.float32)
```

```python
import numpy as np
from contextlib import ExitStack
import concourse.bass as bass
import concourse.mybir as mybir
from concourse.bass_utils import run_bass_kernel_spmd

F32 = mybir.dt.float32
BF16 = mybir.dt.bfloat16
AF = mybir.ActivationFunctionType
ALU = mybir.AluOpType
AX = mybir.AxisListType

D_MODEL = 1024
DEPTH = 4
D_FF = 3584
N_EXPERTS = 8
ALPHA = (2 * DEPTH) ** 0.25
LN_EPS = 1e-5
NCORES = 8
KC = D_MODEL // 128

ENGS = ("pe", "act", "dve", "pool", "sp")


class Buf:
    __slots__ = ("name", "w", "r", "dkey", "dcnt", "skey", "scnt")

    def __init__(self, name):
        self.name = name
        self.w = None
        self.r = {}
        self.dkey = None
        self.dcnt = 0
        self.skey = None
        self.scnt = 0


class Prog:
    def __init__(self):
        self.nc = bass.Bass("TRN2", target_bir_lowering=False)
        self.es = ExitStack()
        self.ops = {e: [] for e in ENGS}
        self.cnt = {e: 0 for e in ENGS}
        self.seen = {e: {} for e in ENGS}
        self.sems = {}
        self.dtot = {}
        self.nbuf = 0
        for e in ("pe", "act", "dve", "pool"):
            self.sems[e] = self.es.enter_context(self.nc.semaphore("s_" + e))
        self.stack = [self.es]
        self.banks = None
        self.bank_i = 0
        self.free_dsems = []
        self.phase_sems = [[]]

    def push(self):
        st = ExitStack()
        self.stack.append(st)
        self.phase_sems.append([])

    def pop(self):
        self.barrier()
        self.flush()
        self.stack.pop().close()
        self.banks = None
        self.free_dsems.extend(self.phase_sems.pop())

    def _dsem(self, dst, prefix):
        if self.free_dsems:
            dst.dkey = self.free_dsems.pop()
            dst.dcnt = self.dtot[dst.dkey]
        else:
            dst.dkey = prefix + dst.name + "_%d" % len(self.sems)
            self.sems[dst.dkey] = self.es.enter_context(self.nc.semaphore(dst.dkey))
        self.phase_sems[-1].append(dst.dkey)

    def barrier(self):
        allev = [(k, self.cnt[k]) for k in ("pe", "act", "dve", "pool") if self.cnt[k] > 0]
        allev += [(k, self.dtot[k]) for k in self.dtot]
        for e in ENGS:
            waits = []
            for k, v in allev:
                if self.seen[e].get(k, 0) >= v:
                    continue
                self.seen[e][k] = v
                waits.append((k, v))
            if waits:
                self.ops[e].append((waits, None, None))

    def bank(self):
        if self.banks is None:
            self.banks = [(self.ps("bank%d_%d" % (i, self.nbuf), [128, 512]), self.buf("bank%d" % i)) for i in range(8)]
        b = self.banks[self.bank_i % 8]
        self.bank_i += 1
        return b

    def sb(self, name, shape, dtype):
        self.nbuf += 1
        return self.stack[-1].enter_context(self.nc.sbuf_tensor("sb%d_%s" % (self.nbuf, name), list(shape), dtype))

    def ps(self, name, shape, dtype=F32):
        self.nbuf += 1
        return self.stack[-1].enter_context(self.nc.psum_tensor("ps%d_%s" % (self.nbuf, name), list(shape), dtype))

    def dram(self, name, shape, dtype, kind):
        return self.nc.dram_tensor(name, list(shape), dtype, kind=kind).ap()

    def buf(self, name=None):
        self.nbuf += 1
        return Buf(name or ("b%d" % self.nbuf))

    def dram_or(self, io, name, shape, dtype, kind):
        if io is not None and name in io:
            return io[name]
        return self.dram(name, shape, dtype, kind)

    def allgather(self, in_ap, out_ap, dst, reads, ncores):
        if dst.dkey is None:
            self._dsem(dst, "c_")
        waits = self._waits("pool", reads, (dst,))
        dst.dcnt += 1
        self.dtot[dst.dkey] = dst.dcnt
        ev = (dst.dkey, dst.dcnt)
        fn = lambda e, i=in_ap, o=out_ap: e.collective_compute(
            "AllGather", ALU.bypass, replica_groups=[list(range(ncores))], ins=[i.opt()], outs=[o.opt()])
        self.ops["pool"].append((waits, fn, (dst.dkey, None)))
        self._commit(ev, reads, (dst,))

    def _waits(self, eng, reads, writes):
        deps = {}

        def add(ev):
            if ev is None:
                return
            k, v = ev
            if deps.get(k, 0) < v:
                deps[k] = v

        for b in reads:
            add(b.w)
        for b in writes:
            add(b.w)
            for k, v in b.r.items():
                add((k, v))
        waits = []
        for k, v in deps.items():
            if k == eng and eng == "pe":
                continue
            if self.seen[eng].get(k, 0) >= v:
                continue
            self.seen[eng][k] = v
            waits.append((k, v))
        return waits

    def _commit(self, ev, reads, writes):
        k, v = ev
        for b in reads:
            if b.r.get(k, 0) < v:
                b.r[k] = v
        for b in writes:
            b.w = ev
            b.r = {}

    def op(self, eng, fn, reads=(), writes=()):
        waits = self._waits(eng, reads, writes)
        self.cnt[eng] += 1
        ev = (eng, self.cnt[eng])
        self.ops[eng].append((waits, fn, (eng, 1)))
        self._commit(ev, reads, writes)

    def dma(self, q, out_ap, in_ap, dst, reads=(), extra_writes=()):
        if dst.dkey is None:
            self._dsem(dst, "d_")
        writes = (dst,) + tuple(extra_writes)
        waits = self._waits(q, reads, writes)
        dst.dcnt += 16
        self.dtot[dst.dkey] = dst.dcnt
        ev = (dst.dkey, dst.dcnt)
        self.ops[q].append((waits, lambda e, o=out_ap, i=in_ap: e.dma_start(out=o, in_=i), (dst.dkey, 16)))
        self._commit(ev, reads, writes)

    def store(self, q, out_ap, in_ap, src, also=()):
        if src.skey is None:
            if self.free_dsems:
                src.skey = self.free_dsems.pop()
                src.scnt = self.dtot[src.skey]
            else:
                src.skey = "s_" + src.name + "_%d" % len(self.sems)
                self.sems[src.skey] = self.es.enter_context(self.nc.semaphore(src.skey))
            self.phase_sems[-1].append(src.skey)
        waits = self._waits(q, [src] + list(also), ())
        src.scnt += 16
        self.dtot[src.skey] = src.scnt
        self.ops[q].append((waits, lambda e, o=out_ap, i=in_ap: e.dma_start(out=o, in_=i), (src.skey, 16)))
        for b in [src] + list(also):
            b.r[src.skey] = src.scnt

    def finish(self, bufs, eng="sp"):
        waits = self._waits(eng, bufs, ())
        self.ops[eng].append((waits, None, None))

    def emit(self):
        self.flush()
        while self.stack:
            self.stack.pop().close()
        return self.nc

    def flush(self):
        nc = self.nc
        if not any(self.ops[e] for e in ENGS):
            return
        ops = self.ops
        self.ops = {e: [] for e in ENGS}
        with nc.Block() as block:
            def run(engname):
                def body(e):
                    for waits, fn, inc in ops[engname]:
                        for k, v in waits:
                            e.wait_ge(self.sems[k], v)
                        if fn is not None:
                            ins = fn(e)
                            if inc[1] is None:
                                ins.then_inc(self.sems[inc[0]])
                            else:
                                ins.then_inc(self.sems[inc[0]], inc[1])
                return body
            block.tensor(run("pe"))
            block.scalar(run("act"))
            block.vector(run("dve"))
            block.gpsimd(run("pool"))
            block.sync(run("sp"))


def mm_group(out_ap, pairs):
    def fn(e):
        n = len(pairs)
        ins = None
        for i, (l, r) in enumerate(pairs):
            ins = e.matmul(out_ap, l, r, start=(i == 0), stop=(i == n - 1))
        return ins
    return fn


class Common:
    def __init__(self, P, ln_g_ap, ln_b_ap, ident_ap, nslots=2):
        self.P = P
        nc = P.nc
        self.ident_f = P.sb("ident_f", [128, 128], F32)
        self.ident = P.sb("ident_bf", [128, 128], BF16)
        self.lng = P.sb("lng", [128, D_MODEL], F32)
        self.lnb = P.sb("lnb", [128, D_MODEL], F32)
        self.neghalf = P.sb("neghalf", [128, 1], F32)
        self.b_ident = P.buf("ident")
        self.b_identf = P.buf("identf")
        self.b_lng = P.buf("lng")
        self.b_lnb = P.buf("lnb")
        self.b_nh = P.buf("nh")
        P.dma("sp", self.ident_f[:], ident_ap, self.b_identf)
        P.dma("sp", self.lng[:], ln_g_ap, self.b_lng)
        P.dma("sp", self.lnb[:], ln_b_ap, self.b_lnb)
        P.op("dve", lambda e: e.tensor_copy(self.ident[:], self.ident_f[:]), reads=[self.b_identf], writes=[self.b_ident])
        P.op("pool", lambda e: e.memset(self.neghalf[:], -0.5), writes=[self.b_nh])
        NS = self.NS = nslots
        self.s = [P.sb("ln_s%d" % i, [128, D_MODEL], F32) for i in range(NS)]
        self.b_s = [P.buf("ln_s%d" % i) for i in range(NS)]
        self.st = [P.sb("ln_st%d" % i, [128, 2, 6], F32) for i in range(NS)]
        self.mv = [P.sb("ln_mv%d" % i, [128, 2], F32) for i in range(NS)]
        self.rs = [P.sb("ln_rs%d" % i, [128, 2], F32) for i in range(NS)]
        self.b_small = [P.buf("ln_small%d" % i) for i in range(NS)]
        self.xo = [P.sb("ln_xo%d" % i, [128, D_MODEL], F32) for i in range(NS)]
        self.b_xo = [P.buf("ln_xo%d" % i) for i in range(NS)]
        self.k = 0

    def layernorm(self, fill_s, fill_reads, out_dram_ap, out_buf, post=None):
        P = self.P
        i = self.k % self.NS
        self.k += 1
        s, st, mv, rs, xo = self.s[i], self.st[i], self.mv[i], self.rs[i], self.xo[i]
        bs, bsm, bxo = self.b_s[i], self.b_small[i], self.b_xo[i]
        for eng, fn in fill_s(s):
            P.op(eng, fn, reads=fill_reads, writes=[bs])
        P.op("dve", lambda e: e.bn_stats(st[:, 0, :], s[:, 0:512]), reads=[bs], writes=[bsm])
        P.op("dve", lambda e: e.bn_stats(st[:, 1, :], s[:, 512:1024]), reads=[bs], writes=[bsm])
        P.op("dve", lambda e: e.bn_aggr(mv[:], st[:].rearrange("p a b -> p (a b)")), reads=[bsm], writes=[bsm])
        P.op("pool", lambda e: e.tensor_scalar(rs[:, 0:1], mv[:, 1:2], LN_EPS, None, ALU.add), reads=[bsm], writes=[bsm])
        P.op("pool", lambda e: e.tensor_tensor(rs[:, 1:2], rs[:, 0:1], self.neghalf[:], ALU.pow), reads=[bsm, self.b_nh], writes=[bsm])
        P.op("dve", lambda e: e.tensor_scalar(s[:], s[:], mv[:, 0:1], rs[:, 1:2], ALU.subtract, ALU.mult), reads=[bs, bsm], writes=[bs])
        P.op("pool", lambda e: e.tensor_tensor(xo[:], s[:], self.lng[:], ALU.mult), reads=[bs, self.b_lng], writes=[bxo])
        P.op("pool", lambda e: e.tensor_tensor(xo[:], xo[:], self.lnb[:], ALU.add), reads=[bxo, self.b_lnb], writes=[bxo])
        if out_dram_ap is not None:
            P.store("sp", out_dram_ap, xo[:], bxo)
        if post is not None:
            post(xo, bxo)


def build_ffn(T, E, TG=1024, P=None, io=None):
    own = P is None
    if own:
        P = Prog()
    P.push()
    nc = P.nc
    NT = T // 128
    NG = T // TG
    TPG = TG // 128
    NTB = TG // 512
    NJB = D_FF // 512

    x_d = P.dram_or(io, "x", [T, D_MODEL], F32, "ExternalInput")
    y_d = P.dram_or(io, "y", [T, D_MODEL], F32, "ExternalOutput")
    lng_d = P.dram_or(io, "ln_g", [128, D_MODEL], F32, "ExternalInput")
    lnb_d = P.dram_or(io, "ln_b", [128, D_MODEL], F32, "ExternalInput")
    id_d = P.dram_or(io, "ident", [128, 128], F32, "ExternalInput")
    wg_d = P.dram_or(io, "wg", [E, D_MODEL, D_FF], F32, "ExternalInput")
    wu_d = P.dram_or(io, "wu", [E, D_MODEL, D_FF], F32, "ExternalInput")
    wd_d = P.dram_or(io, "wd", [E, D_FF, D_MODEL], F32, "ExternalInput")
    if E > 1:
        wr_d = P.dram_or(io, "wr", [D_MODEL, E], F32, "ExternalInput")

    C = Common(P, lng_d, lnb_d, id_d, nslots=2)
    b_y = P.buf("y_dram")

    xT = P.sb("xT", [128, KC, TG], BF16)
    b_xT = [P.buf("xT%d" % t) for t in range(TPG)]
    acc = P.sb("acc", [128, TPG, D_MODEL], F32)
    b_acc = [P.buf("acc%d" % t) for t in range(TPG)]
    NXS = 3
    xs = [P.sb("xs%d" % i, [128, D_MODEL], F32) for i in range(NXS)]
    b_xs = [P.buf("xs%d" % i) for i in range(NXS)]
    xb = [P.sb("xb%d" % i, [128, D_MODEL], BF16) for i in range(NXS)]
    b_xb = [P.buf("xb%d" % i) for i in range(NXS)]
    wg_s = [P.sb("wg%d" % i, [128, KC, 512], BF16) for i in range(2)]
    wu_s = [P.sb("wu%d" % i, [128, KC, 512], BF16) for i in range(2)]
    wd_s = [P.sb("wd%d" % i, [128, 4, D_MODEL], BF16) for i in range(2)]
    b_wg = [P.buf("wg%d" % i) for i in range(2)]
    b_wu = [P.buf("wu%d" % i) for i in range(2)]
    b_wd = [P.buf("wd%d" % i) for i in range(2)]
    hT = [P.sb("hT%d" % i, [128, 4, 512], BF16) for i in range(2)]
    b_hT = [[P.buf("hT%d_%d" % (i, c)) for c in range(4)] for i in range(2)]
    sg = [P.sb("sg%d" % i, [128, 512], F32) for i in range(2)]
    b_sg = [P.buf("sg%d" % i) for i in range(2)]
    if E > 1:
        wr_f = P.sb("wr_f", [128, KC, E], F32)
        wr_s = P.sb("wr_s", [128, KC, E], BF16)
        b_wrf = P.buf("wrf")
        b_wr = P.buf("wr")
        lg = P.sb("lg", [128, TPG, E], F32)
        top = P.sb("top", [128, TPG, 8], F32)
        gsm = P.sb("gsm", [128, TPG, 4], F32)
        gA = P.sb("gA", [128, TPG, E], F32)
        gates = P.sb("gates", [128, TPG, E], F32)
        b_g = [P.buf("gate%d" % t) for t in range(TPG)]
        P.dma("sp", wr_f[:], wr_d.rearrange("(k p) e -> p k e", p=128), b_wrf)
        P.op("dve", lambda e: e.tensor_copy(wr_s[:], wr_f[:]), reads=[b_wrf], writes=[b_wr])

    pg = [P.ps("pg%d" % i, [128, 512]) for i in range(2)]
    pu = [P.ps("pu%d" % i, [128, 512]) for i in range(2)]
    pd = [P.ps("pd%d" % i, [128, 512]) for i in range(2)]
    pt = [P.ps("pt%d" % i, [128, 512]) for i in range(2)]
    b_pg = [P.buf("pg%d" % i) for i in range(2)]
    b_pu = [P.buf("pu%d" % i) for i in range(2)]
    b_pd = [P.buf("pd%d" % i) for i in range(2)]
    b_pt = [P.buf("pt%d" % i) for i in range(2)]

    blocks = [(g, e, jb) for g in range(NG) for e in range(E) for jb in range(NJB)]

    def load_w(idx):
        g, e, jb = blocks[idx]
        s = idx % 2
        c0 = jb * 512
        P.dma("pool", wg_s[s][:], wg_d[e, :, c0:c0 + 512].rearrange("(k p) c -> p k c", p=128), b_wg[s])
        P.dma("pool", wu_s[s][:], wu_d[e, :, c0:c0 + 512].rearrange("(k p) c -> p k c", p=128), b_wu[s])
        P.dma("pool", wd_s[s][:], wd_d[e, c0:c0 + 512, :].rearrange("(k p) c -> p k c", p=128), b_wd[s])

    load_w(0)
    if len(blocks) > 1:
        load_w(1)

    cchunk = [0]
    cdown = [0]
    cunit = [0]

    def emit_gu(idx, tb):
        g, e, jb = blocks[idx]
        s = idx % 2
        hs = cunit[0] % 2
        for c in range(4):
            k = cchunk[0] % 2
            cchunk[0] += 1
            rhs = lambda kc: xT[:, kc, tb * 512:(tb + 1) * 512]
            rd = [b_xT[tb * 4 + i] for i in range(4)]
            P.op("pe", mm_group(pg[k][:], [(wg_s[s][:, kc, c * 128:(c + 1) * 128], rhs(kc)) for kc in range(KC)]),
                 reads=[b_wg[s]] + rd, writes=[b_pg[k]])
            P.op("pe", mm_group(pu[k][:], [(wu_s[s][:, kc, c * 128:(c + 1) * 128], rhs(kc)) for kc in range(KC)]),
                 reads=[b_wu[s]] + rd, writes=[b_pu[k]])
            P.op("act", lambda en, k=k: en.activation(sg[k][:], pg[k][:], AF.Silu), reads=[b_pg[k]], writes=[b_sg[k]])
            P.op("dve", lambda en, k=k, c=c, hs=hs: en.tensor_tensor(hT[hs][:, c, :], sg[k][:], pu[k][:], ALU.mult),
                 reads=[b_sg[k], b_pu[k]], writes=[b_hT[hs][c]])
        u = (idx, tb, hs)
        cunit[0] += 1
        return u

    def emit_down(u, first):
        idx, tb, hs = u
        g, e, jb = blocks[idx]
        s = idx % 2
        for t in range(4):
            tt = tb * 4 + t
            for nb in range(2):
                k = cdown[0] % 2
                cdown[0] += 1
                P.op("pe", mm_group(pd[k][:], [(hT[hs][:, c, t * 128:(t + 1) * 128], wd_s[s][:, c, nb * 512:(nb + 1) * 512]) for c in range(4)]),
                     reads=[b_wd[s]] + b_hT[hs], writes=[b_pd[k]])
                a = acc[:, tt, nb * 512:(nb + 1) * 512]
                if E > 1:
                    gsc = gates[:, tt, e:e + 1]
                    if first:
                        P.op("dve", lambda en, a=a, k=k, gsc=gsc: en.tensor_scalar(a, pd[k][:], gsc, None, ALU.mult),
                             reads=[b_pd[k], b_g[tt]], writes=[b_acc[tt]])
                    else:
                        P.op("dve", lambda en, a=a, k=k, gsc=gsc: en.scalar_tensor_tensor(a, pd[k][:], gsc, a, ALU.mult, ALU.add),
                             reads=[b_pd[k], b_g[tt], b_acc[tt]], writes=[b_acc[tt]])
                else:
                    if first:
                        P.op("dve", lambda en, a=a, k=k: en.tensor_copy(a, pd[k][:]), reads=[b_pd[k]], writes=[b_acc[tt]])
                    else:
                        P.op("dve", lambda en, a=a, k=k: en.tensor_tensor(a, a, pd[k][:], ALU.add),
                             reads=[b_pd[k], b_acc[tt]], writes=[b_acc[tt]])

    xcount = [0]

    def load_x_tile(g, t):
        i = xcount[0] % NXS
        xcount[0] += 1
        r0 = g * TG + t * 128
        P.dma("sp", xs[i][:], x_d[r0:r0 + 128, :], b_xs[i])
        return i

    bidx = 0
    for g in range(NG):
        for t in range(TPG):
            i = load_x_tile(g, t)
            P.op("act", lambda en, i=i: en.copy(xb[i][:], xs[i][:]), reads=[b_xs[i]], writes=[b_xb[i]])
            for half in range(2):
                for q in range(4):
                    kc = half * 4 + q
                    P.op("pe", mm_group(pt[half][:, q * 128:(q + 1) * 128], [(xb[i][:, kc * 128:(kc + 1) * 128], C.ident[:])]),
                         reads=[b_xb[i], C.b_ident], writes=[b_pt[half]])
                P.op("dve", lambda en, half=half, t=t: en.tensor_copy(
                    xT[:, half * 4:(half + 1) * 4, t * 128:(t + 1) * 128], pt[half][:].rearrange("p (a b) -> p a b", a=4)),
                    reads=[b_pt[half]], writes=[b_xT[t]])
            if E > 1:
                P.op("pe", mm_group(pt[0][:, 0:E], [(xT[:, kc, t * 128:(t + 1) * 128], wr_s[:, kc, :]) for kc in range(KC)]),
                     reads=[b_xT[t], b_wr], writes=[b_pt[0]])
                L = lg[:, t, :]
                P.op("dve", lambda en, L=L: en.tensor_copy(L, pt[0][:, 0:E]), reads=[b_pt[0]], writes=[b_g[t]])
                P.op("dve", lambda en, L=L, t=t: en.max(top[:, t, :], L), reads=[b_g[t]], writes=[b_g[t]])
                P.op("dve", lambda en, t=t: en.tensor_tensor(gsm[:, t, 0:1], top[:, t, 1:2], top[:, t, 0:1], ALU.subtract), reads=[b_g[t]], writes=[b_g[t]])
                P.op("act", lambda en, t=t: en.activation(gsm[:, t, 1:2], gsm[:, t, 0:1], AF.Exp), reads=[b_g[t]], writes=[b_g[t]])
                P.op("dve", lambda en, t=t: en.tensor_scalar(gsm[:, t, 2:3], gsm[:, t, 1:2], 1.0, None, ALU.add), reads=[b_g[t]], writes=[b_g[t]])
                P.op("dve", lambda en, t=t: en.reciprocal(gsm[:, t, 2:3], gsm[:, t, 2:3]), reads=[b_g[t]], writes=[b_g[t]])
                P.op("dve", lambda en, t=t: en.tensor_tensor(gsm[:, t, 3:4], gsm[:, t, 1:2], gsm[:, t, 2:3], ALU.mult), reads=[b_g[t]], writes=[b_g[t]])
                P.op("dve", lambda en, t=t: en.tensor_tensor(gsm[:, t, 0:1], gsm[:, t, 2:3], gsm[:, t, 3:4], ALU.subtract), reads=[b_g[t]], writes=[b_g[t]])
                P.op("dve", lambda en, L=L, t=t: en.tensor_scalar(gA[:, t, :], L, top[:, t, 1:2], gsm[:, t, 3:4], ALU.is_ge, ALU.mult), reads=[b_g[t]], writes=[b_g[t]])
                P.op("dve", lambda en, L=L, t=t: en.tensor_scalar(gates[:, t, :], L, top[:, t, 0:1], gsm[:, t, 0:1], ALU.is_ge, ALU.mult), reads=[b_g[t]], writes=[b_g[t]])
                P.op("dve", lambda en, t=t: en.tensor_tensor(gates[:, t, :], gates[:, t, :], gA[:, t, :], ALU.add), reads=[b_g[t]], writes=[b_g[t]])
        pending = None
        for e in range(E):
            for jb in range(NJB):
                first = (e == 0 and jb == 0)
                for tb in range(NTB):
                    u = emit_gu(bidx, tb)
                    if pending is not None:
                        emit_down(*pending)
                    pending = (u, first)
                bidx += 1
                if bidx + 1 < len(blocks):
                    if pending is not None:
                        emit_down(*pending)
                        pending = None
                    load_w(bidx + 1)
        if pending is not None:
            emit_down(*pending)
            pending = None
        LA = 1
        slots = {}
        for t in range(min(LA, TPG)):
            slots[t] = load_x_tile(g, t)
        for t in range(TPG):
            if t + LA < TPG:
                slots[t + LA] = load_x_tile(g, t + LA)
            i = slots[t]
            r0 = g * TG + t * 128

            def fill(s, i=i, t=t):
                return [("dve", lambda en: en.scalar_tensor_tensor(s[:], xs[i][:], float(ALPHA), acc[:, t, :], ALU.mult, ALU.add))]
            C.layernorm(fill, [b_xs[i], b_acc[t]], y_d[r0:r0 + 128, :], b_y)
    P.finish([b_y])
    P.pop()
    if own:
        return P.emit()


def _rep(v):
    return np.ascontiguousarray(np.broadcast_to(np.asarray(v, np.float32)[None, :], (128, v.shape[-1])))


def run_ffn(x_shards, ln_g, ln_b, wg, wu, wd, wr=None, TG=1024):
    T = x_shards[0].shape[0]
    E = wg.shape[0]
    nc = build_ffn(T, E, TG=min(TG, T))
    ident = np.eye(128, dtype=np.float32)
    maps = []
    for xs in x_shards:
        m = {"x": np.ascontiguousarray(xs), "ln_g": _rep(ln_g), "ln_b": _rep(ln_b), "ident": ident,
             "wg": wg, "wu": wu, "wd": wd}
        if E > 1:
            m["wr"] = wr
        maps.append(m)
    res = run_bass_kernel_spmd(nc, maps, core_ids=list(range(len(maps))))
    return [r["y"] for r in res.results]


def emit_proj_ln(P, T, KD, x_d, u_d, w_d, rowgain_d, lng_d, lnb_d, id_d, y_d, b_u, src_fm):
    NE = KD // 128
    NTL = T // 128
    P.push()
    C = Common(P, lng_d, lnb_d, id_d, nslots=4)
    b_y = P.buf("y_dram")
    w_s = P.sb("pw", [128, NE, D_MODEL], BF16)
    b_w = [P.buf("pw%d" % i) for i in range(NE)]
    if rowgain_d is not None:
        rg = P.sb("rg", [128, NE], F32)
        b_rg = P.buf("rg")
        P.dma("sp", rg[:], rowgain_d, b_rg)
        wst = [P.sb("wst%d" % i, [128, D_MODEL], F32) for i in range(2)]
        b_wst = [P.buf("wst%d" % i) for i in range(2)]
        for ec in range(NE):
            i = ec % 2
            P.dma("sp", wst[i][:], w_d[ec * 128:(ec + 1) * 128, :], b_wst[i])
            P.op("dve", lambda en, i=i, ec=ec: en.tensor_scalar(w_s[:, ec, :], wst[i][:], rg[:, ec:ec + 1], None, ALU.mult),
                 reads=[b_wst[i], b_rg], writes=[b_w[ec]])
    else:
        for ec in range(NE):
            P.dma("pool", w_s[:, ec, :], w_d[ec * 128:(ec + 1) * 128, :], b_w[ec])
    ut = [P.sb("ut%d" % i, [128, KD], BF16) for i in range(4)]
    b_ut = [P.buf("ut%d" % i) for i in range(4)]
    uT = [P.sb("uT%d" % i, [128, NE, 128], BF16) for i in range(4)]
    b_uT = [P.buf("uT%d" % i) for i in range(4)]
    xs = [P.sb("pxs%d" % i, [128, D_MODEL], F32) for i in range(4)]
    b_xs = [P.buf("pxs%d" % i) for i in range(4)]
    def loads(t):
        i = t % 4
        r0 = t * 128
        P.dma("sp", xs[i][:], x_d[r0:r0 + 128, :], b_xs[i])
        if src_fm:
            for ec in range(NE):
                P.dma("sp", uT[i][:, ec, :], u_d[ec * 128:(ec + 1) * 128, r0:r0 + 128], b_uT[i], reads=[b_u])
        else:
            P.dma("sp", ut[i][:], u_d[r0:r0 + 128, :], b_ut[i], reads=[b_u])

    LA = 2
    for t in range(min(LA, NTL)):
        loads(t)
    for t in range(NTL):
        if t + LA < NTL:
            loads(t + LA)
        i = t % 4
        r0 = t * 128
        if not src_fm:
            for q4 in range(NE // 4):
                bk, bb = P.bank()
                for j in range(4):
                    ec = q4 * 4 + j
                    P.op("pe", mm_group(bk[:, j * 128:(j + 1) * 128], [(ut[i][:, ec * 128:(ec + 1) * 128], C.ident[:])]),
                         reads=[b_ut[i], C.b_ident], writes=[bb])
                P.op("act", lambda en, q4=q4, bk=bk, i=i: en.copy(uT[i][:, q4 * 4:(q4 + 1) * 4, :], bk[:].rearrange("p (a b) -> p a b", a=4)),
                     reads=[bb], writes=[b_uT[i]])
        bks = [P.bank(), P.bank()]
        for nb in range(2):
            P.op("pe", mm_group(bks[nb][0][:], [(uT[i][:, ec, :], w_s[:, ec, nb * 512:(nb + 1) * 512]) for ec in range(NE)]),
                 reads=[b_uT[i]] + b_w, writes=[bks[nb][1]])

        def fill(s, i=i, bks=bks):
            return [("dve", lambda en, nb=nb: en.scalar_tensor_tensor(s[:, nb * 512:(nb + 1) * 512], xs[i][:, nb * 512:(nb + 1) * 512],
                                                                     float(ALPHA), bks[nb][0][:], ALU.mult, ALU.add)) for nb in range(2)]
        C.layernorm(fill, [b_xs[i], bks[0][1], bks[1][1]], y_d[r0:r0 + 128, :], b_y)
    P.finish([b_y])
    P.pop()


RET_H = 4
GAMMA = [1.0 - 2.0 ** (-5.0 - h) for h in range(RET_H)]


def build_ret(T, mode, P=None, io=None, gathered=False):
    own = P is None
    if own:
        P = Prog()
    NCH = T // 128
    cdec = [g ** 128 for g in GAMMA]
    full = (mode == "B")
    x_d = P.dram_or(io, "x", [T, D_MODEL], F32, "ExternalInput")
    win_d = P.dram_or(io, "w_in", [D_MODEL, 6144], F32, "ExternalInput")
    id_d = P.dram_or(io, "ident", [128, 128], F32, "ExternalInput")
    cos_d = P.dram_or(io, "cosT", [128, T], F32, "ExternalInput")
    sin_d = P.dram_or(io, "sinT", [128, T], F32, "ExternalInput")
    kdec_d = P.dram_or(io, "kdec", [128, RET_H], F32, "ExternalInput")
    if full:
        y_d = P.dram_or(io, "y", [T, D_MODEL], F32, "ExternalOutput")
        wout_d = P.dram_or(io, "w_out", [2048, D_MODEL], F32, "ExternalInput")
        gn_d = P.dram_or(io, "gn", [128, 16], F32, "ExternalInput")
        lng_d = P.dram_or(io, "ln_g", [128, D_MODEL], F32, "ExternalInput")
        lnb_d = P.dram_or(io, "ln_b", [128, D_MODEL], F32, "ExternalInput")
        dm_d = P.dram_or(io, "dmT", [128, RET_H, 128], F32, "ExternalInput")
        qdec_d = P.dram_or(io, "qdec", [128, RET_H, 128], F32, "ExternalInput")
        if gathered:
            rg_d = io["rg"]
            cf_d = io["ret_cf"]
        else:
            rp_d = P.dram("rprev", [3, RET_H, 2, 128, 512], F32, "ExternalInput")
        u_d = P.dram_or(io, "u_scr", [T, 2048], BF16, "Internal")
        b_u = P.buf("u_dram")
    else:
        r_d = P.dram_or(io, "r_out", [RET_H, 2, 128, 512], F32, "ExternalOutput")
        b_rd = P.buf("r_dram")

    P.push()
    ident_f = P.sb("ident_f", [128, 128], F32)
    ident = P.sb("ident", [128, 128], BF16)
    b_idf, b_id = P.buf("idf"), P.buf("id")
    P.dma("sp", ident_f[:], id_d, b_idf)
    P.op("dve", lambda e: e.tensor_copy(ident[:], ident_f[:]), reads=[b_idf], writes=[b_id])
    kdec = P.sb("kdec", [128, RET_H], F32)
    b_kdec = P.buf("kdec")
    P.dma("sp", kdec[:], kdec_d, b_kdec)
    w_in = P.sb("w_in", [128, KC, 6144], BF16)
    b_win = [P.buf("win%d" % k) for k in range(KC)]
    for kc in range(KC):
        P.dma("pool", w_in[:, kc, :], win_d[kc * 128:(kc + 1) * 128, :], b_win[kc])
    R = P.sb("R", [128, RET_H, 2, 512], F32)
    Rb = P.sb("Rb", [128, RET_H, 2, 512], BF16)
    b_R = [P.buf("R%d" % h) for h in range(RET_H)]
    b_Rb = [P.buf("Rb%d" % h) for h in range(RET_H)]
    if full:
        dmT = P.sb("dmT", [128, RET_H, 128], F32)
        qdec = P.sb("qdec", [128, RET_H, 128], F32)
        b_dm, b_qdec = P.buf("dm"), P.buf("qdec")
        P.dma("sp", dmT[:], dm_d, b_dm)
        P.dma("sp", qdec[:], qdec_d, b_qdec)
        epsb = P.sb("eps_b", [128, 1], F32)
        nhb = P.sb("nh_b", [128, 1], F32)
        b_cst = P.buf("cst")
        P.op("pool", lambda e: e.memset(nhb[:], -0.5), writes=[b_cst])
        rtmp = [P.sb("rtmp%d" % i, [128, 2, 512], F32) for i in range(2)]
        b_rtmp = [P.buf("rtmp%d" % i) for i in range(2)]
        cnt = 0
        if gathered:
            cf = P.sb("ret_cf", [128, NCORES * RET_H], F32)
            b_cf = P.buf("ret_cf")
            P.dma("sp", cf[:], cf_d, b_cf)
            for p in range(NCORES):
                for h in range(RET_H):
                    i = cnt % 2
                    cnt += 1
                    csc = cf[:, p * RET_H + h:p * RET_H + h + 1]
                    P.dma("sp", rtmp[i][:], rg_d[p, h].rearrange("c p e -> p c e"), b_rtmp[i])
                    if p == 0:
                        P.op("dve", lambda en, i=i, h=h, csc=csc: en.tensor_scalar(R[:, h, :, :], rtmp[i][:], csc, None, ALU.mult),
                             reads=[b_rtmp[i], b_cf], writes=[b_R[h]])
                    else:
                        P.op("dve", lambda en, i=i, h=h, csc=csc: en.scalar_tensor_tensor(R[:, h, :, :], rtmp[i][:], csc, R[:, h, :, :], ALU.mult, ALU.add),
                             reads=[b_rtmp[i], b_cf, b_R[h]], writes=[b_R[h]])
        else:
            for h in range(RET_H):
                P.dma("sp", R[:, h, :, :], rp_d[0, h].rearrange("c p e -> p c e"), b_R[h])
            for k in (1, 2):
                for h in range(RET_H):
                    coef = float(GAMMA[h] ** (T * k))
                    i = cnt % 2
                    cnt += 1
                    P.dma("sp", rtmp[i][:], rp_d[k, h].rearrange("c p e -> p c e"), b_rtmp[i])
                    P.op("dve", lambda en, i=i, h=h, coef=coef: en.scalar_tensor_tensor(R[:, h, :, :], rtmp[i][:], coef, R[:, h, :, :], ALU.mult, ALU.add),
                         reads=[b_rtmp[i], b_R[h]], writes=[b_R[h]])
        for h in range(RET_H):
            P.op("act", lambda en, h=h: en.copy(Rb[:, h, :, :], R[:, h, :, :]), reads=[b_R[h]], writes=[b_Rb[h]])
    else:
        for h in range(RET_H):
            P.op("pool", lambda en, h=h: en.memset(R[:, h, :, :], 0.0), writes=[b_R[h]])

    def dbl(name, shape, dt):
        return [P.sb("%s%d" % (name, i), shape, dt) for i in range(2)], [P.buf("%s%d" % (name, i)) for i in range(2)]
    xb, b_xb = dbl("xb", [128, D_MODEL], BF16)
    xT, b_xT = dbl("xT", [128, KC, 128], BF16)
    cs, b_cs = dbl("cs", [128, 2, 128], F32)
    qk = [[P.sb("qk%d_%d" % (i, h), [128, 4, 128], BF16) for h in range(RET_H)] for i in range(2)]
    b_qk = [[P.buf("qk%d_%d" % (i, h)) for h in range(RET_H)] for i in range(2)]
    kd = [[P.sb("kd%d_%d" % (i, h), [128, 256], BF16) for h in range(RET_H)] for i in range(2)]
    b_kd = [[P.buf("kd%d_%d" % (i, h)) for h in range(RET_H)] for i in range(2)]
    v, b_v = dbl("v", [128, 2048], BF16)
    b_vh = [[P.buf("v%d_%d" % (i, h)) for h in range(RET_H)] for i in range(2)]
    t1, b_t1 = dbl("t1", [128, 4, 128], F32)
    t2, b_t2 = dbl("t2", [128, 4, 128], F32)
    if full:
        sgt = [P.sb("sgt%d" % i, [128, 2048], BF16) for i in range(2)]
        b_sgt = [[P.buf("sgt%d_%d" % (i, h)) for h in range(RET_H)] for i in range(2)]
        PT = P.sb("PT", [128, RET_H, 128], BF16)
        b_PT = P.buf("PT")
        qd = [P.sb("qd%d" % h, [128, 2, 128], BF16) for h in range(RET_H)]
        b_qd = [P.buf("qd%d" % h) for h in range(RET_H)]
        u = [P.sb("u%d" % i, [128, 2048], BF16) for i in range(2)]
        b_us = [P.buf("u%d" % i) for i in range(2)]
        b_uh = [[P.buf("u%d_%d" % (i, h)) for h in range(RET_H)] for i in range(2)]
        gst = P.sb("gst", [128, RET_H, 6], F32)
        gmv = P.sb("gmv", [128, RET_H, 2], F32)
        grs = P.sb("grs", [128, RET_H, 2], F32)
        b_gs = [P.buf("gs%d" % h) for h in range(RET_H)]

    def stage1(n):
        i = n % 2
        r0 = n * 128
        P.dma("pool", xb[i][:], x_d[r0:r0 + 128, :], b_xb[i])
        P.dma("sp", cs[i][:, 0, :], cos_d[:, r0:r0 + 128], b_cs[i])
        P.dma("sp", cs[i][:, 1, :], sin_d[:, r0:r0 + 128], b_cs[i])
        for half in range(2):
            bk, bb = P.bank()
            for q in range(4):
                kc = half * 4 + q
                P.op("pe", mm_group(bk[:, q * 128:(q + 1) * 128], [(xb[i][:, kc * 128:(kc + 1) * 128], ident[:])]),
                     reads=[b_xb[i], b_id], writes=[bb])
            P.op("dve", lambda en, half=half, bk=bk: en.tensor_copy(xT[i][:, half * 4:(half + 1) * 4, :], bk[:].rearrange("p (a b) -> p a b", a=4)),
                 reads=[bb], writes=[b_xT[i]])
        for h in range(RET_H):
            bk, bb = P.bank()
            for q4, cc in enumerate([2 * h, 2 * h + 1, 8 + 2 * h, 8 + 2 * h + 1]):
                P.op("pe", mm_group(bk[:, q4 * 128:(q4 + 1) * 128], [(w_in[:, kc, cc * 128:(cc + 1) * 128], xT[i][:, kc, :]) for kc in range(KC)]),
                     reads=b_win + [b_xT[i]], writes=[bb])
            j = h % 2
            bk3 = bk[:].rearrange("p (a b) -> p a b", a=4)
            cosb = cs[i][:, 0, :].unsqueeze(1).broadcast_to([128, 4, 128])
            sinb = cs[i][:, 1, :].unsqueeze(1).broadcast_to([128, 4, 128])
            P.op("dve", lambda en, j=j, bk3=bk3, cosb=cosb: en.tensor_tensor(t1[j][:], bk3, cosb, ALU.mult), reads=[bb, b_cs[i]], writes=[b_t1[j]])
            P.op("dve", lambda en, j=j, bk3=bk3, sinb=sinb: en.tensor_tensor(t2[j][:], bk3, sinb, ALU.mult), reads=[bb, b_cs[i]], writes=[b_t2[j]])
            t1v = t1[j][:].rearrange("p (a b) c -> p a b c", a=2)
            t2v = t2[j][:].rearrange("p (a b) c -> p a b c", a=2)
            qkv = qk[i][h][:].rearrange("p (a b) c -> p a b c", a=2)
            P.op("pool", lambda en, t1v=t1v, t2v=t2v, qkv=qkv: en.tensor_tensor(qkv[:, :, 0, :], t1v[:, :, 0, :], t2v[:, :, 1, :], ALU.subtract),
                 reads=[b_t1[j], b_t2[j]], writes=[b_qk[i][h]])
            P.op("pool", lambda en, t1v=t1v, t2v=t2v, qkv=qkv: en.tensor_tensor(qkv[:, :, 1, :], t1v[:, :, 1, :], t2v[:, :, 0, :], ALU.add),
                 reads=[b_t1[j], b_t2[j], b_qk[i][h]], writes=[b_qk[i][h]])
        for nb in range(8 if full else 4):
            bk, bb = P.bank()
            c0 = 2048 + nb * 512
            P.op("pe", mm_group(bk[:], [(xT[i][:, kc, :], w_in[:, kc, c0:c0 + 512]) for kc in range(KC)]), reads=b_win + [b_xT[i]], writes=[bb])
            if nb < 4:
                P.op("act", lambda en, nb=nb, bk=bk: en.copy(v[i][:, nb * 512:(nb + 1) * 512], bk[:]), reads=[bb], writes=[b_vh[i][nb]])
            else:
                hh = nb - 4
                P.op("act", lambda en, hh=hh, bk=bk: en.activation(sgt[i][:, hh * 512:(hh + 1) * 512], bk[:], AF.Silu), reads=[bb], writes=[b_sgt[i][hh]])
        for h in range(RET_H):
            bk2, bb2 = P.bank()
            for dc in range(2):
                P.op("pe", mm_group(bk2[:, dc * 128:(dc + 1) * 128], [(qk[i][h][:, 2 + dc, :], ident[:])]), reads=[b_qk[i][h], b_id], writes=[bb2])
            P.op("act", lambda en, h=h, bk2=bk2: en.activation(kd[i][h][:], bk2[:, 0:256], AF.Copy, scale=kdec[:, h:h + 1]),
                 reads=[bb2, b_kdec], writes=[b_kd[i][h]])

    def stage2(n):
        i = n % 2
        r0 = n * 128
        if full:
            bkS, bbS = P.bank()
            for h in range(RET_H):
                P.op("pe", mm_group(bkS[:, h * 128:(h + 1) * 128], [(qk[i][h][:, 2, :], qk[i][h][:, 0, :]), (qk[i][h][:, 3, :], qk[i][h][:, 1, :])]),
                     reads=[b_qk[i][h]], writes=[bbS])
            P.op("dve", lambda en, bkS=bkS: en.tensor_tensor(PT[:], bkS[:].rearrange("p (a b) -> p a b", a=4), dmT[:], ALU.mult),
                 reads=[bbS, b_dm], writes=[b_PT])
            for h in range(RET_H):
                qdb = qdec[:, h, :].unsqueeze(1).broadcast_to([128, 2, 128])
                P.op("pool", lambda en, h=h, qdb=qdb: en.tensor_tensor(qd[h][:], qk[i][h][:, 0:2, :], qdb, ALU.mult),
                     reads=[b_qk[i][h], b_qdec], writes=[b_qd[h]])
        for h in range(RET_H):
            for dc in range(2):
                bk, bb = P.bank()
                P.op("pe", mm_group(bk[:], [(kd[i][h][:, dc * 128:(dc + 1) * 128], v[i][:, h * 512:(h + 1) * 512])]),
                     reads=[b_kd[i][h], b_vh[i][h]], writes=[bb])
                P.op("dve", lambda en, h=h, dc=dc, bk=bk: en.scalar_tensor_tensor(R[:, h, dc, :], R[:, h, dc, :], float(cdec[h]), bk[:], ALU.mult, ALU.add),
                     reads=[bb, b_R[h]], writes=[b_R[h]])
        if full:
            bR = []
            for h in range(RET_H):
                bkR, bbR = P.bank()
                bR.append((bkR, bbR))
                P.op("pe", mm_group(bkR[:], [(PT[:, h, :], v[i][:, h * 512:(h + 1) * 512]),
                                            (qd[h][:, 0, :], Rb[:, h, 0, :]), (qd[h][:, 1, :], Rb[:, h, 1, :])]),
                     reads=[b_PT, b_vh[i][h], b_qd[h], b_Rb[h]], writes=[bbR])
            for h in range(RET_H):
                bkR, bbR = bR[h]
                P.op("dve", lambda en, h=h, bkR=bkR: en.bn_stats(gst[:, h, :], bkR[:]), reads=[bbR], writes=[b_gs[h]])
                P.op("dve", lambda en, h=h: en.bn_aggr(gmv[:, h, :], gst[:, h, :]), reads=[b_gs[h]], writes=[b_gs[h]])
            for h in range(RET_H):
                P.op("pool", lambda en, h=h: en.tensor_scalar(grs[:, h, 0:1], gmv[:, h, 1:2], LN_EPS, None, ALU.add), reads=[b_gs[h]], writes=[b_gs[h]])
                P.op("pool", lambda en, h=h: en.tensor_tensor(grs[:, h, 1:2], grs[:, h, 0:1], nhb[:], ALU.pow), reads=[b_gs[h], b_cst], writes=[b_gs[h]])
            for h in range(RET_H):
                bkR, bbR = bR[h]
                us = u[i][:, h * 512:(h + 1) * 512]
                P.op("dve", lambda en, h=h, bkR=bkR, us=us: en.tensor_scalar(us, bkR[:], gmv[:, h, 0:1], grs[:, h, 1:2], ALU.subtract, ALU.mult),
                     reads=[bbR, b_gs[h]], writes=[b_uh[i][h]])
            for h in range(RET_H):
                us = u[i][:, h * 512:(h + 1) * 512]
                P.op("pool", lambda en, h=h, us=us: en.tensor_tensor(us, us, sgt[i][:, h * 512:(h + 1) * 512], ALU.mult),
                     reads=[b_uh[i][h], b_sgt[i][h]], writes=[b_uh[i][h]])
            P.store("sp", u_d[r0:r0 + 128, :], u[i][:], b_us[i], also=b_uh[i])
            for h in range(RET_H):
                P.op("act", lambda en, h=h: en.copy(Rb[:, h, :, :], R[:, h, :, :]), reads=[b_R[h]], writes=[b_Rb[h]])

    stage1(0)
    for n in range(NCH):
        if n + 1 < NCH:
            stage1(n + 1)
        stage2(n)
    if not full:
        for h in range(RET_H):
            P.store("sp", r_d[h].rearrange("c p e -> p c e"), R[:, h, :, :], b_R[h])
        P.finish([b_rd])
    P.pop()
    if full:
        emit_proj_ln(P, T, 2048, x_d, u_d, wout_d, gn_d, lng_d, lnb_d, id_d, y_d, b_u, src_fm=False)
    if own:
        return P.emit()


def ret_consts(T, pos0):
    half = 128
    inv = (10000.0 ** (-np.arange(half, dtype=np.float32) / half)).astype(np.float32)
    pos = (pos0 + np.arange(T)).astype(np.float32)
    ang = pos[None, :] * inv[:, None]
    idx = np.arange(128, dtype=np.float64)
    dm = np.zeros((128, RET_H, 128), np.float32)
    qdec = np.zeros((128, RET_H, 128), np.float32)
    kdec = np.zeros((128, RET_H), np.float32)
    for h in range(RET_H):
        lg = np.log(GAMMA[h])
        rel = idx[None, :] - idx[:, None]
        dm[:, h, :] = np.where(rel >= 0, np.exp(lg * np.maximum(rel, 0.0)), 0.0) / 16.0
        qdec[:, h, :] = np.exp(lg * (idx + 1.0))[None, :]
        kdec[:, h] = np.exp(lg * (127.0 - idx)) / 16.0
    return {"cosT": np.cos(ang).astype(np.float32), "sinT": np.sin(ang).astype(np.float32),
            "dmT": dm, "qdec": qdec, "kdec": kdec}


def run_ret(mode, x_shards, pos0s, w_in, w_out=None, gn=None, ln_g=None, ln_b=None, rprevs=None):
    T = x_shards[0].shape[0]
    nc = build_ret(T, mode)
    ident = np.eye(128, dtype=np.float32)
    maps = []
    for c, xs in enumerate(x_shards):
        cst = ret_consts(T, pos0s[c])
        m = {"x": np.ascontiguousarray(xs), "w_in": w_in, "ident": ident, "cosT": cst["cosT"], "sinT": cst["sinT"], "kdec": cst["kdec"]}
        if mode == "B":
            m.update({"w_out": w_out, "gn": np.ascontiguousarray(gn.reshape(16, 128).T), "ln_g": _rep(ln_g), "ln_b": _rep(ln_b),
                      "dmT": cst["dmT"], "qdec": cst["qdec"], "rprev": rprevs[c]})
        maps.append(m)
    res = run_bass_kernel_spmd(nc, maps, core_ids=list(range(len(maps))))
    return [r["r_out" if mode == "A" else "y"] for r in res.results]


DIL = (1, 4, 16)
HALO = 2048
ATT_COLS = 4608


def build_att(T, debug=False, P=None, io=None):
    own = P is None
    if own:
        P = Prog()
    SK = "ExternalOutput" if debug else "Internal"
    H = HALO
    TE = H + T
    xe_d = P.dram_or(io, "x_ext", [TE, D_MODEL], F32, "ExternalInput")
    y_d = P.dram_or(io, "y", [T, D_MODEL], F32, "ExternalOutput")
    w_d = P.dram_or(io, "w_qkv", [D_MODEL, ATT_COLS], F32, "ExternalInput")
    wo_d = P.dram_or(io, "w_out", [512, D_MODEL], F32, "ExternalInput")
    id_d = P.dram_or(io, "ident", [128, 128], F32, "ExternalInput")
    lng_d = P.dram_or(io, "ln_g", [128, D_MODEL], F32, "ExternalInput")
    lnb_d = P.dram_or(io, "ln_b", [128, D_MODEL], F32, "ExternalInput")
    mask_d = P.dram_or(io, "mask2", [128, 256], F32, "ExternalInput")
    hv_d = P.dram_or(io, "halo_valid", [128, 1], F32, "ExternalInput")
    qT_d = P.dram_or(io, "qT_scr", [12, 128, T], BF16, SK)
    kT_d = P.dram_or(io, "kT_scr", [12, 128, TE], BF16, SK)
    v_d = P.dram_or(io, "v_scr", [TE, 1536], BF16, SK)
    oT_d = P.dram_or(io, "oT_scr", [512, T], BF16, SK)
    b_qd, b_kd, b_vd, b_od = P.buf("qT_d"), P.buf("kT_d"), P.buf("v_d"), P.buf("oT_d")

    P.push()
    ident_f = P.sb("ident_f", [128, 128], F32)
    ident = P.sb("ident", [128, 128], BF16)
    b_idf, b_id = P.buf("idf"), P.buf("id")
    P.dma("sp", ident_f[:], id_d, b_idf)
    P.op("dve", lambda e: e.tensor_copy(ident[:], ident_f[:]), reads=[b_idf], writes=[b_id])
    w_s = P.sb("wqkv", [128, KC, ATT_COLS], BF16)
    b_w = [P.buf("wqkv%d" % k) for k in range(KC)]
    for kc in range(KC):
        P.dma("pool", w_s[:, kc, :], w_d[kc * 128:(kc + 1) * 128, :], b_w[kc])
    xb = [P.sb("xb%d" % i, [128, 4, D_MODEL], BF16) for i in range(2)]
    b_xbt = [[P.buf("xb%d_%d" % (i, t)) for t in range(4)] for i in range(2)]
    xT = [P.sb("xT%d" % i, [128, KC, 512], BF16) for i in range(2)]
    b_xT = [P.buf("xT%d" % i) for i in range(2)]
    stg = [P.sb("stg%d" % i, [128, 512], BF16) for i in range(4)]
    b_stg = [P.buf("stg%d" % i) for i in range(4)]
    vst = [P.sb("vst%d" % i, [128, 1536], BF16) for i in range(2)]
    b_vst = [P.buf("vst%d" % i) for i in range(2)]
    nstg = 0
    nv = 0
    for blk in range(TE // 512):
        i = blk % 2
        t0 = blk * 512
        for tt in range(4):
            P.dma("pool", xb[i][:, tt, :], xe_d[t0 + tt * 128:t0 + (tt + 1) * 128, :], b_xbt[i][tt])
        for tt in range(4):
            for half in range(2):
                bk, bb = P.bank()
                for q in range(4):
                    kc = half * 4 + q
                    P.op("pe", mm_group(bk[:, q * 128:(q + 1) * 128], [(xb[i][:, tt, kc * 128:(kc + 1) * 128], ident[:])]),
                         reads=[b_xbt[i][tt], b_id], writes=[bb])
                P.op("dve", lambda en, half=half, bk=bk, tt=tt, i=i: en.tensor_copy(
                    xT[i][:, half * 4:(half + 1) * 4, tt * 128:(tt + 1) * 128], bk[:].rearrange("p (a b) -> p a b", a=4)),
                    reads=[bb], writes=[b_xT[i]])
        is_q = t0 >= H
        for cc in range(24):
            if cc < 12 and not is_q:
                continue
            bk, bb = P.bank()
            P.op("pe", mm_group(bk[:], [(w_s[:, kc, cc * 128:(cc + 1) * 128], xT[i][:, kc, :]) for kc in range(KC)]),
                 reads=b_w + [b_xT[i]], writes=[bb])
            s = nstg % 4
            nstg += 1
            eng = "act" if (nstg % 2) else "dve"
            if eng == "act":
                P.op("act", lambda en, s=s, bk=bk: en.copy(stg[s][:], bk[:]), reads=[bb], writes=[b_stg[s]])
            else:
                P.op("dve", lambda en, s=s, bk=bk: en.tensor_copy(stg[s][:], bk[:]), reads=[bb], writes=[b_stg[s]])
            if cc < 12:
                P.store("sp", qT_d[cc, :, t0 - H:t0 - H + 512], stg[s][:], b_stg[s])
            else:
                P.store("sp", kT_d[cc - 12, :, t0:t0 + 512], stg[s][:], b_stg[s])
        for tt in range(4):
            s = nv % 2
            nv += 1
            for g in range(3):
                bk, bb = P.bank()
                c0 = 3072 + g * 512
                P.op("pe", mm_group(bk[:], [(xT[i][:, kc, tt * 128:(tt + 1) * 128], w_s[:, kc, c0:c0 + 512]) for kc in range(KC)]),
                     reads=b_w + [b_xT[i]], writes=[bb])
                P.op("act", lambda en, s=s, g=g, bk=bk: en.copy(vst[s][:, g * 512:(g + 1) * 512], bk[:]), reads=[bb], writes=[b_vst[s]])
            P.store("sp", v_d[t0 + tt * 128:t0 + (tt + 1) * 128, :], vst[s][:], b_vst[s])
    P.pop()

    P.push()
    mask_f = P.sb("mask_f", [128, 256], F32)
    mask2 = P.sb("mask2", [128, 256], BF16)
    maskH = P.sb("maskH", [128, 256], BF16)
    hv = P.sb("hv", [128, 1], F32)
    ones = P.sb("ones", [128, 128], BF16)
    b_mf, b_m2, b_mH, b_hv, b_ones = P.buf("mf"), P.buf("m2"), P.buf("mH"), P.buf("hv"), P.buf("ones")
    P.dma("sp", mask_f[:], mask_d, b_mf)
    P.dma("sp", hv[:], hv_d, b_hv)
    P.op("dve", lambda e: e.tensor_copy(mask2[:], mask_f[:]), reads=[b_mf], writes=[b_m2])
    P.op("dve", lambda e: e.tensor_copy(maskH[:, 128:256], mask_f[:, 128:256]), reads=[b_mf], writes=[b_mH])
    P.op("dve", lambda e: e.tensor_scalar(maskH[:, 0:128], mask_f[:, 0:128], hv[:, 0:1], None, ALU.mult), reads=[b_mf, b_hv, b_mH], writes=[b_mH])
    P.op("pool", lambda e: e.memset(ones[:], 1.0), writes=[b_ones])
    acc = P.sb("acc", [128, 2, T], F32)
    b_acc = P.buf("acc")
    qT = [P.sb("qT%d" % i, [128, T], BF16) for i in range(2)]
    b_qT = [P.buf("qT%d" % i) for i in range(2)]
    kT = [P.sb("kT%d" % i, [128, TE], BF16) for i in range(2)]
    b_kT = [P.buf("kT%d" % i) for i in range(2)]
    NBMAX = T // 128 + 1
    NVT = 4
    vt = [P.sb("vt%d" % i, [128, NBMAX, 128], BF16) for i in range(NVT)]
    b_vt = [P.buf("vt%d" % i) for i in range(NVT)]
    NET = 4
    ET = [P.sb("ET%d" % i, [128, 256], BF16) for i in range(NET)]
    b_ET = [P.buf("ET%d" % i) for i in range(NET)]
    oT = P.sb("oT", [128, T], BF16)
    b_oT = P.buf("oT")
    rz = P.sb("rz", [128, T], F32)
    b_rz = P.buf("rz")
    scale = float(128 ** -0.5)
    nqk = 0
    nvt = 0
    net = [0]
    for h in range(4):
        for g in range(3):
            d = DIL[g]
            Hg = 128 * d
            L = Hg + T
            i = nqk % 2
            nqk += 1
            P.dma("sp", qT[i][:], qT_d[g * 4 + h], b_qT[i], reads=[b_qd])
            P.dma("sp", kT[i][:, 0:L], kT_d[g * 4 + h, :, H - Hg:H + T], b_kT[i], reads=[b_kd])
            qv = qT[i][:].rearrange("p (m d) -> p d m", d=d)
            kv = kT[i][:, 0:L].rearrange("p (m d) -> p d m", d=d)
            av = acc[:].rearrange("p c (m d) -> p c d m", d=d)
            na = T // (128 * d)
            nb = na + 1
            tiles = []
            vsrc = {}
            for r in range(d):
                j = nvt % NVT
                nvt += 1
                vsrc[r] = (j, bass.AP(v_d.tensor, (H - Hg + r) * 1536 + g * 512 + h * 128,
                                      [[d * 1536, 128], [128 * d * 1536, nb], [1, 128]]))
                for a in range(na):
                    tiles.append((i, j, r, a, g, qv, kv, av))
            def sA(tl):
                i, j, r, a, g, qv, kv, av = tl
                bk, bb = P.bank()
                qa = qv[:, r, a * 128:(a + 1) * 128]
                P.op("pe", mm_group(bk[:, 0:128], [(kv[:, r, a * 128:(a + 1) * 128], qa)]), reads=[b_kT[i], b_qT[i]], writes=[bb])
                P.op("pe", mm_group(bk[:, 128:256], [(kv[:, r, (a + 1) * 128:(a + 2) * 128], qa)]), reads=[b_kT[i], b_qT[i]], writes=[bb])
                e = net[0] % NET
                net[0] += 1
                P.op("act", lambda en, e=e, bk=bk: en.activation(ET[e][:], bk[:, 0:256], AF.Exp, scale=scale), reads=[bb], writes=[b_ET[e]])
                mk, bmk = (maskH, b_mH) if a == 0 else (mask2, b_m2)
                P.op("pool", lambda en, e=e, mk=mk: en.tensor_tensor(ET[e][:], ET[e][:], mk[:], ALU.mult), reads=[b_ET[e], bmk], writes=[b_ET[e]])
                return e

            def sB(tl, e):
                i, j, r, a, g, qv, kv, av = tl
                bo, bbo = P.bank()
                P.op("pe", mm_group(bo[:, 0:128], [(vt[j][:, a, :], ET[e][:, 0:128]), (vt[j][:, a + 1, :], ET[e][:, 128:256])]),
                     reads=[b_vt[j], b_ET[e]], writes=[bbo])
                P.op("pe", mm_group(bo[:, 128:256], [(ones[:], ET[e][:, 0:128]), (ones[:], ET[e][:, 128:256])]),
                     reads=[b_ones, b_ET[e]], writes=[bbo])
                dst = av[:, :, r, a * 128:(a + 1) * 128]
                bo3 = bo[:, 0:256].rearrange("p (c m) -> p c m", c=2)
                if g == 0:
                    P.op("dve", lambda en, dst=dst, bo3=bo3: en.tensor_copy(dst, bo3), reads=[bbo], writes=[b_acc])
                else:
                    P.op("dve", lambda en, dst=dst, bo3=bo3: en.tensor_tensor(dst, dst, bo3, ALU.add), reads=[bbo, b_acc], writes=[b_acc])

            pend = []
            for tl in tiles:
                if tl[3] == 0:
                    jj, src = vsrc[tl[2]]
                    P.dma("sp", vt[jj][:, 0:nb, :], src, b_vt[jj], reads=[b_vd])
                pend.append((tl, sA(tl)))
                if len(pend) > 2:
                    sB(*pend.pop(0))
            for pp in pend:
                sB(*pp)
        P.op("dve", lambda en: en.reciprocal(rz[:], acc[:, 1, :]), reads=[b_acc], writes=[b_rz])
        P.op("dve", lambda en: en.tensor_tensor(oT[:], acc[:, 0, :], rz[:], ALU.mult), reads=[b_acc, b_rz], writes=[b_oT])
        P.store("sp", oT_d[h * 128:(h + 1) * 128, :], oT[:], b_oT)
    P.pop()

    emit_proj_ln(P, T, 512, xe_d[H:TE, :], oT_d, wo_d, None, lng_d, lnb_d, id_d, y_d, b_od, src_fm=True)
    if own:
        return P.emit()


def att_mask():
    j = np.arange(128)[:, None]
    i = np.arange(128)[None, :]
    return np.concatenate([(j >= i), (j <= i)], axis=1).astype(np.float32)


def run_att(x_ext_shards, halo_valid, w_qkv, w_out, ln_g, ln_b, debug=False):
    T = x_ext_shards[0].shape[0] - HALO
    nc = build_att(T, debug)
    ident = np.eye(128, dtype=np.float32)
    maps = []
    for c, xe in enumerate(x_ext_shards):
        maps.append({"x_ext": np.ascontiguousarray(xe), "w_qkv": w_qkv, "w_out": w_out, "ident": ident,
                     "ln_g": _rep(ln_g), "ln_b": _rep(ln_b), "mask2": att_mask(),
                     "halo_valid": np.full((128, 1), float(halo_valid[c]), np.float32)})
    res = run_bass_kernel_spmd(nc, maps, core_ids=list(range(len(maps))))
    if debug:
        return res.results
    return [r["y"] for r in res.results]


def emit_halo(P, T, E1, hsrc, Hg, sel_d):
    H = HALO
    P.push()
    b_hs, b_hg, b_e1 = P.buf("hsrc"), P.buf("Hg"), P.buf("E1halo")
    P.dma("sp", hsrc, E1[H + T - H:H + T, :], b_hs)
    P.allgather(hsrc, Hg, b_hg, [b_hs], NCORES)
    sel = P.sb("hsel", [128, NCORES], F32)
    b_sel = P.buf("hsel")
    P.dma("sp", sel[:], sel_d, b_sel)
    tmp = [P.sb("htmp%d" % i, [128, D_MODEL], F32) for i in range(3)]
    b_tmp = [P.buf("htmp%d" % i) for i in range(3)]
    acc = [P.sb("hacc%d" % i, [128, D_MODEL], F32) for i in range(2)]
    b_acc = [P.buf("hacc%d" % i) for i in range(2)]
    n = 0
    for t in range(H // 128):
        a = t % 2
        for p in range(NCORES):
            i = n % 3
            n += 1
            r0 = p * H + t * 128
            P.dma("sp", tmp[i][:], Hg[r0:r0 + 128, :], b_tmp[i], reads=[b_hg])
            sc = sel[:, p:p + 1]
            if p == 0:
                P.op("dve", lambda en, i=i, a=a, sc=sc: en.tensor_scalar(acc[a][:], tmp[i][:], sc, None, ALU.mult),
                     reads=[b_tmp[i], b_sel], writes=[b_acc[a]])
            else:
                P.op("dve", lambda en, i=i, a=a, sc=sc: en.scalar_tensor_tensor(acc[a][:], tmp[i][:], sc, acc[a][:], ALU.mult, ALU.add),
                     reads=[b_tmp[i], b_sel, b_acc[a]], writes=[b_acc[a]])
        P.store("sp", E1[t * 128:(t + 1) * 128, :], acc[a][:], b_acc[a])
    P.pop()


def build_fused(T):
    P = Prog()
    H = HALO
    inp = lambda n, shp: P.dram(n, shp, F32, "ExternalInput")
    x_d = inp("x", [T, D_MODEL])
    y_d = P.dram("y", [T, D_MODEL], F32, "ExternalOutput")
    lng = inp("ln_g", [2 * DEPTH, 128, D_MODEL])
    lnb = inp("ln_b", [2 * DEPTH, 128, D_MODEL])
    rwin = inp("ret_w_in", [2, D_MODEL, 6144])
    rwout = inp("ret_w_out", [2, 2048, D_MODEL])
    rgn = inp("ret_gn", [2, 128, 16])
    awq = inp("att_w_qkv", [2, D_MODEL, ATT_COLS])
    awo = inp("att_w_out", [2, 512, D_MODEL])
    fwg = inp("ffn_wg", [2, 1, D_MODEL, D_FF])
    fwu = inp("ffn_wu", [2, 1, D_MODEL, D_FF])
    fwd = inp("ffn_wd", [2, 1, D_FF, D_MODEL])
    mwr = inp("moe_wr", [2, D_MODEL, N_EXPERTS])
    mwg = inp("moe_wg", [2, N_EXPERTS, D_MODEL, D_FF])
    mwu = inp("moe_wu", [2, N_EXPERTS, D_MODEL, D_FF])
    mwd = inp("moe_wd", [2, N_EXPERTS, D_FF, D_MODEL])
    ident = inp("ident", [128, 128])
    cosT, sinT = inp("cosT", [128, T]), inp("sinT", [128, T])
    kdec, dmT, qdec = inp("kdec", [128, RET_H]), inp("dmT", [128, RET_H, 128]), inp("qdec", [128, RET_H, 128])
    mask2, hvalid = inp("mask2", [128, 256]), inp("halo_valid", [128, 1])
    hsel, rcf = inp("halo_sel", [128, NCORES]), inp("ret_cf", [128, NCORES * RET_H])
    itn = lambda n, shp, dt=F32: P.dram(n, shp, dt, "Internal")
    E1 = itn("E1", [H + T, D_MODEL])
    M = itn("Mmid", [T, D_MODEL])
    E2 = itn("E2", [T, D_MODEL])
    hsrc = itn("hsrc", [H, D_MODEL])
    Hg = itn("Hg", [NCORES * H, D_MODEL])
    Rl = itn("Rl", [RET_H * 2 * 128, 512])
    Rg = itn("Rg", [NCORES * RET_H * 2 * 128, 512])
    scr = {"u_scr": itn("u_scr", [T, 2048], BF16), "qT_scr": itn("qT_scr", [12, 128, T], BF16),
           "kT_scr": itn("kT_scr", [12, 128, H + T], BF16), "v_scr": itn("v_scr", [H + T, 1536], BF16),
           "oT_scr": itn("oT_scr", [512, T], BF16)}
    Rl5 = Rl.rearrange("(h c p) e -> h c p e", h=RET_H, c=2)
    Rg5 = Rg.rearrange("(n h c p) e -> n h c p e", n=NCORES, h=RET_H, c=2)
    cur = x_d
    for i in range(DEPTH):
        j = i // 2
        last = (i == DEPTH - 1)
        if i % 2 == 0:
            base = {"x": cur, "w_in": rwin[j], "ident": ident, "cosT": cosT, "sinT": sinT, "kdec": kdec}
            ioA = dict(base)
            ioA["r_out"] = Rl5
            build_ret(T, "A", P=P, io=ioA)
            P.push()
            b_rg = P.buf("Rg")
            P.allgather(Rl, Rg, b_rg, [], NCORES)
            P.pop()
            ioB = dict(base)
            ioB.update({"y": M, "w_out": rwout[j], "gn": rgn[j], "ln_g": lng[2 * i], "ln_b": lnb[2 * i], "dmT": dmT, "qdec": qdec,
                        "rg": Rg5, "ret_cf": rcf, "u_scr": scr["u_scr"]})
            build_ret(T, "B", P=P, io=ioB, gathered=True)
            out = E1[H:H + T, :]
            build_ffn(T, 1, TG=min(T, 2048), P=P, io={"x": M, "y": out, "ln_g": lng[2 * i + 1], "ln_b": lnb[2 * i + 1], "ident": ident,
                                     "wg": fwg[j], "wu": fwu[j], "wd": fwd[j]})
            cur = out
        else:
            emit_halo(P, T, E1, hsrc, Hg, hsel)
            ioT = {"x_ext": E1, "y": M, "w_qkv": awq[j], "w_out": awo[j], "ident": ident, "ln_g": lng[2 * i], "ln_b": lnb[2 * i],
                   "mask2": mask2, "halo_valid": hvalid}
            ioT.update({k: scr[k] for k in ("qT_scr", "kT_scr", "v_scr", "oT_scr")})
            build_att(T, P=P, io=ioT)
            out = y_d if last else E2
            build_ffn(T, N_EXPERTS, TG=min(T, 2048), P=P, io={"x": M, "y": out, "ln_g": lng[2 * i + 1], "ln_b": lnb[2 * i + 1], "ident": ident,
                                             "wg": mwg[j], "wu": mwu[j], "wd": mwd[j], "wr": mwr[j]})
            cur = out
    return P.emit()


_PROGS = {}


def kernel(x, ln_gain, ln_bias, ret_w_in, ret_gn_gain, ret_w_out, att_w_qkv, att_w_out,
           ffn_w_gate, ffn_w_up, ffn_w_down, moe_w_router, moe_w_gate, moe_w_up, moe_w_down):
    f = lambda a: np.ascontiguousarray(np.asarray(a, dtype=np.float32))
    x = f(x)
    B, S, D = x.shape
    T = (B * S) // NCORES
    CPB = NCORES // B
    if T not in _PROGS:
        _PROGS[T] = build_fused(T)
    nc = _PROGS[T]
    lg, lb = f(ln_gain).reshape(2 * DEPTH, D), f(ln_bias).reshape(2 * DEPTH, D)
    shared = {
        "ln_g": np.ascontiguousarray(np.broadcast_to(lg[:, None, :], (2 * DEPTH, 128, D))),
        "ln_b": np.ascontiguousarray(np.broadcast_to(lb[:, None, :], (2 * DEPTH, 128, D))),
        "ret_w_in": f(ret_w_in), "ret_w_out": f(ret_w_out),
        "ret_gn": np.ascontiguousarray(f(ret_gn_gain).reshape(2, 16, 128).transpose(0, 2, 1)),
        "att_w_qkv": f(att_w_qkv), "att_w_out": f(att_w_out),
        "ffn_wg": f(ffn_w_gate)[:, None], "ffn_wu": f(ffn_w_up)[:, None], "ffn_wd": f(ffn_w_down)[:, None],
        "moe_wr": f(moe_w_router), "moe_wg": f(moe_w_gate), "moe_wu": f(moe_w_up), "moe_wd": f(moe_w_down),
        "ident": np.eye(128, dtype=np.float32), "mask2": att_mask(),
    }
    xf = x.reshape(B * S, D)
    maps = []
    for c in range(NCORES):
        cst = ret_consts(T, (c % CPB) * T)
        first = (c % CPB == 0)
        hs = np.zeros((128, NCORES), np.float32)
        if not first:
            hs[:, c - 1] = 1.0
        cf = np.zeros((128, NCORES, RET_H), np.float32)
        for p in range(NCORES):
            if p < c and p // CPB == c // CPB:
                for h in range(RET_H):
                    cf[:, p, h] = GAMMA[h] ** (T * (c - 1 - p))
        m = dict(shared)
        m.update({"x": xf[c * T:(c + 1) * T], "cosT": cst["cosT"], "sinT": cst["sinT"], "kdec": cst["kdec"], "dmT": cst["dmT"],
                  "qdec": cst["qdec"], "halo_valid": np.full((128, 1), 0.0 if first else 1.0, np.float32),
                  "halo_sel": hs, "ret_cf": cf.reshape(128, NCORES * RET_H)})
        maps.append(m)
    res = run_bass_kernel_spmd(nc, maps, core_ids=list(range(NCORES))).results
    return np.concatenate([r["y"] for r in res], axis=0).reshape(B, S, D).astype(np.float32)
```

```python
import numpy as np
from contextlib import ExitStack
import concourse.bass as bass
import concourse.mybir as mybir
from concourse.bass_utils import run_bass_kernel_spmd

F32 = mybir.dt.float32
BF16 = mybir.dt.bfloat16
AF = mybir.ActivationFunctionType
ALU = mybir.AluOpType
AX = mybir.AxisListType

D_MODEL = 1024
DEPTH = 4
D_FF = 3584
N_EXPERTS = 8
ALPHA = (2 * DEPTH) ** 0.25
LN_EPS = 1e-5
NCORES = 8
KC = D_MODEL // 128

ENGS = ("pe", "act", "dve", "pool", "sp")


class Buf:
    __slots__ = ("name", "w", "r", "dkey", "dcnt", "skey", "scnt")

    def __init__(self, name):
        self.name = name
        self.w = None
        self.r = {}
        self.dkey = None
        self.dcnt = 0
        self.skey = None
        self.scnt = 0


class Prog:
    def __init__(self):
        self.nc = bass.Bass("TRN2", target_bir_lowering=False)
        self.es = ExitStack()
        self.ops = {e: [] for e in ENGS}
        self.cnt = {e: 0 for e in ENGS}
        self.seen = {e: {} for e in ENGS}
        self.sems = {}
        self.dtot = {}
        self.nbuf = 0
        for e in ("pe", "act", "dve", "pool"):
            self.sems[e] = self.es.enter_context(self.nc.semaphore("s_" + e))
        self.stack = [self.es]
        self.banks = None
        self.bank_i = 0
        self.free_dsems = []
        self.phase_sems = [[]]

    def push(self):
        st = ExitStack()
        self.stack.append(st)
        self.phase_sems.append([])

    def pop(self):
        self.barrier()
        self.flush()
        self.stack.pop().close()
        self.banks = None
        self.free_dsems.extend(self.phase_sems.pop())

    def _dsem(self, dst, prefix):
        if self.free_dsems:
            dst.dkey = self.free_dsems.pop()
            dst.dcnt = self.dtot[dst.dkey]
        else:
            dst.dkey = prefix + dst.name + "_%d" % len(self.sems)
            self.sems[dst.dkey] = self.es.enter_context(self.nc.semaphore(dst.dkey))
        self.phase_sems[-1].append(dst.dkey)

    def barrier(self):
        allev = [(k, self.cnt[k]) for k in ("pe", "act", "dve", "pool") if self.cnt[k] > 0]
        allev += [(k, self.dtot[k]) for k in self.dtot]
        for e in ENGS:
            waits = []
            for k, v in allev:
                if self.seen[e].get(k, 0) >= v:
                    continue
                self.seen[e][k] = v
                waits.append((k, v))
            if waits:
                self.ops[e].append((waits, None, None))

    def bank(self):
        if self.banks is None:
            self.banks = [(self.ps("bank%d_%d" % (i, self.nbuf), [128, 512]), self.buf("bank%d" % i)) for i in range(8)]
        b = self.banks[self.bank_i % 8]
        self.bank_i += 1
        return b

    def sb(self, name, shape, dtype):
        self.nbuf += 1
        return self.stack[-1].enter_context(self.nc.sbuf_tensor("sb%d_%s" % (self.nbuf, name), list(shape), dtype))

    def ps(self, name, shape, dtype=F32):
        self.nbuf += 1
        return self.stack[-1].enter_context(self.nc.psum_tensor("ps%d_%s" % (self.nbuf, name), list(shape), dtype))

    def dram(self, name, shape, dtype, kind):
        return self.nc.dram_tensor(name, list(shape), dtype, kind=kind).ap()

    def buf(self, name=None):
        self.nbuf += 1
        return Buf(name or ("b%d" % self.nbuf))

    def dram_or(self, io, name, shape, dtype, kind):
        if io is not None and name in io:
            return io[name]
        return self.dram(name, shape, dtype, kind)

    def allgather(self, in_ap, out_ap, dst, reads, ncores):
        if dst.dkey is None:
            self._dsem(dst, "c_")
        waits = self._waits("pool", reads, (dst,))
        dst.dcnt += 1
        self.dtot[dst.dkey] = dst.dcnt
        ev = (dst.dkey, dst.dcnt)
        fn = lambda e, i=in_ap, o=out_ap: e.collective_compute(
            "AllGather", ALU.bypass, replica_groups=[list(range(ncores))], ins=[i.opt()], outs=[o.opt()])
        self.ops["pool"].append((waits, fn, (dst.dkey, None)))
        self._commit(ev, reads, (dst,))

    def _waits(self, eng, reads, writes):
        deps = {}

        def add(ev):
            if ev is None:
                return
            k, v = ev
            if deps.get(k, 0) < v:
                deps[k] = v

        for b in reads:
            add(b.w)
        for b in writes:
            add(b.w)
            for k, v in b.r.items():
                add((k, v))
        waits = []
        for k, v in deps.items():
            if k == eng and eng == "pe":
                continue
            if self.seen[eng].get(k, 0) >= v:
                continue
            self.seen[eng][k] = v
            waits.append((k, v))
        return waits

    def _commit(self, ev, reads, writes):
        k, v = ev
        for b in reads:
            if b.r.get(k, 0) < v:
                b.r[k] = v
        for b in writes:
            b.w = ev
            b.r = {}

    def op(self, eng, fn, reads=(), writes=()):
        waits = self._waits(eng, reads, writes)
        self.cnt[eng] += 1
        ev = (eng, self.cnt[eng])
        self.ops[eng].append((waits, fn, (eng, 1)))
        self._commit(ev, reads, writes)

    def dma(self, q, out_ap, in_ap, dst, reads=(), extra_writes=()):
        if dst.dkey is None:
            self._dsem(dst, "d_")
        writes = (dst,) + tuple(extra_writes)
        waits = self._waits(q, reads, writes)
        dst.dcnt += 16
        self.dtot[dst.dkey] = dst.dcnt
        ev = (dst.dkey, dst.dcnt)
        self.ops[q].append((waits, lambda e, o=out_ap, i=in_ap: e.dma_start(out=o, in_=i), (dst.dkey, 16)))
        self._commit(ev, reads, writes)

    def store(self, q, out_ap, in_ap, src, also=()):
        if src.skey is None:
            if self.free_dsems:
                src.skey = self.free_dsems.pop()
                src.scnt = self.dtot[src.skey]
            else:
                src.skey = "s_" + src.name + "_%d" % len(self.sems)
                self.sems[src.skey] = self.es.enter_context(self.nc.semaphore(src.skey))
            self.phase_sems[-1].append(src.skey)
        waits = self._waits(q, [src] + list(also), ())
        src.scnt += 16
        self.dtot[src.skey] = src.scnt
        self.ops[q].append((waits, lambda e, o=out_ap, i=in_ap: e.dma_start(out=o, in_=i), (src.skey, 16)))
        for b in [src] + list(also):
            b.r[src.skey] = src.scnt

    def finish(self, bufs, eng="sp"):
        waits = self._waits(eng, bufs, ())
        self.ops[eng].append((waits, None, None))

    def emit(self):
        self.flush()
        while self.stack:
            self.stack.pop().close()
        return self.nc

    def flush(self):
        nc = self.nc
        if not any(self.ops[e] for e in ENGS):
            return
        ops = self.ops
        self.ops = {e: [] for e in ENGS}
        with nc.Block() as block:
            def run(engname):
                def body(e):
                    for waits, fn, inc in ops[engname]:
                        for k, v in waits:
                            e.wait_ge(self.sems[k], v)
                        if fn is not None:
                            ins = fn(e)
                            if inc[1] is None:
                                ins.then_inc(self.sems[inc[0]])
                            else:
                                ins.then_inc(self.sems[inc[0]], inc[1])
                return body
            block.tensor(run("pe"))
            block.scalar(run("act"))
            block.vector(run("dve"))
            block.gpsimd(run("pool"))
            block.sync(run("sp"))


def mm_group(out_ap, pairs):
    def fn(e):
        n = len(pairs)
        ins = None
        for i, (l, r) in enumerate(pairs):
            ins = e.matmul(out_ap, l, r, start=(i == 0), stop=(i == n - 1))
        return ins
    return fn


class Common:
    def __init__(self, P, ln_g_ap, ln_b_ap, ident_ap, nslots=2):
        self.P = P
        nc = P.nc
        self.ident_f = P.sb("ident_f", [128, 128], F32)
        self.ident = P.sb("ident_bf", [128, 128], BF16)
        self.lng = P.sb("lng", [128, D_MODEL], F32)
        self.lnb = P.sb("lnb", [128, D_MODEL], F32)
        self.neghalf = P.sb("neghalf", [128, 1], F32)
        self.b_ident = P.buf("ident")
        self.b_identf = P.buf("identf")
        self.b_lng = P.buf("lng")
        self.b_lnb = P.buf("lnb")
        self.b_nh = P.buf("nh")
        P.dma("sp", self.ident_f[:], ident_ap, self.b_identf)
        P.dma("sp", self.lng[:], ln_g_ap, self.b_lng)
        P.dma("sp", self.lnb[:], ln_b_ap, self.b_lnb)
        P.op("dve", lambda e: e.tensor_copy(self.ident[:], self.ident_f[:]), reads=[self.b_identf], writes=[self.b_ident])
        P.op("pool", lambda e: e.memset(self.neghalf[:], -0.5), writes=[self.b_nh])
        NS = self.NS = nslots
        self.s = [P.sb("ln_s%d" % i, [128, D_MODEL], F32) for i in range(NS)]
        self.b_s = [P.buf("ln_s%d" % i) for i in range(NS)]
        self.st = [P.sb("ln_st%d" % i, [128, 2, 6], F32) for i in range(NS)]
        self.mv = [P.sb("ln_mv%d" % i, [128, 2], F32) for i in range(NS)]
        self.rs = [P.sb("ln_rs%d" % i, [128, 2], F32) for i in range(NS)]
        self.b_small = [P.buf("ln_small%d" % i) for i in range(NS)]
        self.xo = [P.sb("ln_xo%d" % i, [128, D_MODEL], F32) for i in range(NS)]
        self.b_xo = [P.buf("ln_xo%d" % i) for i in range(NS)]
        self.k = 0

    def layernorm(self, fill_s, fill_reads, out_dram_ap, out_buf, post=None):
        P = self.P
        i = self.k % self.NS
        self.k += 1
        s, st, mv, rs, xo = self.s[i], self.st[i], self.mv[i], self.rs[i], self.xo[i]
        bs, bsm, bxo = self.b_s[i], self.b_small[i], self.b_xo[i]
        for eng, fn in fill_s(s):
            P.op(eng, fn, reads=fill_reads, writes=[bs])
        P.op("dve", lambda e: e.bn_stats(st[:, 0, :], s[:, 0:512]), reads=[bs], writes=[bsm])
        P.op("dve", lambda e: e.bn_stats(st[:, 1, :], s[:, 512:1024]), reads=[bs], writes=[bsm])
        P.op("dve", lambda e: e.bn_aggr(mv[:], st[:].rearrange("p a b -> p (a b)")), reads=[bsm], writes=[bsm])
        P.op("pool", lambda e: e.tensor_scalar(rs[:, 0:1], mv[:, 1:2], LN_EPS, None, ALU.add), reads=[bsm], writes=[bsm])
        P.op("pool", lambda e: e.tensor_tensor(rs[:, 1:2], rs[:, 0:1], self.neghalf[:], ALU.pow), reads=[bsm, self.b_nh], writes=[bsm])
        P.op("dve", lambda e: e.tensor_scalar(s[:], s[:], mv[:, 0:1], rs[:, 1:2], ALU.subtract, ALU.mult), reads=[bs, bsm], writes=[bs])
        P.op("pool", lambda e: e.tensor_tensor(xo[:], s[:], self.lng[:], ALU.mult), reads=[bs, self.b_lng], writes=[bxo])
        P.op("pool", lambda e: e.tensor_tensor(xo[:], xo[:], self.lnb[:], ALU.add), reads=[bxo, self.b_lnb], writes=[bxo])
        if out_dram_ap is not None:
            P.store("sp", out_dram_ap, xo[:], bxo)
        if post is not None:
            post(xo, bxo)


def build_ffn(T, E, TG=1024, P=None, io=None):
    own = P is None
    if own:
        P = Prog()
    P.push()
    nc = P.nc
    NT = T // 128
    NG = T // TG
    TPG = TG // 128
    NTB = TG // 512
    NJB = D_FF // 512

    x_d = P.dram_or(io, "x", [T, D_MODEL], F32, "ExternalInput")
    y_d = P.dram_or(io, "y", [T, D_MODEL], F32, "ExternalOutput")
    lng_d = P.dram_or(io, "ln_g", [128, D_MODEL], F32, "ExternalInput")
    lnb_d = P.dram_or(io, "ln_b", [128, D_MODEL], F32, "ExternalInput")
    id_d = P.dram_or(io, "ident", [128, 128], F32, "ExternalInput")
    wg_d = P.dram_or(io, "wg", [E, D_MODEL, D_FF], F32, "ExternalInput")
    wu_d = P.dram_or(io, "wu", [E, D_MODEL, D_FF], F32, "ExternalInput")
    wd_d = P.dram_or(io, "wd", [E, D_FF, D_MODEL], F32, "ExternalInput")
    if E > 1:
        wr_d = P.dram_or(io, "wr", [D_MODEL, E], F32, "ExternalInput")

    C = Common(P, lng_d, lnb_d, id_d, nslots=2)
    b_y = P.buf("y_dram")

    xT = P.sb("xT", [128, KC, TG], BF16)
    b_xT = [P.buf("xT%d" % t) for t in range(TPG)]
    acc = P.sb("acc", [128, TPG, D_MODEL], F32)
    b_acc = [P.buf("acc%d" % t) for t in range(TPG)]
    NXS = 3
    xs = [P.sb("xs%d" % i, [128, D_MODEL], F32) for i in range(NXS)]
    b_xs = [P.buf("xs%d" % i) for i in range(NXS)]
    xb = [P.sb("xb%d" % i, [128, D_MODEL], BF16) for i in range(NXS)]
    b_xb = [P.buf("xb%d" % i) for i in range(NXS)]
    wg_s = [P.sb("wg%d" % i, [128, KC, 512], BF16) for i in range(2)]
    wu_s = [P.sb("wu%d" % i, [128, KC, 512], BF16) for i in range(2)]
    wd_s = [P.sb("wd%d" % i, [128, 4, D_MODEL], BF16) for i in range(2)]
    b_wg = [P.buf("wg%d" % i) for i in range(2)]
    b_wu = [P.buf("wu%d" % i) for i in range(2)]
    b_wd = [P.buf("wd%d" % i) for i in range(2)]
    hT = [P.sb("hT%d" % i, [128, 4, 512], BF16) for i in range(2)]
    b_hT = [[P.buf("hT%d_%d" % (i, c)) for c in range(4)] for i in range(2)]
    sg = [P.sb("sg%d" % i, [128, 512], F32) for i in range(2)]
    b_sg = [P.buf("sg%d" % i) for i in range(2)]
    if E > 1:
        wr_f = P.sb("wr_f", [128, KC, E], F32)
        wr_s = P.sb("wr_s", [128, KC, E], BF16)
        b_wrf = P.buf("wrf")
        b_wr = P.buf("wr")
        lg = P.sb("lg", [128, TPG, E], F32)
        top = P.sb("top", [128, TPG, 8], F32)
        gsm = P.sb("gsm", [128, TPG, 4], F32)
        gA = P.sb("gA", [128, TPG, E], F32)
        gates = P.sb("gates", [128, TPG, E], F32)
        b_g = [P.buf("gate%d" % t) for t in range(TPG)]
        P.dma("sp", wr_f[:], wr_d.rearrange("(k p) e -> p k e", p=128), b_wrf)
        P.op("dve", lambda e: e.tensor_copy(wr_s[:], wr_f[:]), reads=[b_wrf], writes=[b_wr])

    pg = [P.ps("pg%d" % i, [128, 512]) for i in range(2)]
    pu = [P.ps("pu%d" % i, [128, 512]) for i in range(2)]
    pd = [P.ps("pd%d" % i, [128, 512]) for i in range(2)]
    pt = [P.ps("pt%d" % i, [128, 512]) for i in range(2)]
    b_pg = [P.buf("pg%d" % i) for i in range(2)]
    b_pu = [P.buf("pu%d" % i) for i in range(2)]
    b_pd = [P.buf("pd%d" % i) for i in range(2)]
    b_pt = [P.buf("pt%d" % i) for i in range(2)]

    blocks = [(g, e, jb) for g in range(NG) for e in range(E) for jb in range(NJB)]

    def load_w(idx):
        g, e, jb = blocks[idx]
        s = idx % 2
        c0 = jb * 512
        P.dma("pool", wg_s[s][:], wg_d[e, :, c0:c0 + 512].rearrange("(k p) c -> p k c", p=128), b_wg[s])
        P.dma("pool", wu_s[s][:], wu_d[e, :, c0:c0 + 512].rearrange("(k p) c -> p k c", p=128), b_wu[s])
        P.dma("pool", wd_s[s][:], wd_d[e, c0:c0 + 512, :].rearrange("(k p) c -> p k c", p=128), b_wd[s])

    load_w(0)
    if len(blocks) > 1:
        load_w(1)

    cchunk = [0]
    cdown = [0]
    cunit = [0]

    def emit_gu(idx, tb):
        g, e, jb = blocks[idx]
        s = idx % 2
        hs = cunit[0] % 2
        for c in range(4):
            k = cchunk[0] % 2
            cchunk[0] += 1
            rhs = lambda kc: xT[:, kc, tb * 512:(tb + 1) * 512]
            rd = [b_xT[tb * 4 + i] for i in range(4)]
            P.op("pe", mm_group(pg[k][:], [(wg_s[s][:, kc, c * 128:(c + 1) * 128], rhs(kc)) for kc in range(KC)]),
                 reads=[b_wg[s]] + rd, writes=[b_pg[k]])
            P.op("pe", mm_group(pu[k][:], [(wu_s[s][:, kc, c * 128:(c + 1) * 128], rhs(kc)) for kc in range(KC)]),
                 reads=[b_wu[s]] + rd, writes=[b_pu[k]])
            P.op("act", lambda en, k=k: en.activation(sg[k][:], pg[k][:], AF.Silu), reads=[b_pg[k]], writes=[b_sg[k]])
            P.op("dve", lambda en, k=k, c=c, hs=hs: en.tensor_tensor(hT[hs][:, c, :], sg[k][:], pu[k][:], ALU.mult),
                 reads=[b_sg[k], b_pu[k]], writes=[b_hT[hs][c]])
        u = (idx, tb, hs)
        cunit[0] += 1
        return u

    def emit_down(u, first):
        idx, tb, hs = u
        g, e, jb = blocks[idx]
        s = idx % 2
        for t in range(4):
            tt = tb * 4 + t
            for nb in range(2):
                k = cdown[0] % 2
                cdown[0] += 1
                P.op("pe", mm_group(pd[k][:], [(hT[hs][:, c, t * 128:(t + 1) * 128], wd_s[s][:, c, nb * 512:(nb + 1) * 512]) for c in range(4)]),
                     reads=[b_wd[s]] + b_hT[hs], writes=[b_pd[k]])
                a = acc[:, tt, nb * 512:(nb + 1) * 512]
                if E > 1:
                    gsc = gates[:, tt, e:e + 1]
                    if first:
                        P.op("dve", lambda en, a=a, k=k, gsc=gsc: en.tensor_scalar(a, pd[k][:], gsc, None, ALU.mult),
                             reads=[b_pd[k], b_g[tt]], writes=[b_acc[tt]])
                    else:
                        P.op("dve", lambda en, a=a, k=k, gsc=gsc: en.scalar_tensor_tensor(a, pd[k][:], gsc, a, ALU.mult, ALU.add),
                             reads=[b_pd[k], b_g[tt], b_acc[tt]], writes=[b_acc[tt]])
                else:
                    if first:
                        P.op("dve", lambda en, a=a, k=k: en.tensor_copy(a, pd[k][:]), reads=[b_pd[k]], writes=[b_acc[tt]])
                    else:
                        P.op("dve", lambda en, a=a, k=k: en.tensor_tensor(a, a, pd[k][:], ALU.add),
                             reads=[b_pd[k], b_acc[tt]], writes=[b_acc[tt]])

    xcount = [0]

    def load_x_tile(g, t):
        i = xcount[0] % NXS
        xcount[0] += 1
        r0 = g * TG + t * 128
        P.dma("sp", xs[i][:], x_d[r0:r0 + 128, :], b_xs[i])
        return i

    bidx = 0
    for g in range(NG):
        for t in range(TPG):
            i = load_x_tile(g, t)
            P.op("act", lambda en, i=i: en.copy(xb[i][:], xs[i][:]), reads=[b_xs[i]], writes=[b_xb[i]])
            for half in range(2):
                for q in range(4):
                    kc = half * 4 + q
                    P.op("pe", mm_group(pt[half][:, q * 128:(q + 1) * 128], [(xb[i][:, kc * 128:(kc + 1) * 128], C.ident[:])]),
                         reads=[b_xb[i], C.b_ident], writes=[b_pt[half]])
                P.op("dve", lambda en, half=half, t=t: en.tensor_copy(
                    xT[:, half * 4:(half + 1) * 4, t * 128:(t + 1) * 128], pt[half][:].rearrange("p (a b) -> p a b", a=4)),
                    reads=[b_pt[half]], writes=[b_xT[t]])
            if E > 1:
                P.op("pe", mm_group(pt[0][:, 0:E], [(xT[:, kc, t * 128:(t + 1) * 128], wr_s[:, kc, :]) for kc in range(KC)]),
                     reads=[b_xT[t], b_wr], writes=[b_pt[0]])
                L = lg[:, t, :]
                P.op("dve", lambda en, L=L: en.tensor_copy(L, pt[0][:, 0:E]), reads=[b_pt[0]], writes=[b_g[t]])
                P.op("dve", lambda en, L=L, t=t: en.max(top[:, t, :], L), reads=[b_g[t]], writes=[b_g[t]])
                P.op("dve", lambda en, t=t: en.tensor_tensor(gsm[:, t, 0:1], top[:, t, 1:2], top[:, t, 0:1], ALU.subtract), reads=[b_g[t]], writes=[b_g[t]])
                P.op("act", lambda en, t=t: en.activation(gsm[:, t, 1:2], gsm[:, t, 0:1], AF.Exp), reads=[b_g[t]], writes=[b_g[t]])
                P.op("dve", lambda en, t=t: en.tensor_scalar(gsm[:, t, 2:3], gsm[:, t, 1:2], 1.0, None, ALU.add), reads=[b_g[t]], writes=[b_g[t]])
                P.op("dve", lambda en, t=t: en.reciprocal(gsm[:, t, 2:3], gsm[:, t, 2:3]), reads=[b_g[t]], writes=[b_g[t]])
                P.op("dve", lambda en, t=t: en.tensor_tensor(gsm[:, t, 3:4], gsm[:, t, 1:2], gsm[:, t, 2:3], ALU.mult), reads=[b_g[t]], writes=[b_g[t]])
                P.op("dve", lambda en, t=t: en.tensor_tensor(gsm[:, t, 0:1], gsm[:, t, 2:3], gsm[:, t, 3:4], ALU.subtract), reads=[b_g[t]], writes=[b_g[t]])
                P.op("dve", lambda en, L=L, t=t: en.tensor_scalar(gA[:, t, :], L, top[:, t, 1:2], gsm[:, t, 3:4], ALU.is_ge, ALU.mult), reads=[b_g[t]], writes=[b_g[t]])
                P.op("dve", lambda en, L=L, t=t: en.tensor_scalar(gates[:, t, :], L, top[:, t, 0:1], gsm[:, t, 0:1], ALU.is_ge, ALU.mult), reads=[b_g[t]], writes=[b_g[t]])
                P.op("dve", lambda en, t=t: en.tensor_tensor(gates[:, t, :], gates[:, t, :], gA[:, t, :], ALU.add), reads=[b_g[t]], writes=[b_g[t]])
        pending = None
        for e in range(E):
            for jb in range(NJB):
                first = (e == 0 and jb == 0)
                for tb in range(NTB):
                    u = emit_gu(bidx, tb)
                    if pending is not None:
                        emit_down(*pending)
                    pending = (u, first)
                bidx += 1
                if bidx + 1 < len(blocks):
                    if pending is not None:
                        emit_down(*pending)
                        pending = None
                    load_w(bidx + 1)
        if pending is not None:
            emit_down(*pending)
            pending = None
        LA = 1
        slots = {}
        for t in range(min(LA, TPG)):
            slots[t] = load_x_tile(g, t)
        for t in range(TPG):
            if t + LA < TPG:
                slots[t + LA] = load_x_tile(g, t + LA)
            i = slots[t]
            r0 = g * TG + t * 128

            def fill(s, i=i, t=t):
                return [("dve", lambda en: en.scalar_tensor_tensor(s[:], xs[i][:], float(ALPHA), acc[:, t, :], ALU.mult, ALU.add))]
            C.layernorm(fill, [b_xs[i], b_acc[t]], y_d[r0:r0 + 128, :], b_y)
    P.finish([b_y])
    P.pop()
    if own:
        return P.emit()


def _rep(v):
    return np.ascontiguousarray(np.broadcast_to(np.asarray(v, np.float32)[None, :], (128, v.shape[-1])))


def run_ffn(x_shards, ln_g, ln_b, wg, wu, wd, wr=None, TG=1024):
    T = x_shards[0].shape[0]
    E = wg.shape[0]
    nc = build_ffn(T, E, TG=min(TG, T))
    ident = np.eye(128, dtype=np.float32)
    maps = []
    for xs in x_shards:
        m = {"x": np.ascontiguousarray(xs), "ln_g": _rep(ln_g), "ln_b": _rep(ln_b), "ident": ident,
             "wg": wg, "wu": wu, "wd": wd}
        if E > 1:
            m["wr"] = wr
        maps.append(m)
    res = run_bass_kernel_spmd(nc, maps, core_ids=list(range(len(maps))))
    return [r["y"] for r in res.results]


def emit_proj_ln(P, T, KD, x_d, u_d, w_d, rowgain_d, lng_d, lnb_d, id_d, y_d, b_u, src_fm):
    NE = KD // 128
    NTL = T // 128
    P.push()
    C = Common(P, lng_d, lnb_d, id_d, nslots=4)
    b_y = P.buf("y_dram")
    w_s = P.sb("pw", [128, NE, D_MODEL], BF16)
    b_w = [P.buf("pw%d" % i) for i in range(NE)]
    if rowgain_d is not None:
        rg = P.sb("rg", [128, NE], F32)
        b_rg = P.buf("rg")
        P.dma("sp", rg[:], rowgain_d, b_rg)
        wst = [P.sb("wst%d" % i, [128, D_MODEL], F32) for i in range(2)]
        b_wst = [P.buf("wst%d" % i) for i in range(2)]
        for ec in range(NE):
            i = ec % 2
            P.dma("sp", wst[i][:], w_d[ec * 128:(ec + 1) * 128, :], b_wst[i])
            P.op("dve", lambda en, i=i, ec=ec: en.tensor_scalar(w_s[:, ec, :], wst[i][:], rg[:, ec:ec + 1], None, ALU.mult),
                 reads=[b_wst[i], b_rg], writes=[b_w[ec]])
    else:
        for ec in range(NE):
            P.dma("pool", w_s[:, ec, :], w_d[ec * 128:(ec + 1) * 128, :], b_w[ec])
    ut = [P.sb("ut%d" % i, [128, KD], BF16) for i in range(4)]
    b_ut = [P.buf("ut%d" % i) for i in range(4)]
    uT = [P.sb("uT%d" % i, [128, NE, 128], BF16) for i in range(4)]
    b_uT = [P.buf("uT%d" % i) for i in range(4)]
    xs = [P.sb("pxs%d" % i, [128, D_MODEL], F32) for i in range(4)]
    b_xs = [P.buf("pxs%d" % i) for i in range(4)]
    def loads(t):
        i = t % 4
        r0 = t * 128
        P.dma("sp", xs[i][:], x_d[r0:r0 + 128, :], b_xs[i])
        if src_fm:
            for ec in range(NE):
                P.dma("sp", uT[i][:, ec, :], u_d[ec * 128:(ec + 1) * 128, r0:r0 + 128], b_uT[i], reads=[b_u])
        else:
            P.dma("sp", ut[i][:], u_d[r0:r0 + 128, :], b_ut[i], reads=[b_u])

    LA = 2
    for t in range(min(LA, NTL)):
        loads(t)
    for t in range(NTL):
        if t + LA < NTL:
            loads(t + LA)
        i = t % 4
        r0 = t * 128
        if not src_fm:
            for q4 in range(NE // 4):
                bk, bb = P.bank()
                for j in range(4):
                    ec = q4 * 4 + j
                    P.op("pe", mm_group(bk[:, j * 128:(j + 1) * 128], [(ut[i][:, ec * 128:(ec + 1) * 128], C.ident[:])]),
                         reads=[b_ut[i], C.b_ident], writes=[bb])
                P.op("act", lambda en, q4=q4, bk=bk, i=i: en.copy(uT[i][:, q4 * 4:(q4 + 1) * 4, :], bk[:].rearrange("p (a b) -> p a b", a=4)),
                     reads=[bb], writes=[b_uT[i]])
        bks = [P.bank(), P.bank()]
        for nb in range(2):
            P.op("pe", mm_group(bks[nb][0][:], [(uT[i][:, ec, :], w_s[:, ec, nb * 512:(nb + 1) * 512]) for ec in range(NE)]),
                 reads=[b_uT[i]] + b_w, writes=[bks[nb][1]])

        def fill(s, i=i, bks=bks):
            return [("dve", lambda en, nb=nb: en.scalar_tensor_tensor(s[:, nb * 512:(nb + 1) * 512], xs[i][:, nb * 512:(nb + 1) * 512],
                                                                     float(ALPHA), bks[nb][0][:], ALU.mult, ALU.add)) for nb in range(2)]
        C.layernorm(fill, [b_xs[i], bks[0][1], bks[1][1]], y_d[r0:r0 + 128, :], b_y)
    P.finish([b_y])
    P.pop()


RET_H = 4
GAMMA = [1.0 - 2.0 ** (-5.0 - h) for h in range(RET_H)]


def build_ret(T, mode, P=None, io=None, gathered=False):
    own = P is None
    if own:
        P = Prog()
    NCH = T // 128
    cdec = [g ** 128 for g in GAMMA]
    full = (mode == "B")
    x_d = P.dram_or(io, "x", [T, D_MODEL], F32, "ExternalInput")
    win_d = P.dram_or(io, "w_in", [D_MODEL, 6144], F32, "ExternalInput")
    id_d = P.dram_or(io, "ident", [128, 128], F32, "ExternalInput")
    cos_d = P.dram_or(io, "cosT", [128, T], F32, "ExternalInput")
    sin_d = P.dram_or(io, "sinT", [128, T], F32, "ExternalInput")
    kdec_d = P.dram_or(io, "kdec", [128, RET_H], F32, "ExternalInput")
    if full:
        y_d = P.dram_or(io, "y", [T, D_MODEL], F32, "ExternalOutput")
        wout_d = P.dram_or(io, "w_out", [2048, D_MODEL], F32, "ExternalInput")
        gn_d = P.dram_or(io, "gn", [128, 16], F32, "ExternalInput")
        lng_d = P.dram_or(io, "ln_g", [128, D_MODEL], F32, "ExternalInput")
        lnb_d = P.dram_or(io, "ln_b", [128, D_MODEL], F32, "ExternalInput")
        dm_d = P.dram_or(io, "dmT", [128, RET_H, 128], F32, "ExternalInput")
        qdec_d = P.dram_or(io, "qdec", [128, RET_H, 128], F32, "ExternalInput")
        if gathered:
            rg_d = io["rg"]
            cf_d = io["ret_cf"]
        else:
            rp_d = P.dram("rprev", [3, RET_H, 2, 128, 512], F32, "ExternalInput")
        u_d = P.dram_or(io, "u_scr", [T, 2048], BF16, "Internal")
        b_u = P.buf("u_dram")
    else:
        r_d = P.dram_or(io, "r_out", [RET_H, 2, 128, 512], F32, "ExternalOutput")
        b_rd = P.buf("r_dram")

    P.push()
    ident_f = P.sb("ident_f", [128, 128], F32)
    ident = P.sb("ident", [128, 128], BF16)
    b_idf, b_id = P.buf("idf"), P.buf("id")
    P.dma("sp", ident_f[:], id_d, b_idf)
    P.op("dve", lambda e: e.tensor_copy(ident[:], ident_f[:]), reads=[b_idf], writes=[b_id])
    kdec = P.sb("kdec", [128, RET_H], F32)
    b_kdec = P.buf("kdec")
    P.dma("sp", kdec[:], kdec_d, b_kdec)
    w_in = P.sb("w_in", [128, KC, 6144], BF16)
    b_win = [P.buf("win%d" % k) for k in range(KC)]
    for kc in range(KC):
        P.dma("pool", w_in[:, kc, :], win_d[kc * 128:(kc + 1) * 128, :], b_win[kc])
    R = P.sb("R", [128, RET_H, 2, 512], F32)
    Rb = P.sb("Rb", [128, RET_H, 2, 512], BF16)
    b_R = [P.buf("R%d" % h) for h in range(RET_H)]
    b_Rb = [P.buf("Rb%d" % h) for h in range(RET_H)]
    if full:
        dmT = P.sb("dmT", [128, RET_H, 128], F32)
        qdec = P.sb("qdec", [128, RET_H, 128], F32)
        b_dm, b_qdec = P.buf("dm"), P.buf("qdec")
        P.dma("sp", dmT[:], dm_d, b_dm)
        P.dma("sp", qdec[:], qdec_d, b_qdec)
        epsb = P.sb("eps_b", [128, 1], F32)
        nhb = P.sb("nh_b", [128, 1], F32)
        b_cst = P.buf("cst")
        P.op("pool", lambda e: e.memset(nhb[:], -0.5), writes=[b_cst])
        rtmp = [P.sb("rtmp%d" % i, [128, 2, 512], F32) for i in range(2)]
        b_rtmp = [P.buf("rtmp%d" % i) for i in range(2)]
        cnt = 0
        if gathered:
            cf = P.sb("ret_cf", [128, NCORES * RET_H], F32)
            b_cf = P.buf("ret_cf")
            P.dma("sp", cf[:], cf_d, b_cf)
            for p in range(NCORES):
                for h in range(RET_H):
                    i = cnt % 2
                    cnt += 1
                    csc = cf[:, p * RET_H + h:p * RET_H + h + 1]
                    P.dma("sp", rtmp[i][:], rg_d[p, h].rearrange("c p e -> p c e"), b_rtmp[i])
                    if p == 0:
                        P.op("dve", lambda en, i=i, h=h, csc=csc: en.tensor_scalar(R[:, h, :, :], rtmp[i][:], csc, None, ALU.mult),
                             reads=[b_rtmp[i], b_cf], writes=[b_R[h]])
                    else:
                        P.op("dve", lambda en, i=i, h=h, csc=csc: en.scalar_tensor_tensor(R[:, h, :, :], rtmp[i][:], csc, R[:, h, :, :], ALU.mult, ALU.add),
                             reads=[b_rtmp[i], b_cf, b_R[h]], writes=[b_R[h]])
        else:
            for h in range(RET_H):
                P.dma("sp", R[:, h, :, :], rp_d[0, h].rearrange("c p e -> p c e"), b_R[h])
            for k in (1, 2):
                for h in range(RET_H):
                    coef = float(GAMMA[h] ** (T * k))
                    i = cnt % 2
                    cnt += 1
                    P.dma("sp", rtmp[i][:], rp_d[k, h].rearrange("c p e -> p c e"), b_rtmp[i])
                    P.op("dve", lambda en, i=i, h=h, coef=coef: en.scalar_tensor_tensor(R[:, h, :, :], rtmp[i][:], coef, R[:, h, :, :], ALU.mult, ALU.add),
                         reads=[b_rtmp[i], b_R[h]], writes=[b_R[h]])
        for h in range(RET_H):
            P.op("act", lambda en, h=h: en.copy(Rb[:, h, :, :], R[:, h, :, :]), reads=[b_R[h]], writes=[b_Rb[h]])
    else:
        for h in range(RET_H):
            P.op("pool", lambda en, h=h: en.memset(R[:, h, :, :], 0.0), writes=[b_R[h]])

    def dbl(name, shape, dt):
        return [P.sb("%s%d" % (name, i), shape, dt) for i in range(2)], [P.buf("%s%d" % (name, i)) for i in range(2)]
    xb = [P.sb("xb%d" % i, [128, D_MODEL], BF16) for i in range(3)]
    b_xb = [P.buf("xb%d" % i) for i in range(3)]
    xT, b_xT = dbl("xT", [128, KC, 128], BF16)
    cs = [P.sb("cs%d" % i, [128, 2, 128], F32) for i in range(3)]
    b_cs = [P.buf("cs%d" % i) for i in range(3)]
    qk = [[P.sb("qk%d_%d" % (i, h), [128, 4, 128], BF16) for h in range(RET_H)] for i in range(2)]
    b_qk = [[P.buf("qk%d_%d" % (i, h)) for h in range(RET_H)] for i in range(2)]
    kd = [[P.sb("kd%d_%d" % (i, h), [128, 256], BF16) for h in range(RET_H)] for i in range(2)]
    b_kd = [[P.buf("kd%d_%d" % (i, h)) for h in range(RET_H)] for i in range(2)]
    v, b_v = dbl("v", [128, 2048], BF16)
    b_vh = [[P.buf("v%d_%d" % (i, h)) for h in range(RET_H)] for i in range(2)]
    t1, b_t1 = dbl("t1", [128, 4, 128], F32)
    t2, b_t2 = dbl("t2", [128, 4, 128], F32)
    if full:
        sgt = [P.sb("sgt%d" % i, [128, 2048], BF16) for i in range(2)]
        b_sgt = [[P.buf("sgt%d_%d" % (i, h)) for h in range(RET_H)] for i in range(2)]
        PT = P.sb("PT", [128, RET_H, 128], BF16)
        b_PT = P.buf("PT")
        qd = [P.sb("qd%d" % h, [128, 2, 128], BF16) for h in range(RET_H)]
        b_qd = [P.buf("qd%d" % h) for h in range(RET_H)]
        u = [P.sb("u%d" % i, [128, 2048], BF16) for i in range(2)]
        b_us = [P.buf("u%d" % i) for i in range(2)]
        b_uh = [[P.buf("u%d_%d" % (i, h)) for h in range(RET_H)] for i in range(2)]
        gst = P.sb("gst", [128, RET_H, 6], F32)
        gmv = P.sb("gmv", [128, RET_H, 2], F32)
        grs = P.sb("grs", [128, RET_H, 2], F32)
        b_gs = [P.buf("gs%d" % h) for h in range(RET_H)]

    def prefetch(n):
        i3 = n % 3
        r0 = n * 128
        P.dma("pool", xb[i3][:], x_d[r0:r0 + 128, :], b_xb[i3])
        P.dma("sp", cs[i3][:, 0, :], cos_d[:, r0:r0 + 128], b_cs[i3])
        P.dma("sp", cs[i3][:, 1, :], sin_d[:, r0:r0 + 128], b_cs[i3])

    def stage1(n):
        i = n % 2
        i3 = n % 3
        r0 = n * 128
        for half in range(2):
            bk, bb = P.bank()
            for q in range(4):
                kc = half * 4 + q
                P.op("pe", mm_group(bk[:, q * 128:(q + 1) * 128], [(xb[i3][:, kc * 128:(kc + 1) * 128], ident[:])]),
                     reads=[b_xb[i3], b_id], writes=[bb])
            P.op("dve", lambda en, half=half, bk=bk: en.tensor_copy(xT[i][:, half * 4:(half + 1) * 4, :], bk[:].rearrange("p (a b) -> p a b", a=4)),
                 reads=[bb], writes=[b_xT[i]])
        lo = 0 if full else 2
        for h in range(RET_H):
            bk, bb = P.bank()
            for q4, cc in enumerate([2 * h, 2 * h + 1, 8 + 2 * h, 8 + 2 * h + 1]):
                if q4 < lo:
                    continue
                P.op("pe", mm_group(bk[:, q4 * 128:(q4 + 1) * 128], [(w_in[:, kc, cc * 128:(cc + 1) * 128], xT[i][:, kc, :]) for kc in range(KC)]),
                     reads=b_win + [b_xT[i]], writes=[bb])
            j = h % 2
            nq = 4 - lo
            bk3 = bk[:].rearrange("p (a b) -> p a b", a=4)[:, lo:4, :]
            cosb = cs[i3][:, 0, :].unsqueeze(1).broadcast_to([128, nq, 128])
            sinb = cs[i3][:, 1, :].unsqueeze(1).broadcast_to([128, nq, 128])
            P.op("dve", lambda en, j=j, bk3=bk3, cosb=cosb: en.tensor_tensor(t1[j][:, lo:4, :], bk3, cosb, ALU.mult), reads=[bb, b_cs[i3]], writes=[b_t1[j]])
            P.op("dve", lambda en, j=j, bk3=bk3, sinb=sinb: en.tensor_tensor(t2[j][:, lo:4, :], bk3, sinb, ALU.mult), reads=[bb, b_cs[i3]], writes=[b_t2[j]])
            a0 = lo // 2
            t1v = t1[j][:].rearrange("p (a b) c -> p a b c", a=2)[:, a0:2]
            t2v = t2[j][:].rearrange("p (a b) c -> p a b c", a=2)[:, a0:2]
            qkv = qk[i][h][:].rearrange("p (a b) c -> p a b c", a=2)[:, a0:2]
            P.op("pool", lambda en, t1v=t1v, t2v=t2v, qkv=qkv: en.tensor_tensor(qkv[:, :, 0, :], t1v[:, :, 0, :], t2v[:, :, 1, :], ALU.subtract),
                 reads=[b_t1[j], b_t2[j]], writes=[b_qk[i][h]])
            P.op("pool", lambda en, t1v=t1v, t2v=t2v, qkv=qkv: en.tensor_tensor(qkv[:, :, 1, :], t1v[:, :, 1, :], t2v[:, :, 0, :], ALU.add),
                 reads=[b_t1[j], b_t2[j], b_qk[i][h]], writes=[b_qk[i][h]])
        for nb in range(8 if full else 4):
            bk, bb = P.bank()
            c0 = 2048 + nb * 512
            P.op("pe", mm_group(bk[:], [(xT[i][:, kc, :], w_in[:, kc, c0:c0 + 512]) for kc in range(KC)]), reads=b_win + [b_xT[i]], writes=[bb])
            if nb < 4:
                P.op("act", lambda en, nb=nb, bk=bk: en.copy(v[i][:, nb * 512:(nb + 1) * 512], bk[:]), reads=[bb], writes=[b_vh[i][nb]])
            else:
                hh = nb - 4
                P.op("act", lambda en, hh=hh, bk=bk: en.activation(sgt[i][:, hh * 512:(hh + 1) * 512], bk[:], AF.Silu), reads=[bb], writes=[b_sgt[i][hh]])
        for h in range(RET_H):
            bk2, bb2 = P.bank()
            for dc in range(2):
                P.op("pe", mm_group(bk2[:, dc * 128:(dc + 1) * 128], [(qk[i][h][:, 2 + dc, :], ident[:])]), reads=[b_qk[i][h], b_id], writes=[bb2])
            P.op("act", lambda en, h=h, bk2=bk2: en.activation(kd[i][h][:], bk2[:, 0:256], AF.Copy, scale=kdec[:, h:h + 1]),
                 reads=[bb2, b_kdec], writes=[b_kd[i][h]])

    def stage2(n):
        i = n % 2
        r0 = n * 128
        if n + 2 < NCH:
            prefetch(n + 2)
        if full:
            bkS, bbS = P.bank()
            for h in range(RET_H):
                P.op("pe", mm_group(bkS[:, h * 128:(h + 1) * 128], [(qk[i][h][:, 2, :], qk[i][h][:, 0, :]), (qk[i][h][:, 3, :], qk[i][h][:, 1, :])]),
                     reads=[b_qk[i][h]], writes=[bbS])
            P.op("dve", lambda en, bkS=bkS: en.tensor_tensor(PT[:], bkS[:].rearrange("p (a b) -> p a b", a=4), dmT[:], ALU.mult),
                 reads=[bbS, b_dm], writes=[b_PT])
            for h in range(RET_H):
                qdb = qdec[:, h, :].unsqueeze(1).broadcast_to([128, 2, 128])
                P.op("pool", lambda en, h=h, qdb=qdb: en.tensor_tensor(qd[h][:], qk[i][h][:, 0:2, :], qdb, ALU.mult),
                     reads=[b_qk[i][h], b_qdec], writes=[b_qd[h]])
        for h in range(RET_H):
            for dc in range(2):
                bk, bb = P.bank()
                P.op("pe", mm_group(bk[:], [(kd[i][h][:, dc * 128:(dc + 1) * 128], v[i][:, h * 512:(h + 1) * 512])]),
                     reads=[b_kd[i][h], b_vh[i][h]], writes=[bb])
                P.op("dve", lambda en, h=h, dc=dc, bk=bk: en.scalar_tensor_tensor(R[:, h, dc, :], R[:, h, dc, :], float(cdec[h]), bk[:], ALU.mult, ALU.add),
                     reads=[bb, b_R[h]], writes=[b_R[h]])
        if full:
            bR = []
            for h in range(RET_H):
                bkR, bbR = P.bank()
                bR.append((bkR, bbR))
                P.op("pe", mm_group(bkR[:], [(PT[:, h, :], v[i][:, h * 512:(h + 1) * 512]),
                                            (qd[h][:, 0, :], Rb[:, h, 0, :]), (qd[h][:, 1, :], Rb[:, h, 1, :])]),
                     reads=[b_PT, b_vh[i][h], b_qd[h], b_Rb[h]], writes=[bbR])
            for h in range(RET_H):
                bkR, bbR = bR[h]
                P.op("dve", lambda en, h=h, bkR=bkR: en.bn_stats(gst[:, h, :], bkR[:]), reads=[bbR], writes=[b_gs[h]])
                P.op("dve", lambda en, h=h: en.bn_aggr(gmv[:, h, :], gst[:, h, :]), reads=[b_gs[h]], writes=[b_gs[h]])
            for h in range(RET_H):
                P.op("pool", lambda en, h=h: en.tensor_scalar(grs[:, h, 0:1], gmv[:, h, 1:2], LN_EPS, None, ALU.add), reads=[b_gs[h]], writes=[b_gs[h]])
                P.op("pool", lambda en, h=h: en.tensor_tensor(grs[:, h, 1:2], grs[:, h, 0:1], nhb[:], ALU.pow), reads=[b_gs[h], b_cst], writes=[b_gs[h]])
            for h in range(RET_H):
                bkR, bbR = bR[h]
                us = u[i][:, h * 512:(h + 1) * 512]
                P.op("dve", lambda en, h=h, bkR=bkR, us=us: en.tensor_scalar(us, bkR[:], gmv[:, h, 0:1], grs[:, h, 1:2], ALU.subtract, ALU.mult),
                     reads=[bbR, b_gs[h]], writes=[b_uh[i][h]])
            for h in range(RET_H):
                us = u[i][:, h * 512:(h + 1) * 512]
                P.op("pool", lambda en, h=h, us=us: en.tensor_tensor(us, us, sgt[i][:, h * 512:(h + 1) * 512], ALU.mult),
                     reads=[b_uh[i][h], b_sgt[i][h]], writes=[b_uh[i][h]])
            P.store("sp", u_d[r0:r0 + 128, :], u[i][:], b_us[i], also=b_uh[i])
            for h in range(RET_H):
                P.op("act", lambda en, h=h: en.copy(Rb[:, h, :, :], R[:, h, :, :]), reads=[b_R[h]], writes=[b_Rb[h]])

    prefetch(0)
    if NCH > 1:
        prefetch(1)
    stage1(0)
    for n in range(NCH):
        if n + 1 < NCH:
            stage1(n + 1)
        stage2(n)
    if not full:
        for h in range(RET_H):
            P.store("sp", r_d[h].rearrange("c p e -> p c e"), R[:, h, :, :], b_R[h])
        P.finish([b_rd])
    P.pop()
    if full:
        emit_proj_ln(P, T, 2048, x_d, u_d, wout_d, gn_d, lng_d, lnb_d, id_d, y_d, b_u, src_fm=False)
    if own:
        return P.emit()


def ret_consts(T, pos0):
    half = 128
    inv = (10000.0 ** (-np.arange(half, dtype=np.float32) / half)).astype(np.float32)
    pos = (pos0 + np.arange(T)).astype(np.float32)
    ang = pos[None, :] * inv[:, None]
    idx = np.arange(128, dtype=np.float64)
    dm = np.zeros((128, RET_H, 128), np.float32)
    qdec = np.zeros((128, RET_H, 128), np.float32)
    kdec = np.zeros((128, RET_H), np.float32)
    for h in range(RET_H):
        lg = np.log(GAMMA[h])
        rel = idx[None, :] - idx[:, None]
        dm[:, h, :] = np.where(rel >= 0, np.exp(lg * np.maximum(rel, 0.0)), 0.0) / 16.0
        qdec[:, h, :] = np.exp(lg * (idx + 1.0))[None, :]
        kdec[:, h] = np.exp(lg * (127.0 - idx)) / 16.0
    return {"cosT": np.cos(ang).astype(np.float32), "sinT": np.sin(ang).astype(np.float32),
            "dmT": dm, "qdec": qdec, "kdec": kdec}


def run_ret(mode, x_shards, pos0s, w_in, w_out=None, gn=None, ln_g=None, ln_b=None, rprevs=None):
    T = x_shards[0].shape[0]
    nc = build_ret(T, mode)
    ident = np.eye(128, dtype=np.float32)
    maps = []
    for c, xs in enumerate(x_shards):
        cst = ret_consts(T, pos0s[c])
        m = {"x": np.ascontiguousarray(xs), "w_in": w_in, "ident": ident, "cosT": cst["cosT"], "sinT": cst["sinT"], "kdec": cst["kdec"]}
        if mode == "B":
            m.update({"w_out": w_out, "gn": np.ascontiguousarray(gn.reshape(16, 128).T), "ln_g": _rep(ln_g), "ln_b": _rep(ln_b),
                      "dmT": cst["dmT"], "qdec": cst["qdec"], "rprev": rprevs[c]})
        maps.append(m)
    res = run_bass_kernel_spmd(nc, maps, core_ids=list(range(len(maps))))
    return [r["r_out" if mode == "A" else "y"] for r in res.results]


DIL = (1, 4, 16)
HALO = 2048
ATT_COLS = 4608


def build_att(T, debug=False, P=None, io=None):
    own = P is None
    if own:
        P = Prog()
    SK = "ExternalOutput" if debug else "Internal"
    H = HALO
    TE = H + T
    xe_d = P.dram_or(io, "x_ext", [TE, D_MODEL], F32, "ExternalInput")
    xh_d = io.get("x_halo") if io is not None else None
    y_d = P.dram_or(io, "y", [T, D_MODEL], F32, "ExternalOutput")
    w_d = P.dram_or(io, "w_qkv", [D_MODEL, ATT_COLS], F32, "ExternalInput")
    wo_d = P.dram_or(io, "w_out", [512, D_MODEL], F32, "ExternalInput")
    id_d = P.dram_or(io, "ident", [128, 128], F32, "ExternalInput")
    lng_d = P.dram_or(io, "ln_g", [128, D_MODEL], F32, "ExternalInput")
    lnb_d = P.dram_or(io, "ln_b", [128, D_MODEL], F32, "ExternalInput")
    mask_d = P.dram_or(io, "mask2", [128, 256], F32, "ExternalInput")
    hv_d = P.dram_or(io, "halo_valid", [128, 1], F32, "ExternalInput")
    qT_d = P.dram_or(io, "qT_scr", [12, 128, T], BF16, SK)
    kT_d = P.dram_or(io, "kT_scr", [12, 128, TE], BF16, SK)
    v_d = P.dram_or(io, "v_scr", [TE, 1536], BF16, SK)
    oT_d = P.dram_or(io, "oT_scr", [512, T], BF16, SK)
    b_qd, b_kd, b_vd, b_od = P.buf("qT_d"), P.buf("kT_d"), P.buf("v_d"), P.buf("oT_d")

    P.push()
    ident_f = P.sb("ident_f", [128, 128], F32)
    ident = P.sb("ident", [128, 128], BF16)
    b_idf, b_id = P.buf("idf"), P.buf("id")
    P.dma("sp", ident_f[:], id_d, b_idf)
    P.op("dve", lambda e: e.tensor_copy(ident[:], ident_f[:]), reads=[b_idf], writes=[b_id])
    w_s = P.sb("wqkv", [128, KC, ATT_COLS], BF16)
    b_w = [P.buf("wqkv%d" % k) for k in range(KC)]
    for kc in range(KC):
        P.dma("pool", w_s[:, kc, :], w_d[kc * 128:(kc + 1) * 128, :], b_w[kc])
    xb = [P.sb("xb%d" % i, [128, 4, D_MODEL], BF16) for i in range(2)]
    b_xbt = [[P.buf("xb%d_%d" % (i, t)) for t in range(4)] for i in range(2)]
    xT = [P.sb("xT%d" % i, [128, KC, 512], BF16) for i in range(2)]
    b_xT = [P.buf("xT%d" % i) for i in range(2)]
    stg = [P.sb("stg%d" % i, [128, 512], BF16) for i in range(4)]
    b_stg = [P.buf("stg%d" % i) for i in range(4)]
    vst = [P.sb("vst%d" % i, [128, 1536], BF16) for i in range(2)]
    b_vst = [P.buf("vst%d" % i) for i in range(2)]
    nstg = 0
    nv = 0
    for blk in range(TE // 512):
        i = blk % 2
        t0 = blk * 512
        for tt in range(4):
            if xh_d is not None and t0 < H:
                P.dma("pool", xb[i][:, tt, :], xh_d[t0 + tt * 128:t0 + (tt + 1) * 128, :], b_xbt[i][tt])
            else:
                P.dma("pool", xb[i][:, tt, :], xe_d[t0 + tt * 128:t0 + (tt + 1) * 128, :], b_xbt[i][tt])
        for tt in range(4):
            for half in range(2):
                bk, bb = P.bank()
                for q in range(4):
                    kc = half * 4 + q
                    P.op("pe", mm_group(bk[:, q * 128:(q + 1) * 128], [(xb[i][:, tt, kc * 128:(kc + 1) * 128], ident[:])]),
                         reads=[b_xbt[i][tt], b_id], writes=[bb])
                P.op("dve", lambda en, half=half, bk=bk, tt=tt, i=i: en.tensor_copy(
                    xT[i][:, half * 4:(half + 1) * 4, tt * 128:(tt + 1) * 128], bk[:].rearrange("p (a b) -> p a b", a=4)),
                    reads=[bb], writes=[b_xT[i]])
        is_q = t0 >= H
        for cc in range(24):
            if cc < 12 and not is_q:
                continue
            bk, bb = P.bank()
            P.op("pe", mm_group(bk[:], [(w_s[:, kc, cc * 128:(cc + 1) * 128], xT[i][:, kc, :]) for kc in range(KC)]),
                 reads=b_w + [b_xT[i]], writes=[bb])
            s = nstg % 4
            nstg += 1
            eng = "act" if (nstg % 2) else "dve"
            if eng == "act":
                P.op("act", lambda en, s=s, bk=bk: en.copy(stg[s][:], bk[:]), reads=[bb], writes=[b_stg[s]])
            else:
                P.op("dve", lambda en, s=s, bk=bk: en.tensor_copy(stg[s][:], bk[:]), reads=[bb], writes=[b_stg[s]])
            if cc < 12:
                P.store("sp", qT_d[cc, :, t0 - H:t0 - H + 512], stg[s][:], b_stg[s])
            else:
                P.store("sp", kT_d[cc - 12, :, t0:t0 + 512], stg[s][:], b_stg[s])
        for tt in range(4):
            s = nv % 2
            nv += 1
            for g in range(3):
                bk, bb = P.bank()
                c0 = 3072 + g * 512
                P.op("pe", mm_group(bk[:], [(xT[i][:, kc, tt * 128:(tt + 1) * 128], w_s[:, kc, c0:c0 + 512]) for kc in range(KC)]),
                     reads=b_w + [b_xT[i]], writes=[bb])
                P.op("act", lambda en, s=s, g=g, bk=bk: en.copy(vst[s][:, g * 512:(g + 1) * 512], bk[:]), reads=[bb], writes=[b_vst[s]])
            P.store("sp", v_d[t0 + tt * 128:t0 + (tt + 1) * 128, :], vst[s][:], b_vst[s])
    P.pop()

    P.push()
    mask_f = P.sb("mask_f", [128, 256], F32)
    mask2 = P.sb("mask2", [128, 256], BF16)
    maskH = P.sb("maskH", [128, 256], BF16)
    hv = P.sb("hv", [128, 1], F32)
    ones = P.sb("ones", [128, 128], BF16)
    b_mf, b_m2, b_mH, b_hv, b_ones = P.buf("mf"), P.buf("m2"), P.buf("mH"), P.buf("hv"), P.buf("ones")
    P.dma("sp", mask_f[:], mask_d, b_mf)
    P.dma("sp", hv[:], hv_d, b_hv)
    P.op("dve", lambda e: e.tensor_copy(mask2[:], mask_f[:]), reads=[b_mf], writes=[b_m2])
    P.op("dve", lambda e: e.tensor_copy(maskH[:, 128:256], mask_f[:, 128:256]), reads=[b_mf], writes=[b_mH])
    P.op("dve", lambda e: e.tensor_scalar(maskH[:, 0:128], mask_f[:, 0:128], hv[:, 0:1], None, ALU.mult), reads=[b_mf, b_hv, b_mH], writes=[b_mH])
    P.op("pool", lambda e: e.memset(ones[:], 1.0), writes=[b_ones])
    acc = P.sb("acc", [128, 2, T], F32)
    b_acc = P.buf("acc")
    qT = [P.sb("qT%d" % i, [128, T], BF16) for i in range(2)]
    b_qT = [P.buf("qT%d" % i) for i in range(2)]
    kT = [P.sb("kT%d" % i, [128, TE], BF16) for i in range(2)]
    b_kT = [P.buf("kT%d" % i) for i in range(2)]
    NBMAX = T // 128 + 1
    NVT = 4
    vt = [P.sb("vt%d" % i, [128, NBMAX, 128], BF16) for i in range(NVT)]
    b_vt = [P.buf("vt%d" % i) for i in range(NVT)]
    NET = 4
    ET = [P.sb("ET%d" % i, [128, 256], BF16) for i in range(NET)]
    b_ET = [P.buf("ET%d" % i) for i in range(NET)]
    oT = P.sb("oT", [128, T], BF16)
    b_oT = P.buf("oT")
    rz = P.sb("rz", [128, T], F32)
    b_rz = P.buf("rz")
    scale = float(128 ** -0.5)
    nqk = 0
    nvt = 0
    net = [0]
    for h in range(4):
        for g in range(3):
            d = DIL[g]
            Hg = 128 * d
            L = Hg + T
            i = nqk % 2
            nqk += 1
            P.dma("sp", qT[i][:], qT_d[g * 4 + h], b_qT[i], reads=[b_qd])
            P.dma("sp", kT[i][:, 0:L], kT_d[g * 4 + h, :, H - Hg:H + T], b_kT[i], reads=[b_kd])
            qv = qT[i][:].rearrange("p (m d) -> p d m", d=d)
            kv = kT[i][:, 0:L].rearrange("p (m d) -> p d m", d=d)
            av = acc[:].rearrange("p c (m d) -> p c d m", d=d)
            na = T // (128 * d)
            nb = na + 1
            tiles = []
            vsrc = {}
            for r in range(d):
                j = nvt % NVT
                nvt += 1
                vsrc[r] = (j, bass.AP(v_d.tensor, (H - Hg + r) * 1536 + g * 512 + h * 128,
                                      [[d * 1536, 128], [128 * d * 1536, nb], [1, 128]]))
                for a in range(na):
                    tiles.append((i, j, r, a, g, qv, kv, av))
            def sA(tl):
                i, j, r, a, g, qv, kv, av = tl
                bk, bb = P.bank()
                qa = qv[:, r, a * 128:(a + 1) * 128]
                P.op("pe", mm_group(bk[:, 0:128], [(kv[:, r, a * 128:(a + 1) * 128], qa)]), reads=[b_kT[i], b_qT[i]], writes=[bb])
                P.op("pe", mm_group(bk[:, 128:256], [(kv[:, r, (a + 1) * 128:(a + 2) * 128], qa)]), reads=[b_kT[i], b_qT[i]], writes=[bb])
                e = net[0] % NET
                net[0] += 1
                P.op("act", lambda en, e=e, bk=bk: en.activation(ET[e][:], bk[:, 0:256], AF.Exp, scale=scale), reads=[bb], writes=[b_ET[e]])
                mk, bmk = (maskH, b_mH) if a == 0 else (mask2, b_m2)
                P.op("dve", lambda en, e=e, mk=mk: en.tensor_tensor(ET[e][:], ET[e][:], mk[:], ALU.mult), reads=[b_ET[e], bmk], writes=[b_ET[e]])
                return e

            def sB(tl, e):
                i, j, r, a, g, qv, kv, av = tl
                bo, bbo = P.bank()
                P.op("pe", mm_group(bo[:, 0:128], [(vt[j][:, a, :], ET[e][:, 0:128]), (vt[j][:, a + 1, :], ET[e][:, 128:256])]),
                     reads=[b_vt[j], b_ET[e]], writes=[bbo])
                P.op("pe", mm_group(bo[:, 128:256], [(ones[:], ET[e][:, 0:128]), (ones[:], ET[e][:, 128:256])]),
                     reads=[b_ones, b_ET[e]], writes=[bbo])
                dst = av[:, :, r, a * 128:(a + 1) * 128]
                bo3 = bo[:, 0:256].rearrange("p (c m) -> p c m", c=2)
                if g == 0:
                    P.op("dve", lambda en, dst=dst, bo3=bo3: en.tensor_copy(dst, bo3), reads=[bbo], writes=[b_acc])
                else:
                    P.op("dve", lambda en, dst=dst, bo3=bo3: en.tensor_tensor(dst, dst, bo3, ALU.add), reads=[bbo, b_acc], writes=[b_acc])

            pend = []
            for tl in tiles:
                if tl[3] == 0:
                    jj, src = vsrc[tl[2]]
                    P.dma("sp", vt[jj][:, 0:nb, :], src, b_vt[jj], reads=[b_vd])
                pend.append((tl, sA(tl)))
                if len(pend) > 2:
                    sB(*pend.pop(0))
            for pp in pend:
                sB(*pp)
        P.op("dve", lambda en: en.reciprocal(rz[:], acc[:, 1, :]), reads=[b_acc], writes=[b_rz])
        P.op("dve", lambda en: en.tensor_tensor(oT[:], acc[:, 0, :], rz[:], ALU.mult), reads=[b_acc, b_rz], writes=[b_oT])
        P.store("sp", oT_d[h * 128:(h + 1) * 128, :], oT[:], b_oT)
    P.pop()

    emit_proj_ln(P, T, 512, xe_d[H:TE, :], oT_d, wo_d, None, lng_d, lnb_d, id_d, y_d, b_od, src_fm=True)
    if own:
        return P.emit()


def att_mask():
    j = np.arange(128)[:, None]
    i = np.arange(128)[None, :]
    return np.concatenate([(j >= i), (j <= i)], axis=1).astype(np.float32)


def run_att(x_ext_shards, halo_valid, w_qkv, w_out, ln_g, ln_b, debug=False):
    T = x_ext_shards[0].shape[0] - HALO
    nc = build_att(T, debug)
    ident = np.eye(128, dtype=np.float32)
    maps = []
    for c, xe in enumerate(x_ext_shards):
        maps.append({"x_ext": np.ascontiguousarray(xe), "w_qkv": w_qkv, "w_out": w_out, "ident": ident,
                     "ln_g": _rep(ln_g), "ln_b": _rep(ln_b), "mask2": att_mask(),
                     "halo_valid": np.full((128, 1), float(halo_valid[c]), np.float32)})
    res = run_bass_kernel_spmd(nc, maps, core_ids=list(range(len(maps))))
    if debug:
        return res.results
    return [r["y"] for r in res.results]


def emit_halo(P, T, E1, hsrc, Hg, Xh, sel_d):
    H = HALO
    NA = H // 128
    P.push()
    b_hs, b_hg = P.buf("hsrc"), P.buf("Hg")
    P.dma("pool", hsrc, E1[T:H + T, :], b_hs)
    P.allgather(hsrc, Hg, b_hg, [b_hs], NCORES)
    sel = P.sb("hsel", [128, NCORES], F32)
    b_sel = P.buf("hsel")
    P.dma("sp", sel[:], sel_d, b_sel)
    cand = [P.sb("hcand%d" % i, [128, NA, D_MODEL], BF16) for i in range(2)]
    b_cand = [P.buf("hcand%d" % i) for i in range(2)]
    acc = P.sb("hacc", [128, NA, D_MODEL], BF16)
    b_acc = P.buf("hacc")
    for p in range(NCORES):
        i = p % 2
        P.dma("sp", cand[i][:], Hg[p * H:(p + 1) * H, :].rearrange("(a q) c -> q a c", q=128), b_cand[i], reads=[b_hg])
        sc = sel[:, p:p + 1]
        if p == 0:
            P.op("dve", lambda en, i=i, sc=sc: en.tensor_scalar(acc[:], cand[i][:], sc, None, ALU.mult),
                 reads=[b_cand[i], b_sel], writes=[b_acc])
        else:
            P.op("dve", lambda en, i=i, sc=sc: en.scalar_tensor_tensor(acc[:], cand[i][:], sc, acc[:], ALU.mult, ALU.add),
                 reads=[b_cand[i], b_sel, b_acc], writes=[b_acc])
    P.store("sp", Xh.rearrange("(a q) c -> q a c", q=128), acc[:], b_acc)
    P.pop()


def build_fused(T):
    P = Prog()
    H = HALO
    inp = lambda n, shp: P.dram(n, shp, F32, "ExternalInput")
    x_d = inp("x", [T, D_MODEL])
    y_d = P.dram("y", [T, D_MODEL], F32, "ExternalOutput")
    lng = inp("ln_g", [2 * DEPTH, 128, D_MODEL])
    lnb = inp("ln_b", [2 * DEPTH, 128, D_MODEL])
    rwin = inp("ret_w_in", [2, D_MODEL, 6144])
    rwout = inp("ret_w_out", [2, 2048, D_MODEL])
    rgn = inp("ret_gn", [2, 128, 16])
    awq = inp("att_w_qkv", [2, D_MODEL, ATT_COLS])
    awo = inp("att_w_out", [2, 512, D_MODEL])
    fwg = inp("ffn_wg", [2, 1, D_MODEL, D_FF])
    fwu = inp("ffn_wu", [2, 1, D_MODEL, D_FF])
    fwd = inp("ffn_wd", [2, 1, D_FF, D_MODEL])
    mwr = inp("moe_wr", [2, D_MODEL, N_EXPERTS])
    mwg = inp("moe_wg", [2, N_EXPERTS, D_MODEL, D_FF])
    mwu = inp("moe_wu", [2, N_EXPERTS, D_MODEL, D_FF])
    mwd = inp("moe_wd", [2, N_EXPERTS, D_FF, D_MODEL])
    ident = inp("ident", [128, 128])
    cosT, sinT = inp("cosT", [128, T]), inp("sinT", [128, T])
    kdec, dmT, qdec = inp("kdec", [128, RET_H]), inp("dmT", [128, RET_H, 128]), inp("qdec", [128, RET_H, 128])
    mask2, hvalid = inp("mask2", [128, 256]), inp("halo_valid", [128, 1])
    hsel, rcf = inp("halo_sel", [128, NCORES]), inp("ret_cf", [128, NCORES * RET_H])
    itn = lambda n, shp, dt=F32: P.dram(n, shp, dt, "Internal")
    E1 = itn("E1", [H + T, D_MODEL])
    M = itn("Mmid", [T, D_MODEL])
    E2 = itn("E2", [T, D_MODEL])
    hsrc = itn("hsrc", [H, D_MODEL], BF16)
    Hg = itn("Hg", [NCORES * H, D_MODEL], BF16)
    Xh = itn("Xh", [H, D_MODEL], BF16)
    Rl = itn("Rl", [RET_H * 2 * 128, 512])
    Rg = itn("Rg", [NCORES * RET_H * 2 * 128, 512])
    scr = {"u_scr": itn("u_scr", [T, 2048], BF16), "qT_scr": itn("qT_scr", [12, 128, T], BF16),
           "kT_scr": itn("kT_scr", [12, 128, H + T], BF16), "v_scr": itn("v_scr", [H + T, 1536], BF16),
           "oT_scr": itn("oT_scr", [512, T], BF16)}
    Rl5 = Rl.rearrange("(h c p) e -> h c p e", h=RET_H, c=2)
    Rg5 = Rg.rearrange("(n h c p) e -> n h c p e", n=NCORES, h=RET_H, c=2)
    cur = x_d
    for i in range(DEPTH):
        j = i // 2
        last = (i == DEPTH - 1)
        if i % 2 == 0:
            base = {"x": cur, "w_in": rwin[j], "ident": ident, "cosT": cosT, "sinT": sinT, "kdec": kdec}
            ioA = dict(base)
            ioA["r_out"] = Rl5
            build_ret(T, "A", P=P, io=ioA)
            P.push()
            b_rg = P.buf("Rg")
            P.allgather(Rl, Rg, b_rg, [], NCORES)
            P.pop()
            ioB = dict(base)
            ioB.update({"y": M, "w_out": rwout[j], "gn": rgn[j], "ln_g": lng[2 * i], "ln_b": lnb[2 * i], "dmT": dmT, "qdec": qdec,
                        "rg": Rg5, "ret_cf": rcf, "u_scr": scr["u_scr"]})
            build_ret(T, "B", P=P, io=ioB, gathered=True)
            out = E1[H:H + T, :]
            build_ffn(T, 1, TG=min(T, 2048), P=P, io={"x": M, "y": out, "ln_g": lng[2 * i + 1], "ln_b": lnb[2 * i + 1], "ident": ident,
                                     "wg": fwg[j], "wu": fwu[j], "wd": fwd[j]})
            cur = out
        else:
            emit_halo(P, T, E1, hsrc, Hg, Xh, hsel)
            ioT = {"x_halo": Xh, "x_ext": E1, "y": M, "w_qkv": awq[j], "w_out": awo[j], "ident": ident, "ln_g": lng[2 * i], "ln_b": lnb[2 * i],
                   "mask2": mask2, "halo_valid": hvalid}
            ioT.update({k: scr[k] for k in ("qT_scr", "kT_scr", "v_scr", "oT_scr")})
            build_att(T, P=P, io=ioT)
            out = y_d if last else E2
            build_ffn(T, N_EXPERTS, TG=min(T, 2048), P=P, io={"x": M, "y": out, "ln_g": lng[2 * i + 1], "ln_b": lnb[2 * i + 1], "ident": ident,
                                             "wg": mwg[j], "wu": mwu[j], "wd": mwd[j], "wr": mwr[j]})
            cur = out
    return P.emit()


_PROGS = {}


def kernel(x, ln_gain, ln_bias, ret_w_in, ret_gn_gain, ret_w_out, att_w_qkv, att_w_out,
           ffn_w_gate, ffn_w_up, ffn_w_down, moe_w_router, moe_w_gate, moe_w_up, moe_w_down):
    f = lambda a: np.ascontiguousarray(np.asarray(a, dtype=np.float32))
    x = f(x)
    B, S, D = x.shape
    T = (B * S) // NCORES
    CPB = NCORES // B
    if T not in _PROGS:
        _PROGS[T] = build_fused(T)
    nc = _PROGS[T]
    lg, lb = f(ln_gain).reshape(2 * DEPTH, D), f(ln_bias).reshape(2 * DEPTH, D)
    shared = {
        "ln_g": np.ascontiguousarray(np.broadcast_to(lg[:, None, :], (2 * DEPTH, 128, D))),
        "ln_b": np.ascontiguousarray(np.broadcast_to(lb[:, None, :], (2 * DEPTH, 128, D))),
        "ret_w_in": f(ret_w_in), "ret_w_out": f(ret_w_out),
        "ret_gn": np.ascontiguousarray(f(ret_gn_gain).reshape(2, 16, 128).transpose(0, 2, 1)),
        "att_w_qkv": f(att_w_qkv), "att_w_out": f(att_w_out),
        "ffn_wg": f(ffn_w_gate)[:, None], "ffn_wu": f(ffn_w_up)[:, None], "ffn_wd": f(ffn_w_down)[:, None],
        "moe_wr": f(moe_w_router), "moe_wg": f(moe_w_gate), "moe_wu": f(moe_w_up), "moe_wd": f(moe_w_down),
        "ident": np.eye(128, dtype=np.float32), "mask2": att_mask(),
    }
    xf = x.reshape(B * S, D)
    maps = []
    for c in range(NCORES):
        cst = ret_consts(T, (c % CPB) * T)
        first = (c % CPB == 0)
        hs = np.zeros((128, NCORES), np.float32)
        if not first:
            hs[:, c - 1] = 1.0
        cf = np.zeros((128, NCORES, RET_H), np.float32)
        for p in range(NCORES):
            if p < c and p // CPB == c // CPB:
                for h in range(RET_H):
                    cf[:, p, h] = GAMMA[h] ** (T * (c - 1 - p))
        m = dict(shared)
        m.update({"x": xf[c * T:(c + 1) * T], "cosT": cst["cosT"], "sinT": cst["sinT"], "kdec": cst["kdec"], "dmT": cst["dmT"],
                  "qdec": cst["qdec"], "halo_valid": np.full((128, 1), 0.0 if first else 1.0, np.float32),
                  "halo_sel": hs, "ret_cf": cf.reshape(128, NCORES * RET_H)})
        maps.append(m)
    res = run_bass_kernel_spmd(nc, maps, core_ids=list(range(NCORES))).results
    return np.concatenate([r["y"] for r in res], axis=0).reshape(B, S, D).astype(np.float32)
```
